# Optimizing a Trainium2 kernel written in Bass

```python
import math
import jax, jax.numpy as jnp
from jax import lax
import numpy as np

D_MODEL = 1024
BATCH = 8
SEQ = 4096
DEPTH = 4

MIX_WIDTH = D_MODEL
HALF = MIX_WIDTH // 2
S5_GROUP = 16
S5_GROUPS = HALF // S5_GROUP
S5_STATE = 64
ML_HEADS = 4
ML_DV = HALF // ML_HEADS
ML_DK = ML_DV // 2
ML_CHUNK = 64
ML_NORM_EPS = 1e-6
RW_HEAD = 64
RW_HEADS = HALF // RW_HEAD
RW_DECAY_LORA = 64
RW_AAA_LORA = 64
RW_GATE_LORA = 128
RW_GN_EPS = 64e-5
GD_HEADS = 4
GD_HEAD = HALF // GD_HEADS
GD_CONV = 4
GD_CHUNK = 64
D_FF = 2816
N_EXPERTS = 8
TOP_K = 2
D_FF_EXPERT = 3584
N_EVEN = (DEPTH + 1) // 2
N_ODD = DEPTH // 2
DN_ALPHA = (2 * DEPTH) ** 0.25
DN_BETA = (8 * DEPTH) ** -0.25
LN_EPS = 1e-5
EVEN_COLS = HALF + 2 * ML_HEADS * ML_DK + 2 * HALF + 2 * ML_HEADS
RW_COLS = 3 * HALF + RW_DECAY_LORA + RW_AAA_LORA + RW_GATE_LORA
ODD_COLS = RW_COLS + 4 * HALF + 2 * GD_HEADS

kernel_name = 'hybrid_s5_mlstm_rwkv7_gdn_moe'


def _split(z, sizes):
    return jnp.split(z, np.cumsum(sizes)[:-1].tolist(), axis=-1)


def _to_heads(t, n_heads):
    b, s, _ = t.shape
    return t.reshape(b, s, n_heads, -1).transpose(0, 2, 1, 3)


def _from_heads(t):
    b, h, s, d = t.shape
    return t.transpose(0, 2, 1, 3).reshape(b, s, h * d)


def _to_chunks(t, size):
    b, h, s = t.shape[:3]
    t = t.reshape((b, h, s // size, size) + t.shape[3:])
    return jnp.moveaxis(t, 2, 0)


def _from_chunks(t):
    t = jnp.moveaxis(t, 0, 2)
    b, h, nc, l = t.shape[:4]
    return t.reshape((b, h, nc * l) + t.shape[4:])


def _l2norm(t):
    return t * lax.rsqrt(jnp.sum(t * t, -1, keepdims=True) + 1e-6)


def _layer_norm(x, g, b):
    xf = x.astype(jnp.float32)
    mu = jnp.mean(xf, -1, keepdims=True)
    var = jnp.mean(jnp.square(xf - mu), -1, keepdims=True)
    return ((xf - mu) * lax.rsqrt(var + LN_EPS) * g + b).astype(x.dtype)


def _cplx_combine(ei, ej):
    ar_i, ai_i, br_i, bi_i = ei
    ar_j, ai_j, br_j, bi_j = ej
    return (ar_j * ar_i - ai_j * ai_i,
            ar_j * ai_i + ai_j * ar_i,
            ar_j * br_i - ai_j * bi_i + br_j,
            ar_j * bi_i + ai_j * br_i + bi_j)


def _s5(u, lam_re, lam_im, log_dt, b_re, b_im, c_re, c_im, d_skip, w_glu, b_glu):
    bsz, s, _ = u.shape
    lam_re = lam_re.astype(jnp.float32)
    lam_im = lam_im.astype(jnp.float32)
    dt = jnp.exp(log_dt.astype(jnp.float32))[:, None]
    mag = jnp.exp(lam_re * dt)
    ar, ai = mag * jnp.cos(lam_im * dt), mag * jnp.sin(lam_im * dt)
    den = lam_re * lam_re + lam_im * lam_im
    fr = ((ar - 1.0) * lam_re + ai * lam_im) / den
    fi = (ai * lam_re - (ar - 1.0) * lam_im) / den
    bbr = fr[..., None] * b_re - fi[..., None] * b_im
    bbi = fr[..., None] * b_im + fi[..., None] * b_re
    ut = u.reshape(bsz, s, S5_GROUPS, S5_GROUP).transpose(1, 0, 2, 3)
    bur = jnp.einsum('sbgh,gph->sbgp', ut, bbr)
    bui = jnp.einsum('sbgh,gph->sbgp', ut, bbi)
    a_r = jnp.broadcast_to(ar, (s, 1) + ar.shape)
    a_i = jnp.broadcast_to(ai, (s, 1) + ai.shape)
    _, _, xr, xi = lax.associative_scan(_cplx_combine, (a_r, a_i, bur, bui), axis=0)
    y = jnp.einsum('sbgp,ghp->sbgh', xr, c_re) - jnp.einsum('sbgp,ghp->sbgh', xi, c_im)
    y = y.transpose(1, 0, 2, 3).reshape(bsz, s, HALF) + d_skip * u
    y = jax.nn.gelu(y)
    return y * jax.nn.sigmoid(y @ w_glu + b_glu)


def _mlstm(q, k, v, o_pre, i_pre, f_pre, norm_g):
    bsz = q.shape[0]
    qh = _to_chunks(_to_heads(q, ML_HEADS) * ML_DK ** -0.5, ML_CHUNK)
    kh = _to_chunks(_to_heads(k, ML_HEADS), ML_CHUNK)
    vh = _to_chunks(_to_heads(v, ML_HEADS), ML_CHUNK)
    ig = _to_chunks(i_pre.transpose(0, 2, 1), ML_CHUNK)
    lf = _to_chunks(jax.nn.log_sigmoid(f_pre).transpose(0, 2, 1), ML_CHUNK)
    causal = jnp.tril(jnp.ones((ML_CHUNK, ML_CHUNK), bool))

    def step(carry, inp):
        c_mat, n_vec, m = carry
        qc, kc, vc, ic, fc = inp
        b = jnp.cumsum(fc, axis=-1)
        log_d = jnp.where(causal, b[..., :, None] - b[..., None, :] + ic[..., None, :], -jnp.inf)
        log_inter = b + m[..., None]
        m_t = jnp.maximum(log_inter, jnp.max(log_d, -1))
        d_mat = jnp.exp(log_d - m_t[..., None])
        inter = jnp.exp(log_inter - m_t)
        s_mat = jnp.einsum('bhtd,bhsd->bhts', qc, kc) * d_mat
        num = jnp.einsum('bhts,bhsv->bhtv', s_mat, vc) + inter[..., None] * jnp.einsum('bhtd,bhdv->bhtv', qc, c_mat)
        den = jnp.sum(s_mat, -1) + inter * jnp.einsum('bhtd,bhd->bht', qc, n_vec)
        h = num / jnp.maximum(jnp.abs(den), jnp.exp(-m_t))[..., None]
        b_last = b[..., -1]
        log_s = b_last[..., None] - b + ic
        m_new = jnp.maximum(b_last + m, jnp.max(log_s, -1))
        w_s = jnp.exp(log_s - m_new[..., None])
        dec = jnp.exp(b_last + m - m_new)
        c_mat = dec[..., None, None] * c_mat + jnp.einsum('bhs,bhsd,bhsv->bhdv', w_s, kc, vc)
        n_vec = dec[..., None] * n_vec + jnp.einsum('bhs,bhsd->bhd', w_s, kc)
        return (c_mat, n_vec, m_new), h

    init = (jnp.zeros((bsz, ML_HEADS, ML_DK, ML_DV), jnp.float32),
            jnp.zeros((bsz, ML_HEADS, ML_DK), jnp.float32),
            jnp.zeros((bsz, ML_HEADS), jnp.float32))
    _, h = lax.scan(step, init, (qh, kh, vh, ig, lf))
    h = _from_chunks(h)
    mu = jnp.mean(h, -1, keepdims=True)
    var = jnp.mean(jnp.square(h - mu), -1, keepdims=True)
    h = (h - mu) * lax.rsqrt(var + ML_NORM_EPS)
    return _from_heads(h) * norm_g * jax.nn.sigmoid(o_pre)


def _rwkv7(feat, mu, w0, w_up, a0, a_up, g_up, k_k, k_a, r_k, ln_g, ln_b):
    bsz, s, _ = feat.shape
    prev = jnp.pad(feat, ((0, 0), (1, 0), (0, 0)))[:, :-1]
    feat = feat + mu * (prev - feat)
    r, k, v, wd, ad, gd = _split(feat, [HALF, HALF, HALF, RW_DECAY_LORA, RW_AAA_LORA, RW_GATE_LORA])
    log_w = -jnp.exp(-jax.nn.softplus(-(w0 + jnp.tanh(wd) @ w_up)) - 0.5)
    a = jax.nn.sigmoid(a0 + ad @ a_up)
    g = jax.nn.sigmoid(gd) @ g_up

    def hs(t):
        return t.reshape(bsz, s, RW_HEADS, RW_HEAD)

    kk = _l2norm(hs(k * k_k))
    k = k * (1.0 + (a - 1.0) * k_a)
    rh, kh, vh, ah, wh = hs(r), hs(k), hs(v), hs(a), hs(jnp.exp(log_w))

    def tm(t):
        return jnp.moveaxis(t, 1, 0)

    def step(state, inp):
        rt, wt, kt, vt, kkt, at = inp
        sa = jnp.einsum('bhvk,bhk->bhv', state, -kkt)
        state = (state * wt[:, :, None, :] + sa[..., None] * (kkt * at)[:, :, None, :]
                 + vt[..., None] * kt[:, :, None, :])
        return state, jnp.einsum('bhvk,bhk->bhv', state, rt)

    init = jnp.zeros((bsz, RW_HEADS, RW_HEAD, RW_HEAD), jnp.float32)
    _, y = lax.scan(step, init, (tm(rh), tm(wh), tm(kh), tm(vh), tm(kk), tm(ah)))
    y = jnp.moveaxis(y, 0, 1)
    ym = jnp.mean(y, -1, keepdims=True)
    yv = jnp.mean(jnp.square(y - ym), -1, keepdims=True)
    y = ((y - ym) * lax.rsqrt(yv + RW_GN_EPS)).reshape(bsz, s, HALF) * ln_g + ln_b
    bonus = jnp.sum(rh * kh * r_k.reshape(RW_HEADS, RW_HEAD), -1, keepdims=True) * vh
    return (y + bonus.reshape(bsz, s, HALF)) * g


def _causal_dwconv(x, w):
    c = x.shape[-1]
    return lax.conv_general_dilated(x, w.astype(x.dtype)[:, None, :], window_strides=(1,),
                                    padding=[(GD_CONV - 1, 0)],
                                    dimension_numbers=('NWC', 'WIO', 'NWC'),
                                    feature_group_count=c)


def _gated_deltanet(q, k, v, gate, beta_pre, alpha_pre, conv_w, a_log, dt_bias, norm_g):
    bsz = q.shape[0]
    qkv = jax.nn.silu(_causal_dwconv(jnp.concatenate([q, k, v], -1), conv_w))
    q, k, v = _split(qkv, [HALF, HALF, HALF])
    L = GD_CHUNK
    qh = _to_chunks(_l2norm(_to_heads(q, GD_HEADS)) * GD_HEAD ** -0.5, L)
    kh = _to_chunks(_l2norm(_to_heads(k, GD_HEADS)), L)
    vh = _to_chunks(_to_heads(v, GD_HEADS), L)
    beta = _to_chunks(jax.nn.sigmoid(beta_pre).transpose(0, 2, 1), L)
    g = -jnp.exp(a_log)[:, None] * jax.nn.softplus(alpha_pre + dt_bias).transpose(0, 2, 1)
    dec = jnp.cumsum(_to_chunks(g, L), axis=-1)
    causal = jnp.tril(jnp.ones((L, L), bool))
    strict = jnp.tril(jnp.ones((L, L), bool), -1)
    gam = jnp.exp(jnp.where(causal, dec[..., :, None] - dec[..., None, :], -jnp.inf))
    kb = kh * beta[..., None]
    a_mat = jnp.where(strict, jnp.einsum('...td,...sd->...ts', kb, kh) * gam, 0.0)
    eye = jnp.eye(L, dtype=a_mat.dtype)
    rhs = jnp.concatenate([vh * beta[..., None], kb * jnp.exp(dec)[..., None]], -1)
    sol = lax.linalg.triangular_solve(a_mat + eye, rhs, left_side=True, lower=True)
    u_val, w_cum = sol[..., :GD_HEAD], sol[..., GD_HEAD:]
    qk = jnp.einsum('...td,...sd->...ts', qh, kh) * gam
    q_dec = qh * jnp.exp(dec)[..., None]
    k_dec = kh * jnp.exp(dec[..., -1:] - dec)[..., None]
    d_last = jnp.exp(dec[..., -1])

    def step(state, inp):
        qk_c, qd_c, kd_c, u_c, w_c, dl = inp
        u = u_c - jnp.einsum('bhld,bhdv->bhlv', w_c, state)
        o = jnp.einsum('bhld,bhdv->bhlv', qd_c, state) + jnp.einsum('bhts,bhsv->bhtv', qk_c, u)
        state = dl[..., None, None] * state + jnp.einsum('bhsd,bhsv->bhdv', kd_c, u)
        return state, o

    init = jnp.zeros((bsz, GD_HEADS, GD_HEAD, GD_HEAD), jnp.float32)
    _, o = lax.scan(step, init, (qk, q_dec, k_dec, u_val, w_cum, d_last))
    o = _from_chunks(o)
    o = o * lax.rsqrt(jnp.mean(o * o, -1, keepdims=True) + 1e-6) * norm_g
    return _from_heads(o) * jax.nn.silu(gate)


def _even_mixer(x, w_in, w_out, lam_re, lam_im, log_dt, b_re, b_im, c_re, c_im, d_skip, w_glu, b_glu,
                gate_bias, norm_g):
    z = (x @ w_in).astype(jnp.float32)
    u, q, k, v, o_pre, i_pre, f_pre = _split(
        z, [HALF, ML_HEADS * ML_DK, ML_HEADS * ML_DK, HALF, HALF, ML_HEADS, ML_HEADS])
    ya = _s5(u, lam_re, lam_im, log_dt, b_re, b_im, c_re, c_im, d_skip, w_glu, b_glu)
    yb = _mlstm(q, k, v, o_pre, i_pre + gate_bias[:ML_HEADS], f_pre + gate_bias[ML_HEADS:], norm_g)
    return jnp.concatenate([ya, yb], -1).astype(x.dtype) @ w_out


def _odd_mixer(x, w_in, w_out, mu, w0, w_up, a0, a_up, g_up, k_k, k_a, r_k, ln_g, ln_b,
               conv_w, a_log, dt_bias, norm_g):
    z = (x @ w_in).astype(jnp.float32)
    feat, q, k, v, gate, beta_pre, alpha_pre = _split(
        z, [RW_COLS, HALF, HALF, HALF, HALF, GD_HEADS, GD_HEADS])
    yc = _rwkv7(feat, mu, w0, w_up, a0, a_up, g_up, k_k, k_a, r_k, ln_g, ln_b)
    yd = _gated_deltanet(q, k, v, gate, beta_pre, alpha_pre, conv_w, a_log, dt_bias, norm_g)
    return jnp.concatenate([yc, yd], -1).astype(x.dtype) @ w_out


def _swiglu(x, wg, wu, wd):
    return (jax.nn.silu(x @ wg) * (x @ wu)) @ wd


def _moe(x, router, wg, wu, wd):
    bsz, s, d = x.shape
    xt = x.reshape(-1, d)
    logits = (xt @ router).astype(jnp.float32)
    top_v, top_i = lax.top_k(logits, TOP_K)
    gates = jax.nn.softmax(top_v, axis=-1)
    comb = jnp.sum(jax.nn.one_hot(top_i, N_EXPERTS, dtype=jnp.float32) * gates[..., None], axis=1)
    y = jnp.zeros(xt.shape, jnp.float32)
    for e in range(N_EXPERTS):
        y = y + comb[:, e:e + 1] * _swiglu(xt, wg[e], wu[e], wd[e])
    return y.astype(x.dtype).reshape(bsz, s, d)


def setup_inputs(seed: int = 0) -> dict:
    key = jax.random.key(seed)
    ks = iter(jax.random.split(key, 64))

    def nrm(shape, scale):
        return jax.random.normal(next(ks), shape, jnp.float32) * scale

    def unif(shape, lo, hi):
        return jax.random.uniform(next(ks), shape, jnp.float32, lo, hi)

    NE, NO, D = N_EVEN, N_ODD, D_MODEL
    G, P = S5_GROUPS, S5_STATE
    x = nrm((BATCH, SEQ, D), 1.0)
    ln_g = 1.0 + nrm((DEPTH, 2, D), 0.02)
    ln_b = nrm((DEPTH, 2, D), 0.02)
    ev_w_in = nrm((NE, D, EVEN_COLS), D ** -0.5)
    s5_lam_re = -0.5 + nrm((NE, G, P), 0.01)
    s5_lam_im = math.pi * jnp.arange(P, dtype=jnp.float32) + nrm((NE, G, P), 0.01)
    s5_log_dt = unif((NE, G), math.log(1e-3), math.log(1e-1))
    s5_b_re = nrm((NE, G, P, S5_GROUP), (2 * S5_GROUP) ** -0.5)
    s5_b_im = nrm((NE, G, P, S5_GROUP), (2 * S5_GROUP) ** -0.5)
    s5_c_re = nrm((NE, G, S5_GROUP, P), (2 * P) ** -0.5)
    s5_c_im = nrm((NE, G, S5_GROUP, P), (2 * P) ** -0.5)
    s5_d = nrm((NE, HALF), 1.0)
    s5_w_glu = nrm((NE, HALF, HALF), HALF ** -0.5)
    s5_b_glu = nrm((NE, HALF), 0.02)
    ml_gate_bias = jnp.concatenate(
        [nrm((NE, ML_HEADS), 0.1), jnp.linspace(3.0, 6.0, ML_HEADS)[None] + nrm((NE, ML_HEADS), 0.1)], -1)
    ml_norm_g = 1.0 + nrm((NE, HALF), 0.02)
    ev_w_out = nrm((NE, MIX_WIDTH, D), MIX_WIDTH ** -0.5 * DN_BETA)
    ffn_w_gate = nrm((NE, D, D_FF), D ** -0.5)
    ffn_w_up = nrm((NE, D, D_FF), D ** -0.5)
    ffn_w_down = nrm((NE, D_FF, D), D_FF ** -0.5 * DN_BETA)
    od_w_in = nrm((NO, D, ODD_COLS), D ** -0.5)
    rw_mu = unif((NO, RW_COLS), 0.0, 1.0)
    rw_w0 = jnp.linspace(-6.5, -1.5, HALF)[None] + nrm((NO, HALF), 0.1)
    rw_w_up = nrm((NO, RW_DECAY_LORA, HALF), 0.1)
    rw_a0 = nrm((NO, HALF), 0.1)
    rw_a_up = nrm((NO, RW_AAA_LORA, HALF), 0.5 * RW_AAA_LORA ** -0.5)
    rw_g_up = nrm((NO, RW_GATE_LORA, HALF), RW_GATE_LORA ** -0.5)
    rw_k_k = 0.85 + nrm((NO, HALF), 0.02)
    rw_k_a = 1.0 + nrm((NO, HALF), 0.02)
    rw_r_k = nrm((NO, HALF), 0.1)
    rw_ln_g = 1.0 + nrm((NO, HALF), 0.02)
    rw_ln_b = nrm((NO, HALF), 0.02)
    gd_conv = nrm((NO, GD_CONV, 3 * HALF), GD_CONV ** -0.5)
    gd_a_log = jnp.log(unif((NO, GD_HEADS), 1.0, 16.0))
    dt = jnp.exp(unif((NO, GD_HEADS), math.log(1e-3), math.log(1e-1)))
    gd_dt_bias = dt + jnp.log(-jnp.expm1(-dt))
    gd_norm_g = 1.0 + nrm((NO, GD_HEAD), 0.02)
    od_w_out = nrm((NO, MIX_WIDTH, D), MIX_WIDTH ** -0.5 * DN_BETA)
    moe_router = nrm((NO, D, N_EXPERTS), D ** -0.5)
    moe_w_gate = nrm((NO, N_EXPERTS, D, D_FF_EXPERT), D ** -0.5)
    moe_w_up = nrm((NO, N_EXPERTS, D, D_FF_EXPERT), D ** -0.5)
    moe_w_down = nrm((NO, N_EXPERTS, D_FF_EXPERT, D), D_FF_EXPERT ** -0.5 * DN_BETA)
    return {'x': x, 'ln_g': ln_g, 'ln_b': ln_b,
            'ev_w_in': ev_w_in, 's5_lam_re': s5_lam_re, 's5_lam_im': s5_lam_im, 's5_log_dt': s5_log_dt,
            's5_b_re': s5_b_re, 's5_b_im': s5_b_im, 's5_c_re': s5_c_re, 's5_c_im': s5_c_im,
            's5_d': s5_d, 's5_w_glu': s5_w_glu, 's5_b_glu': s5_b_glu,
            'ml_gate_bias': ml_gate_bias, 'ml_norm_g': ml_norm_g, 'ev_w_out': ev_w_out,
            'ffn_w_gate': ffn_w_gate, 'ffn_w_up': ffn_w_up, 'ffn_w_down': ffn_w_down,
            'od_w_in': od_w_in, 'rw_mu': rw_mu, 'rw_w0': rw_w0, 'rw_w_up': rw_w_up, 'rw_a0': rw_a0,
            'rw_a_up': rw_a_up, 'rw_g_up': rw_g_up, 'rw_k_k': rw_k_k, 'rw_k_a': rw_k_a, 'rw_r_k': rw_r_k,
            'rw_ln_g': rw_ln_g, 'rw_ln_b': rw_ln_b, 'gd_conv': gd_conv, 'gd_a_log': gd_a_log,
            'gd_dt_bias': gd_dt_bias, 'gd_norm_g': gd_norm_g, 'od_w_out': od_w_out,
            'moe_router': moe_router, 'moe_w_gate': moe_w_gate, 'moe_w_up': moe_w_up, 'moe_w_down': moe_w_down}


def reference(x, ln_g, ln_b,
              ev_w_in, s5_lam_re, s5_lam_im, s5_log_dt, s5_b_re, s5_b_im, s5_c_re, s5_c_im,
              s5_d, s5_w_glu, s5_b_glu, ml_gate_bias, ml_norm_g, ev_w_out,
              ffn_w_gate, ffn_w_up, ffn_w_down,
              od_w_in, rw_mu, rw_w0, rw_w_up, rw_a0, rw_a_up, rw_g_up, rw_k_k, rw_k_a, rw_r_k,
              rw_ln_g, rw_ln_b, gd_conv, gd_a_log, gd_dt_bias, gd_norm_g, od_w_out,
              moe_router, moe_w_gate, moe_w_up, moe_w_down):
    for layer in range(DEPTH):
        i = layer // 2
        if layer % 2 == 0:
            h = _even_mixer(x, ev_w_in[i], ev_w_out[i], s5_lam_re[i], s5_lam_im[i], s5_log_dt[i],
                            s5_b_re[i], s5_b_im[i], s5_c_re[i], s5_c_im[i], s5_d[i], s5_w_glu[i],
                            s5_b_glu[i], ml_gate_bias[i], ml_norm_g[i])
            x = _layer_norm(DN_ALPHA * x + h, ln_g[layer, 0], ln_b[layer, 0])
            f = _swiglu(x, ffn_w_gate[i], ffn_w_up[i], ffn_w_down[i])
            x = _layer_norm(DN_ALPHA * x + f, ln_g[layer, 1], ln_b[layer, 1])
        else:
            h = _odd_mixer(x, od_w_in[i], od_w_out[i], rw_mu[i], rw_w0[i], rw_w_up[i], rw_a0[i],
                           rw_a_up[i], rw_g_up[i], rw_k_k[i], rw_k_a[i], rw_r_k[i], rw_ln_g[i],
                           rw_ln_b[i], gd_conv[i], gd_a_log[i], gd_dt_bias[i], gd_norm_g[i])
            x = _layer_norm(DN_ALPHA * x + h, ln_g[layer, 0], ln_b[layer, 0])
            f = _moe(x, moe_router[i], moe_w_gate[i], moe_w_up[i], moe_w_down[i])
            x = _layer_norm(DN_ALPHA * x + f, ln_g[layer, 1], ln_b[layer, 1])
    return x
```

```python
import numpy as np
import concourse.bass as bass
import concourse.mybir as mybir
from concourse.bass_utils import run_bass_kernel_spmd

F32 = mybir.dt.float32
BF16 = mybir.dt.bfloat16
I32 = mybir.dt.int32
AF = mybir.ActivationFunctionType
ALU = mybir.AluOpType
AX = mybir.AxisListType

ENGS = ["tensor", "vector", "scalar", "gpsimd", "sync"]
WRITE_KW = ("out", "accum_out", "out_max", "out_indices")
SEM_LIMIT = 30000
N_DMA_SEMS = 12


def _is_ap(x):
    return hasattr(x, "tensor") and hasattr(x, "ap")


class _EngProxy:
    def __init__(self, sched, eng):
        self._s = sched
        self._e = eng

    def __getattr__(self, meth):
        def call(*args, **kwargs):
            return self._s._record(self._e, meth, args, kwargs)
        return call


class Sched:
    def __init__(self, nc, same_engine_sync=True):
        self.nc = nc
        self.ops = []
        self.same_engine_sync = same_engine_sync
        self.tensor = _EngProxy(self, "tensor")
        self.vector = _EngProxy(self, "vector")
        self.scalar = _EngProxy(self, "scalar")
        self.gpsimd = _EngProxy(self, "gpsimd")
        self.sync = _EngProxy(self, "sync")
        self._ctx = []

    def sb(self, name, shape, dtype=F32):
        g = self.nc.sbuf_tensor(name, list(shape), dtype)
        t = g.__enter__()
        self._ctx.append(g)
        return t

    def ps(self, name, shape, dtype=F32):
        g = self.nc.psum_tensor(name, list(shape), dtype)
        t = g.__enter__()
        self._ctx.append(g)
        return t

    @staticmethod
    def _box(a):
        name = a.tensor.name
        apl = a.ap
        off = a.offset
        if "DRam" in type(a.tensor).__name__:
            ext = sum(st * (c - 1) for st, c in apl) + 1
            return (name, 0, 1, off, off + ext)
        if "PSum" in type(a.tensor).__name__:
            return (name, 0, 128, 0, 1 << 30)
        row = apl[0][0]
        if row <= 0:
            row = 1 << 30
        p0 = off // row
        f0 = off % row
        ext = sum(st * (c - 1) for st, c in apl[1:]) + 1
        return (name, p0, p0 + apl[0][1], f0, f0 + ext)

    def _record(self, eng, meth, args, kwargs):
        reads, writes = [], []
        extra_r = kwargs.pop("_reads", None)
        extra_w = kwargs.pop("_writes", None)
        for i, a in enumerate(args):
            if _is_ap(a):
                (writes if i == 0 else reads).append(self._box(a))
        for k, a in kwargs.items():
            if _is_ap(a):
                (writes if k in WRITE_KW else reads).append(self._box(a))
        if extra_r:
            reads += [self._box(a) for a in extra_r]
        if extra_w:
            writes += [self._box(a) for a in extra_w]
        for bx in list(reads):
            if bx[4] == (1 << 30) and bx not in writes:
                writes.append(bx)
        is_dma = meth in ("dma_start", "dma_start_transpose", "indirect_dma_start")
        self.ops.append(dict(eng=eng, meth=meth, args=args, kwargs=kwargs,
                             reads=reads, writes=writes, dma=is_dma))
        return len(self.ops) - 1

    def emit(self):
        nc = self.nc
        ops = self.ops
        n = len(ops)
        W = {}
        R = {}
        deps = [None] * n

        def ov(a, b):
            return a[1] < b[2] and b[1] < a[2] and a[3] < b[4] and b[3] < a[4]

        def inside(a, b):
            return a[1] >= b[1] and a[2] <= b[2] and a[3] >= b[3] and a[4] <= b[4]

        for i, op in enumerate(ops):
            d = set()
            for bx in op["reads"]:
                for (b2, j) in W.get(bx[0], ()):
                    if ov(bx, b2):
                        d.add(j)
            for bx in op["writes"]:
                for (b2, j) in W.get(bx[0], ()):
                    if ov(bx, b2):
                        d.add(j)
                for (b2, j) in R.get(bx[0], ()):
                    if ov(bx, b2):
                        d.add(j)
            d.discard(i)
            deps[i] = d
            for bx in op["writes"]:
                nm = bx[0]
                W[nm] = [(b2, j) for (b2, j) in W.get(nm, ()) if not inside(b2, bx)] + [(bx, i)]
                R[nm] = [(b2, j) for (b2, j) in R.get(nm, ()) if not inside(b2, bx)]
            for bx in op["reads"]:
                nm = bx[0]
                lst = [(b2, j) for (b2, j) in R.get(nm, ()) if not (b2 == bx and ops[j]["eng"] == op["eng"] and not ops[j]["dma"])]
                lst.append((bx, i))
                R[nm] = lst
        needed = [False] * n
        for i, op in enumerate(ops):
            keep = set()
            for j in deps[i]:
                pj = ops[j]
                if pj["eng"] == op["eng"] and not pj["dma"]:
                    if op["eng"] == "tensor" and not op["dma"]:
                        continue
                    if op["eng"] == "sync":
                        continue
                    if not self.same_engine_sync and not op["dma"]:
                        continue
                keep.add(j)
            deps[i] = keep
            for j in keep:
                needed[j] = True
        sems = {}
        self._semguards = []

        def new_sem(name):
            g = nc.semaphore(name)
            s = g.__enter__()
            self._semguards.append(g)
            return s

        eng_sem = {e: new_sem(f"s_{e}_0") for e in ENGS}
        eng_cnt = {e: 0 for e in ENGS}
        eng_gen = {e: 0 for e in ENGS}
        dma_sems = {e: [new_sem(f"d_{e}_{i}") for i in range(N_DMA_SEMS)] for e in ("sync", "gpsimd", "scalar")}
        dma_cnt = {e: [0] * N_DMA_SEMS for e in dma_sems}
        dma_rr = {e: 0 for e in dma_sems}
        dma_last_tok = {e: [None] * N_DMA_SEMS for e in dma_sems}
        token = [None] * n
        prewait = [None] * n
        for i, op in enumerate(ops):
            e = op["eng"]
            if op["dma"]:
                r = dma_rr[e]
                dma_rr[e] = (r + 1) % N_DMA_SEMS
                prewait[i] = dma_last_tok[e][r]
                dma_cnt[e][r] += 16
                if dma_cnt[e][r] > SEM_LIMIT:
                    dma_sems[e][r] = new_sem(f"d_{e}_{r}_{i}")
                    dma_cnt[e][r] = 16
                    prewait[i] = dma_last_tok[e][r]
                token[i] = (dma_sems[e][r], dma_cnt[e][r])
                dma_last_tok[e][r] = token[i]
            elif needed[i]:
                if eng_cnt[e] >= SEM_LIMIT:
                    eng_gen[e] += 1
                    eng_sem[e] = new_sem(f"s_{e}_{eng_gen[e]}")
                    eng_cnt[e] = 0
                eng_cnt[e] += 1
                token[i] = (eng_sem[e], eng_cnt[e])
        final_tokens = []
        for e in dma_sems:
            for t in dma_last_tok[e]:
                if t is not None:
                    final_tokens.append(t)
        per_eng = {e: [] for e in ENGS}
        for i, op in enumerate(ops):
            per_eng[op["eng"]].append(i)
        self.n_waits = 0
        sched = self

        def run_engine(ename, eobj):
            seen = {}
            def wait(tok):
                s, v = tok
                key = id(s)
                if seen.get(key, 0) >= v:
                    return
                seen[key] = v
                eobj.wait_ge(s, v)
                sched.n_waits += 1
            for i in per_eng[ename]:
                op = ops[i]
                for j in sorted(deps[i]):
                    wait(token[j])
                if prewait[i] is not None:
                    wait(prewait[i])
                ins = getattr(eobj, op["meth"])(*op["args"], **op["kwargs"])
                if token[i] is not None:
                    ins.then_inc(token[i][0], 16 if op["dma"] else 1)
            if ename == "sync":
                for t in final_tokens:
                    wait(t)

        with nc.Block() as block:
            @block.tensor
            def _(e):
                run_engine("tensor", e)

            @block.vector
            def _(e):
                run_engine("vector", e)

            @block.scalar
            def _(e):
                run_engine("scalar", e)

            @block.gpsimd
            def _(e):
                run_engine("gpsimd", e)

            @block.sync
            def _(e):
                run_engine("sync", e)
        for g in reversed(self._semguards):
            g.__exit__(None, None, None)
        for g in reversed(self._ctx):
            g.__exit__(None, None, None)
        return nc
DN_ALPHA = 8 ** 0.25
LN_EPS = 1e-5


def make_consts():
    c = {}
    i = np.arange(128)
    c["ident"] = np.eye(128, dtype=np.float32)
    c["tri"] = (i[:, None] <= i[None, :]).astype(np.float32)
    c["ones"] = np.ones((128, 128), np.float32)
    c["negT"] = np.where(i[:, None] <= i[None, :], 0.0, -30000.0).astype(np.float32)
    c["neg"] = np.where(i[:, None] >= i[None, :], 0.0, -30000.0).astype(np.float32)
    c["mT_strict"] = (i[:, None] < i[None, :]).astype(np.float32)
    c["m_strict"] = (i[:, None] > i[None, :]).astype(np.float32)
    blk = (i[:, None] // 64) == (i[None, :] // 64)
    c["btri"] = ((i[:, None] <= i[None, :]) & blk).astype(np.float32)
    c["bmT_incl"] = ((i[:, None] <= i[None, :]) & blk).astype(np.float32)
    c["bmT_strict"] = ((i[:, None] < i[None, :]) & blk).astype(np.float32)
    c["bm_strict"] = ((i[:, None] > i[None, :]) & blk).astype(np.float32)
    ci = np.zeros((128, 128), np.float32)
    ci[:64, 0] = 1.0
    ci[64:, 1] = 1.0
    c["chunkind"] = ci
    c["iota_row"] = np.tile(i[None, :].astype(np.float32), (128, 1))
    c["iota_col"] = np.tile(i[:, None].astype(np.float32), (1, 128))
    names = list(c.keys())
    arr = np.concatenate([c[k] for k in names], axis=1)
    offs = {k: n * 128 for n, k in enumerate(names)}
    return arr, offs


CONST_ARR, CONST_OFF = make_consts()


class Prog:
    def __init__(self):
        self.nc = bass.Bass("TRN2", target_bir_lowering=False)
        self.S = Sched(self.nc)
        self.cd = self.din("consts", CONST_ARR.shape)
        self.csb = self.S.sb("csb", CONST_ARR.shape)
        self.S.sync.dma_start(out=self.csb[:], in_=self.cd)
        self._n = 0

    def din(self, name, shape):
        return self.nc.dram_tensor(name, list(shape), F32, kind="ExternalInput").ap()

    def dout(self, name, shape):
        return self.nc.dram_tensor(name, list(shape), F32, kind="ExternalOutput").ap()

    def C(self, name, rows=128, cols=128):
        o = CONST_OFF[name]
        return self.csb[0:rows, o:o + cols]

    def sb(self, name, shape, dtype=F32):
        return self.S.sb(name, shape, dtype)

    def rot(self, name, shape, n=2):
        return [self.S.sb(f"{name}_{i}", shape) for i in range(n)]

    def bc_row(self, name, dram_row, n):
        t = self.S.sb(name, [128, n])
        self.S.sync.dma_start(out=t[:], in_=dram_row.partition_broadcast(128))
        return t

    def transpose(self, dst, src, ps, rows=128, cols=128, eng="vector"):
        S = self.S
        S.tensor.transpose(out=ps, in_=src, identity=self.C("ident", rows, rows))
        if eng == "vector":
            S.vector.tensor_copy(out=dst, in_=ps)
        else:
            S.scalar.copy(out=dst, in_=ps)


def layernorm(P, src, dst, g_bc, b_bc, scr, D=1024, eps=LN_EPS):
    S = P.S
    n = src.shape[0]
    st = {k: v[0:n, :] for k, v in scr.items()}
    S.vector.reduce_sum(out=st["s1"], in_=src, axis=AX.X)
    S.vector.tensor_scalar(out=st["mean"], in0=st["s1"], scalar1=-1.0 / D, scalar2=None, op0=ALU.mult)
    S.vector.tensor_scalar(out=dst, in0=src, scalar1=st["mean"][:, 0:1], scalar2=None, op0=ALU.add)
    S.scalar.activation(out=st["sq"][:, 0:D], in_=dst, func=AF.Square, accum_out=st["ss"])
    S.scalar.activation(out=st["std"], in_=st["ss"], func=AF.Sqrt, bias=st["eps"][:, 0:1], scale=1.0 / D)
    S.vector.reciprocal(out=st["rstd"], in_=st["std"])
    if g_bc is not None:
        S.vector.scalar_tensor_tensor(out=dst, in0=dst, scalar=st["rstd"][:, 0:1], in1=g_bc, op0=ALU.mult, op1=ALU.mult)
        S.vector.tensor_tensor(out=dst, in0=dst, in1=b_bc, op=ALU.add)
    else:
        S.vector.tensor_scalar(out=dst, in0=dst, scalar1=st["rstd"][:, 0:1], scalar2=None, op0=ALU.mult)


def ln_scratch(P, name, D=1024, eps=LN_EPS):
    st = {k: P.sb(f"{name}_{k}", [128, 1]) for k in ("s1", "mean", "ss", "std", "rstd", "eps")}
    st["sq"] = P.sb(f"{name}_sq", [128, D])
    P.S.vector.memset(st["eps"][:], eps)
    return st


def build_proj(S_tok, C):
    P = Prog()
    S = P.S
    x = P.din("x", [S_tok, 1024])
    w = P.din("w", [1024, C])
    z = P.dout("z", [S_tok, C])
    wsb = P.sb("wsb", [128, 8, C])
    for k in range(8):
        S.sync.dma_start(out=wsb[:, k, :], in_=w[k * 128:(k + 1) * 128, :])
    nt = S_tok // 128
    xt = P.rot("xt", [128, 1024])
    xT = P.rot("xT", [128, 8, 128])
    zt = P.rot("zt", [128, C])
    pst = [S.ps(f"pst{i}", [128, 512]) for i in range(2)]
    psm = [S.ps(f"psm{i}", [128, 512]) for i in range(4)]
    chunks = [(c0, min(512, C - c0)) for c0 in range(0, C, 512)]
    for t in range(nt):
        xs = xt[t % 2]
        S.sync.dma_start(out=xs[:], in_=x[t * 128:(t + 1) * 128, :])
        for k in range(8):
            P.transpose(xT[t % 2][:, k, :], xs[:, k * 128:(k + 1) * 128], pst[k % 2][:, 0:128],
                        eng="vector" if k % 2 == 0 else "scalar")
        for ci, (c0, cw) in enumerate(chunks):
            ps = psm[ci % 4]
            for k in range(8):
                S.tensor.matmul(ps[:, 0:cw], lhsT=xT[t % 2][:, k, :], rhs=wsb[:, k, c0:c0 + cw],
                                start=(k == 0), stop=(k == 7))
            if ci % 2 == 0:
                S.vector.tensor_copy(out=zt[t % 2][:, c0:c0 + cw], in_=ps[:, 0:cw])
            else:
                S.scalar.copy(out=zt[t % 2][:, c0:c0 + cw], in_=ps[:, 0:cw])
        S.gpsimd.dma_start(out=z[t * 128:(t + 1) * 128, :], in_=zt[t % 2][:])
    S.emit()
    return P.nc
def build_cf(S_tok, n_exp, d_ff):
    P = Prog()
    S = P.S
    x = P.din("x", [S_tok, 1024])
    ya = P.din("ya", [S_tok, 512])
    yb = P.din("yb", [S_tok, 512])
    w_out = P.din("w_out", [1024, 1024])
    lnp = P.din("lnp", [4, 1024])
    wg = P.din("wg", [n_exp, 1024, d_ff])
    wu = P.din("wu", [n_exp, 1024, d_ff])
    wd = P.din("wd", [n_exp, d_ff, 1024])
    if n_exp > 1:
        router = P.din("router", [1024, 8])
    xo = P.dout("xo", [S_tok, 1024])
    TB = 512 if S_tok >= 512 else S_tok
    NT = TB // 128
    wo_sb = P.sb("wo_sb", [128, 8, 1024])
    for k in range(8):
        S.sync.dma_start(out=wo_sb[:, k, :], in_=w_out[k * 128:(k + 1) * 128, :])
    g0 = P.bc_row("g0", lnp[0:1, :], 1024)
    b0 = P.bc_row("b0", lnp[1:2, :], 1024)
    g1 = P.bc_row("g1", lnp[2:3, :], 1024)
    b1 = P.bc_row("b1", lnp[3:4, :], 1024)
    if n_exp > 1:
        r_sb = P.sb("r_sb", [128, 8, 8])
        for k in range(8):
            S.sync.dma_start(out=r_sb[:, k, :], in_=router[k * 128:(k + 1) * 128, :])
    lns = ln_scratch(P, "lns")
    xt = P.rot("xt", [128, 1024])
    yt = P.rot("yt", [128, 1024])
    yT = P.rot("yT", [128, 8, 128])
    x1 = P.sb("x1", [128, NT, 1024])
    x1T = P.sb("x1T", [128, 8, TB])
    facc = P.sb("facc", [128, NT, 1024])
    comb = P.sb("comb", [128, NT, 8])
    FC = 256
    nfc = d_ff // FC
    wg_sb = P.rot("wg_sb", [128, 8, FC])
    wu_sb = P.rot("wu_sb", [128, 8, FC])
    wd_sb = P.rot("wd_sb", [128, FC // 128, 1024])
    hT = P.rot("hT", [128, FC // 128, TB])
    sg = P.rot("sg", [128, TB])
    pst = [S.ps(f"pst{i}", [128, 512]) for i in range(2)]
    pg = [S.ps(f"pg{i}", [128, 512]) for i in range(2)]
    pu = [S.ps(f"pu{i}", [128, 512]) for i in range(2)]
    pd = [S.ps(f"pd{i}", [128, 512]) for i in range(2)]
    sm = {k: P.sb(f"sm_{k}", [128, 8]) for k in ("lg", "m1", "mk1", "l2", "m2", "mk2", "d", "g1", "g2", "t")}
    nblk = S_tok // TB
    it = 0
    for blk in range(nblk):
        for tt in range(NT):
            t0 = blk * TB + tt * 128
            xs, ys = xt[tt % 2], yt[tt % 2]
            S.sync.dma_start(out=xs[:], in_=x[t0:t0 + 128, :])
            S.sync.dma_start(out=ys[:, 0:512], in_=ya[t0:t0 + 128, :])
            S.sync.dma_start(out=ys[:, 512:1024], in_=yb[t0:t0 + 128, :])
            for k in range(8):
                P.transpose(yT[tt % 2][:, k, :], ys[:, k * 128:(k + 1) * 128], pst[k % 2][:, 0:128],
                            eng="vector" if k % 2 == 0 else "scalar")
            for half in range(2):
                ps = pd[half]
                for k in range(8):
                    S.tensor.matmul(ps[:], lhsT=yT[tt % 2][:, k, :], rhs=wo_sb[:, k, half * 512:(half + 1) * 512],
                                    start=(k == 0), stop=(k == 7))
                S.vector.scalar_tensor_tensor(out=xs[:, half * 512:(half + 1) * 512], in0=xs[:, half * 512:(half + 1) * 512],
                                              scalar=DN_ALPHA, in1=ps[:], op0=ALU.mult, op1=ALU.add)
            layernorm(P, xs[:], x1[:, tt, :], g0[:], b0[:], lns)
            for k in range(8):
                P.transpose(x1T[:, k, tt * 128:(tt + 1) * 128], x1[:, tt, k * 128:(k + 1) * 128], pst[k % 2][:, 0:128],
                            eng="vector" if k % 2 == 0 else "scalar")
            if n_exp > 1:
                ps = pd[0]
                for k in range(8):
                    S.tensor.matmul(ps[:, 0:8], lhsT=x1T[:, k, tt * 128:(tt + 1) * 128], rhs=r_sb[:, k, :],
                                    start=(k == 0), stop=(k == 7))
                S.vector.tensor_copy(out=sm["lg"][:], in_=ps[:, 0:8])
                S.vector.reduce_max(out=sm["m1"][:, 0:1], in_=sm["lg"][:], axis=AX.X)
                S.vector.tensor_scalar(out=sm["mk1"][:], in0=sm["lg"][:], scalar1=sm["m1"][:, 0:1], scalar2=None, op0=ALU.is_ge)
                S.vector.scalar_tensor_tensor(out=sm["l2"][:], in0=sm["mk1"][:], scalar=-1e30, in1=sm["lg"][:], op0=ALU.mult, op1=ALU.add)
                S.vector.reduce_max(out=sm["m2"][:, 0:1], in_=sm["l2"][:], axis=AX.X)
                S.vector.tensor_scalar(out=sm["mk2"][:], in0=sm["l2"][:], scalar1=sm["m2"][:, 0:1], scalar2=None, op0=ALU.is_ge)
                S.vector.tensor_tensor(out=sm["d"][:, 0:1], in0=sm["m2"][:, 0:1], in1=sm["m1"][:, 0:1], op=ALU.subtract)
                S.scalar.activation(out=sm["t"][:, 0:1], in_=sm["d"][:, 0:1], func=AF.Exp)
                S.vector.tensor_scalar(out=sm["t"][:, 0:1], in0=sm["t"][:, 0:1], scalar1=1.0, scalar2=None, op0=ALU.add)
                S.vector.reciprocal(out=sm["g1"][:, 0:1], in_=sm["t"][:, 0:1])
                S.vector.tensor_scalar(out=sm["g2"][:, 0:1], in0=sm["g1"][:, 0:1], scalar1=-1.0, scalar2=1.0, op0=ALU.mult, op1=ALU.add)
                S.vector.tensor_scalar(out=comb[:, tt, :], in0=sm["mk1"][:], scalar1=sm["g1"][:, 0:1], scalar2=None, op0=ALU.mult)
                S.vector.scalar_tensor_tensor(out=comb[:, tt, :], in0=sm["mk2"][:], scalar=sm["g2"][:, 0:1], in1=comb[:, tt, :],
                                              op0=ALU.mult, op1=ALU.add)
        first = True
        for e in range(n_exp):
            for fc in range(nfc):
                b = it % 2
                it += 1
                f0 = fc * FC
                S.sync.dma_start(out=wg_sb[b][:], in_=wg[e, :, f0:f0 + FC].rearrange("(k p) f -> p k f", p=128))
                S.sync.dma_start(out=wu_sb[b][:], in_=wu[e, :, f0:f0 + FC].rearrange("(k p) f -> p k f", p=128))
                S.sync.dma_start(out=wd_sb[b][:], in_=wd[e, f0:f0 + FC, :].rearrange("(k p) f -> p k f", p=128))
                for ft in range(FC // 128):
                    for k in range(8):
                        S.tensor.matmul(pg[ft % 2][:, 0:TB], lhsT=wg_sb[b][:, k, ft * 128:(ft + 1) * 128], rhs=x1T[:, k, :],
                                        start=(k == 0), stop=(k == 7))
                    for k in range(8):
                        S.tensor.matmul(pu[ft % 2][:, 0:TB], lhsT=wu_sb[b][:, k, ft * 128:(ft + 1) * 128], rhs=x1T[:, k, :],
                                        start=(k == 0), stop=(k == 7))
                    S.scalar.activation(out=sg[ft % 2][:], in_=pg[ft % 2][:, 0:TB], func=AF.Silu)
                    S.vector.tensor_tensor(out=hT[b][:, ft, :], in0=sg[ft % 2][:], in1=pu[ft % 2][:, 0:TB], op=ALU.mult)
                for tt in range(NT):
                    for half in range(2):
                        ps = pd[(tt * 2 + half) % 2]
                        for ft in range(FC // 128):
                            S.tensor.matmul(ps[:], lhsT=hT[b][:, ft, tt * 128:(tt + 1) * 128],
                                            rhs=wd_sb[b][:, ft, half * 512:(half + 1) * 512],
                                            start=(ft == 0), stop=(ft == FC // 128 - 1))
                        fa = facc[:, tt, half * 512:(half + 1) * 512]
                        if n_exp > 1:
                            if first:
                                S.vector.tensor_scalar(out=fa, in0=ps[:], scalar1=comb[:, tt, e:e + 1], scalar2=None, op0=ALU.mult)
                            else:
                                S.vector.scalar_tensor_tensor(out=fa, in0=ps[:], scalar=comb[:, tt, e:e + 1], in1=fa,
                                                              op0=ALU.mult, op1=ALU.add)
                        else:
                            if first:
                                S.vector.tensor_copy(out=fa, in_=ps[:])
                            else:
                                S.vector.tensor_tensor(out=fa, in0=fa, in1=ps[:], op=ALU.add)
                first = False
        for tt in range(NT):
            t0 = blk * TB + tt * 128
            S.vector.scalar_tensor_tensor(out=facc[:, tt, :], in0=x1[:, tt, :], scalar=DN_ALPHA, in1=facc[:, tt, :],
                                          op0=ALU.mult, op1=ALU.add)
            layernorm(P, facc[:, tt, :], x1[:, tt, :], g1[:], b1[:], lns)
            S.gpsimd.dma_start(out=xo[t0:t0 + 128, :], in_=x1[:, tt, :])
    S.emit()
    return P.nc
def build_mlstm(S_tok):
    P = Prog()
    S = P.S
    z = P.din("z", [S_tok, 2056])
    gb = P.din("gb", [1, 8])
    ng = P.din("ng", [1, 512])
    y = P.dout("y", [S_tok, 512])
    gbb = P.bc_row("gbb", gb, 8)
    ngb = P.bc_row("ngb", ng, 512)
    Caug = P.sb("Caug", [64, 4, 129])
    S.vector.memset(Caug[:], 0.0)
    zt = P.rot("zt", [128, 1544])
    vaug = P.rot("vaug", [128, 4, 129])
    for b in range(2):
        S.vector.memset(vaug[b][:], 1.0)
    yt = P.rot("yt", [128, 512])
    sm = {k: P.sb(f"m_{k}", [128, 8]) for k in ("ig", "gf", "e", "lf", "bb", "eb", "es", "ebt", "tmp", "den", "rec")}
    LFtri = P.rot("LFtri", [128, 128])
    Rm = P.rot("Rm", [128, 128])
    ED = P.rot("ED", [128, 128])
    SDT = P.rot("SDT", [128, 128])
    qT = P.rot("qT", [64, 128])
    kT = P.rot("kT", [64, 128])
    qs = P.rot("qs", [128, 64])
    qsT = P.rot("qsT", [64, 128])
    ks = P.rot("ks", [128, 64])
    hh = P.rot("hh", [128, 128])
    sg = P.rot("sg", [128, 512])
    lns = ln_scratch(P, "mln", D=128, eps=1e-6)
    pT = [S.ps(f"pT{i}", [128, 512]) for i in range(2)]
    pD = S.ps("pD", [128, 512])
    pS = S.ps("pS", [128, 512])
    pN = S.ps("pN", [128, 512])
    pC = S.ps("pC", [128, 512])
    pB = S.ps("pB", [128, 512])
    ident, tri, ones, negT = P.C("ident"), P.C("tri"), P.C("ones"), P.C("negT")
    for t in range(S_tok // 128):
        t0 = t * 128
        Z = zt[t % 2]
        S.sync.dma_start(out=Z[:], in_=z[t0:t0 + 128, 512:2056])
        q, k, v, o = Z[:, 0:256], Z[:, 256:512], Z[:, 512:1024], Z[:, 1024:1536]
        S.vector.tensor_tensor(out=sm["ig"][:, 0:4], in0=Z[:, 1536:1540], in1=gbb[:, 0:4], op=ALU.add)
        S.vector.tensor_tensor(out=sm["gf"][:, 0:4], in0=Z[:, 1540:1544], in1=gbb[:, 4:8], op=ALU.add)
        S.scalar.activation(out=sm["e"][:, 0:4], in_=sm["gf"][:, 0:4], func=AF.Exp, scale=-1.0)
        S.vector.tensor_scalar(out=sm["e"][:, 0:4], in0=sm["e"][:, 0:4], scalar1=1.0, scalar2=None, op0=ALU.add)
        S.scalar.activation(out=sm["lf"][:, 0:4], in_=sm["e"][:, 0:4], func=AF.Ln)
        S.vector.tensor_scalar(out=sm["lf"][:, 0:4], in0=sm["lf"][:, 0:4], scalar1=-1.0, scalar2=None, op0=ALU.mult)
        S.tensor.matmul(pB[:, 0:4], lhsT=tri, rhs=sm["lf"][:, 0:4], start=True, stop=True)
        S.tensor.matmul(pB[:, 4:8], lhsT=ones, rhs=sm["lf"][:, 0:4], start=True, stop=True)
        S.vector.tensor_copy(out=sm["bb"][:], in_=pB[:, 0:8])
        S.scalar.activation(out=sm["eb"][:, 0:4], in_=sm["bb"][:, 0:4], func=AF.Exp)
        S.scalar.activation(out=sm["ebt"][:, 0:4], in_=sm["bb"][:, 4:8], func=AF.Exp)
        S.vector.tensor_tensor(out=sm["tmp"][:, 0:4], in0=sm["bb"][:, 4:8], in1=sm["bb"][:, 0:4], op=ALU.subtract)
        S.vector.tensor_tensor(out=sm["tmp"][:, 0:4], in0=sm["tmp"][:, 0:4], in1=sm["ig"][:, 0:4], op=ALU.add)
        S.scalar.activation(out=sm["es"][:, 0:4], in_=sm["tmp"][:, 0:4], func=AF.Exp)
        VA = vaug[t % 2]
        S.vector.tensor_copy(out=VA[:, :, 0:128], in_=v.rearrange("p (h d) -> p h d", h=4))
        S.scalar.activation(out=sg[t % 2][:], in_=o, func=AF.Sigmoid)
        Y = yt[t % 2]
        for h in range(4):
            u = (t * 4 + h) % 2
            S.vector.tensor_scalar(out=LFtri[u][:], in0=tri, scalar1=sm["lf"][:, h:h + 1], scalar2=None, op0=ALU.mult)
            S.vector.scalar_tensor_tensor(out=Rm[u][:], in0=ident, scalar=sm["ig"][:, h:h + 1], in1=LFtri[u][:],
                                          op0=ALU.mult, op1=ALU.subtract)
            S.tensor.matmul(pD[:, 0:128], lhsT=ones, rhs=LFtri[u][:], start=True, stop=False)
            S.tensor.matmul(pD[:, 0:128], lhsT=Rm[u][:], rhs=ones, start=False, stop=False)
            S.tensor.matmul(pD[:, 0:128], lhsT=ident, rhs=negT, start=False, stop=True)
            S.scalar.activation(out=ED[u][:], in_=pD[:, 0:128], func=AF.Exp)
            qh, kh = q[:, h * 64:(h + 1) * 64], k[:, h * 64:(h + 1) * 64]
            P.transpose(qT[u][:], qh, pT[0][0:64, 0:128], rows=128, cols=64)
            P.transpose(kT[u][:], kh, pT[1][0:64, 0:128], rows=128, cols=64, eng="scalar")
            S.vector.tensor_scalar(out=qs[u][:], in0=qh, scalar1=sm["eb"][:, h:h + 1], scalar2=0.125, op0=ALU.mult, op1=ALU.mult)
            P.transpose(qsT[u][:], qs[u][:], pT[0][0:64, 0:128], rows=128, cols=64)
            S.tensor.matmul(pS[:, 0:128], lhsT=kT[u][:], rhs=qT[u][:], start=True, stop=True)
            S.vector.scalar_tensor_tensor(out=SDT[u][:], in0=pS[:, 0:128], scalar=0.125, in1=ED[u][:], op0=ALU.mult, op1=ALU.mult)
            S.tensor.matmul(pN[:, 0:129], lhsT=SDT[u][:], rhs=VA[:, h, :], start=True, stop=False)
            S.tensor.matmul(pN[:, 0:129], lhsT=qsT[u][:], rhs=Caug[:, h, :], start=False, stop=True)
            S.scalar.activation(out=sm["den"][:, 0:1], in_=pN[:, 128:129], func=AF.Abs)
            S.vector.tensor_scalar_max(out=sm["den"][:, 0:1], in0=sm["den"][:, 0:1], scalar1=1.0)
            S.vector.reciprocal(out=sm["rec"][:, 0:1], in_=sm["den"][:, 0:1])
            S.vector.tensor_scalar(out=hh[u][:], in0=pN[:, 0:128], scalar1=sm["rec"][:, 0:1], scalar2=None, op0=ALU.mult)
            S.vector.tensor_scalar(out=ks[u][:], in0=kh, scalar1=sm["es"][:, h:h + 1], scalar2=None, op0=ALU.mult)
            S.tensor.matmul(pC[0:64, 0:129], lhsT=ks[u][:], rhs=VA[:, h, :], start=True, stop=True)
            S.vector.scalar_tensor_tensor(out=Caug[:, h, :], in0=Caug[:, h, :], scalar=sm["ebt"][0:64, h:h + 1], in1=pC[0:64, 0:129],
                                          op0=ALU.mult, op1=ALU.add)
            layernorm(P, hh[u][:], Y[:, h * 128:(h + 1) * 128], None, None, lns, D=128, eps=1e-6)
        S.vector.tensor_tensor(out=Y[:], in0=Y[:], in1=ngb[:], op=ALU.mult)
        S.vector.tensor_tensor(out=Y[:], in0=Y[:], in1=sg[t % 2][:], op=ALU.mult)
        S.gpsimd.dma_start(out=y[t0:t0 + 128, :], in_=Y[:])
    S.emit()
    return P.nc
class _Stop(Exception):
    pass


def build_gdn(S_tok, stop=99):
    try:
        return _build_gdn(S_tok, stop)
    except _Stop as e:
        P = e.args[0]
        P.S.emit()
        return P.nc


def _build_gdn(S_tok, stop):
    P = Prog()
    S = P.S
    z = P.din("z", [S_tok, 3848])
    conv_w = P.din("conv_w", [4, 1536])
    a_log = P.din("a_log", [1, 4])
    dt_bias = P.din("dt_bias", [1, 4])
    norm_g = P.din("norm_g", [1, 128])
    y = P.dout("y", [S_tok, 512])
    cw = [P.bc_row(f"cw{i}", conv_w[i:i + 1, :], 1536) for i in range(4)]
    alb = P.bc_row("alb", a_log, 4)
    dtb = P.bc_row("dtb", dt_bias, 4)
    ngb = P.bc_row("ngb", norm_g, 128)
    ea = P.sb("ea", [128, 4])
    S.scalar.activation(out=ea[:], in_=alb[:], func=AF.Exp)
    state = P.sb("state", [128, 4, 128])
    S.vector.memset(state[:], 0.0)
    xs = [P.rot(f"xs{k}", [128, 1536]) for k in range(4)]
    gt = P.rot("gt", [128, 520])
    acc = P.rot("acc", [128, 1536])
    tmp = P.rot("ctmp", [128, 1536])
    sq = P.sb("sq", [128, 1024])
    yt = P.rot("yt", [128, 512])
    sgt = P.rot("sgt", [128, 512])
    sm = {k: P.sb(f"g_{k}", [128, 8]) for k in ("ss", "rn", "beta", "sp", "g", "dd", "edec", "ek", "edl", "t", "ss2", "rs")}
    eps6 = P.sb("eps6", [128, 1])
    S.vector.memset(eps6[:], 1e-6)
    names = ["Gtri", "nGtri", "GT", "GTs", "G", "Gs", "kb", "kT", "kbT", "qn", "qT", "kn", "rhs_v", "rhs_k", "nwT", "u", "QKT",
             "qd", "qdT", "kd", "o", "osq"]
    B = {n: P.rot(n, [128, 128]) for n in names}
    X = P.rot("X", [128, 128], 2)
    Y = P.rot("Y", [128, 128], 2)
    Pm = P.rot("Pm", [128, 128], 2)
    pT = [S.ps(f"pT{i}", [128, 512]) for i in range(2)]
    pG = S.ps("pG", [128, 512])
    pA = S.ps("pA", [128, 512])
    pI = [S.ps(f"pI{i}", [128, 512]) for i in range(2)]
    pU = S.ps("pU", [128, 512])
    pS = S.ps("pS", [128, 512])
    ident, tri, ones, negT, neg = P.C("ident"), P.C("tri"), P.C("ones"), P.C("negT"), P.C("neg")
    mT_s, m_s = P.C("mT_strict"), P.C("m_strict")
    for t in range(S_tok // 128):
        t0 = t * 128
        b = t % 2
        for k in range(4):
            if t0 - k < 0:
                S.vector.memset(xs[k][b][0:32, :], 0.0)
                S.sync.dma_start(out=xs[k][b][k:128, :], in_=z[0:128 - k, 1792:3328])
            else:
                S.sync.dma_start(out=xs[k][b][:], in_=z[t0 - k:t0 - k + 128, 1792:3328])
        G = gt[b]
        S.sync.dma_start(out=G[:], in_=z[t0:t0 + 128, 3328:3848])
        S.vector.tensor_tensor(out=acc[b][:], in0=xs[0][b][:], in1=cw[3][:], op=ALU.mult)
        for k in (1, 2, 3):
            S.vector.tensor_tensor(out=tmp[b][:], in0=xs[k][b][:], in1=cw[3 - k][:], op=ALU.mult)
            S.vector.tensor_tensor(out=acc[b][:], in0=acc[b][:], in1=tmp[b][:], op=ALU.add)
        A = acc[b]
        if stop == 1:
            raise _Stop(P)
        S.scalar.activation(out=A[:], in_=A[:], func=AF.Silu)
        S.scalar.activation(out=sq[:], in_=A[:, 0:1024], func=AF.Square)
        S.vector.reduce_sum(out=sm["ss"][:, 0:8], in_=sq[:].rearrange("p (h d) -> p h d", h=8), axis=AX.X)
        S.scalar.activation(out=sm["ss"][:, 0:8], in_=sm["ss"][:, 0:8], func=AF.Sqrt, bias=eps6[:, 0:1], scale=1.0)
        S.vector.reciprocal(out=sm["rn"][:, 0:8], in_=sm["ss"][:, 0:8])
        S.scalar.activation(out=sm["beta"][:, 0:4], in_=G[:, 512:516], func=AF.Sigmoid)
        S.vector.tensor_tensor(out=sm["sp"][:, 0:4], in0=G[:, 516:520], in1=dtb[:], op=ALU.add)
        S.scalar.activation(out=sm["sp"][:, 0:4], in_=sm["sp"][:, 0:4], func=AF.Exp)
        S.vector.tensor_scalar(out=sm["sp"][:, 0:4], in0=sm["sp"][:, 0:4], scalar1=1.0, scalar2=None, op0=ALU.add)
        S.scalar.activation(out=sm["sp"][:, 0:4], in_=sm["sp"][:, 0:4], func=AF.Ln)
        S.vector.scalar_tensor_tensor(out=sm["g"][:, 0:4], in0=sm["sp"][:, 0:4], scalar=-1.0, in1=ea[:], op0=ALU.mult, op1=ALU.mult)
        S.tensor.matmul(pS[:, 0:4], lhsT=tri, rhs=sm["g"][:, 0:4], start=True, stop=True)
        S.tensor.matmul(pS[:, 4:8], lhsT=ones, rhs=sm["g"][:, 0:4], start=True, stop=True)
        S.vector.tensor_copy(out=sm["dd"][:], in_=pS[:, 0:8])
        S.scalar.activation(out=sm["edec"][:, 0:4], in_=sm["dd"][:, 0:4], func=AF.Exp)
        S.scalar.activation(out=sm["edl"][:, 0:4], in_=sm["dd"][:, 4:8], func=AF.Exp)
        S.vector.tensor_tensor(out=sm["t"][:, 0:4], in0=sm["dd"][:, 4:8], in1=sm["dd"][:, 0:4], op=ALU.subtract)
        S.scalar.activation(out=sm["ek"][:, 0:4], in_=sm["t"][:, 0:4], func=AF.Exp)
        S.scalar.activation(out=sgt[b][:], in_=G[:, 0:512], func=AF.Silu)
        if stop == 2:
            raise _Stop(P)
        Yt = yt[b]
        for h in range(4):
            u = (t * 4 + h) % 2
            T_ = {n: B[n][u] for n in names}
            qh, kh, vh = A[:, h * 128:(h + 1) * 128], A[:, 512 + h * 128:512 + (h + 1) * 128], A[:, 1024 + h * 128:1024 + (h + 1) * 128]
            S.vector.tensor_scalar(out=T_["Gtri"][:], in0=tri, scalar1=sm["g"][:, h:h + 1], scalar2=None, op0=ALU.mult)
            S.vector.tensor_scalar(out=T_["nGtri"][:], in0=T_["Gtri"][:], scalar1=-1.0, scalar2=None, op0=ALU.mult)
            S.tensor.matmul(pG[:, 0:128], lhsT=ones, rhs=T_["Gtri"][:], start=True, stop=False)
            S.tensor.matmul(pG[:, 0:128], lhsT=T_["nGtri"][:], rhs=ones, start=False, stop=False)
            S.tensor.matmul(pG[:, 0:128], lhsT=ident, rhs=negT, start=False, stop=True)
            S.tensor.matmul(pG[:, 128:256], lhsT=T_["Gtri"][:], rhs=ones, start=True, stop=False)
            S.tensor.matmul(pG[:, 128:256], lhsT=ones, rhs=T_["nGtri"][:], start=False, stop=False)
            S.tensor.matmul(pG[:, 128:256], lhsT=ident, rhs=neg, start=False, stop=True)
            S.scalar.activation(out=T_["GT"][:], in_=pG[:, 0:128], func=AF.Exp)
            S.scalar.activation(out=T_["G"][:], in_=pG[:, 128:256], func=AF.Exp)
            S.vector.tensor_tensor(out=T_["GTs"][:], in0=T_["GT"][:], in1=mT_s, op=ALU.mult)
            S.vector.tensor_tensor(out=T_["Gs"][:], in0=T_["G"][:], in1=m_s, op=ALU.mult)
            if stop == 3:
                raise _Stop(P)
            S.vector.tensor_scalar(out=T_["qn"][:], in0=qh, scalar1=sm["rn"][:, h:h + 1], scalar2=128 ** -0.5, op0=ALU.mult, op1=ALU.mult)
            S.vector.tensor_scalar(out=T_["kn"][:], in0=kh, scalar1=sm["rn"][:, 4 + h:5 + h], scalar2=None, op0=ALU.mult)
            S.vector.tensor_scalar(out=T_["kb"][:], in0=T_["kn"][:], scalar1=sm["beta"][:, h:h + 1], scalar2=None, op0=ALU.mult)
            P.transpose(T_["kT"][:], T_["kn"][:], pT[0][:, 0:128])
            P.transpose(T_["kbT"][:], T_["kb"][:], pT[1][:, 0:128], eng="scalar")
            P.transpose(T_["qT"][:], T_["qn"][:], pT[0][:, 0:128])
            S.vector.tensor_scalar(out=T_["qd"][:], in0=T_["qn"][:], scalar1=sm["edec"][:, h:h + 1], scalar2=None, op0=ALU.mult)
            P.transpose(T_["qdT"][:], T_["qd"][:], pT[1][:, 0:128], eng="scalar")
            S.vector.tensor_scalar(out=T_["kd"][:], in0=T_["kn"][:], scalar1=sm["ek"][:, h:h + 1], scalar2=None, op0=ALU.mult)
            S.vector.tensor_scalar(out=T_["rhs_v"][:], in0=vh, scalar1=sm["beta"][:, h:h + 1], scalar2=None, op0=ALU.mult)
            S.vector.tensor_scalar(out=T_["rhs_k"][:], in0=T_["kb"][:], scalar1=sm["edec"][:, h:h + 1], scalar2=None, op0=ALU.mult)
            if stop == 4:
                raise _Stop(P)
            S.tensor.matmul(pA[:, 0:128], lhsT=T_["kT"][:], rhs=T_["kbT"][:], start=True, stop=True)
            S.tensor.matmul(pA[:, 128:256], lhsT=T_["kbT"][:], rhs=T_["kT"][:], start=True, stop=True)
            S.tensor.matmul(pA[:, 256:384], lhsT=T_["kT"][:], rhs=T_["qT"][:], start=True, stop=True)
            S.vector.scalar_tensor_tensor(out=X[0][:], in0=pA[:, 0:128], scalar=-1.0, in1=T_["GTs"][:], op0=ALU.mult, op1=ALU.mult)
            S.vector.scalar_tensor_tensor(out=Y[0][:], in0=pA[:, 128:256], scalar=-1.0, in1=T_["Gs"][:], op0=ALU.mult, op1=ALU.mult)
            S.vector.tensor_tensor(out=T_["QKT"][:], in0=pA[:, 256:384], in1=T_["GT"][:], op=ALU.mult)
            S.vector.tensor_tensor(out=Pm[0][:], in0=X[0][:], in1=ident, op=ALU.add)
            if stop == 5:
                raise _Stop(P)
            nst = 6
            for n in range(nst):
                a, c = n % 2, (n + 1) % 2
                if n < nst - 1:
                    S.tensor.matmul(pI[0][:, 0:128], lhsT=Y[a][:], rhs=X[a][:], start=True, stop=True)
                S.tensor.matmul(pI[0][:, 128:256], lhsT=X[a][:], rhs=Y[a][:], start=True, stop=True)
                if n < nst - 1:
                    S.scalar.copy(out=X[c][:], in_=pI[0][:, 0:128])
                S.vector.tensor_copy(out=Y[c][:], in_=pI[0][:, 128:256])
                S.tensor.matmul(pI[1][:, 0:128], lhsT=Y[c][:], rhs=Pm[a][:], start=True, stop=True)
                S.vector.tensor_tensor(out=Pm[c][:], in0=Pm[a][:], in1=pI[1][:, 0:128], op=ALU.add)
            TT = Pm[nst % 2]
            if stop == 6:
                raise _Stop(P)
            S.tensor.matmul(pS[:, 128:256], lhsT=T_["rhs_k"][:], rhs=TT[:], start=True, stop=True)
            S.scalar.mul(out=T_["nwT"][:], in_=pS[:, 128:256], mul=-1.0)
            S.tensor.matmul(pU[:, 0:128], lhsT=TT[:], rhs=T_["rhs_v"][:], start=True, stop=False)
            S.tensor.matmul(pU[:, 0:128], lhsT=T_["nwT"][:], rhs=state[:, h, :], start=False, stop=True)
            S.vector.tensor_copy(out=T_["u"][:], in_=pU[:, 0:128])
            S.tensor.matmul(pU[:, 128:256], lhsT=T_["qdT"][:], rhs=state[:, h, :], start=True, stop=False)
            S.tensor.matmul(pU[:, 128:256], lhsT=T_["QKT"][:], rhs=T_["u"][:], start=False, stop=True)
            S.tensor.matmul(pS[:, 256:384], lhsT=T_["kd"][:], rhs=T_["u"][:], start=True, stop=True)
            S.scalar.copy(out=T_["o"][:], in_=pU[:, 128:256])
            S.vector.scalar_tensor_tensor(out=state[:, h, :], in0=state[:, h, :], scalar=sm["edl"][:, h:h + 1], in1=pS[:, 256:384],
                                          op0=ALU.mult, op1=ALU.add)
            if stop == 7:
                raise _Stop(P)
            S.scalar.activation(out=T_["osq"][:], in_=T_["o"][:], func=AF.Square, accum_out=sm["ss2"][:, 0:1])
            S.scalar.activation(out=sm["rs"][:, 0:1], in_=sm["ss2"][:, 0:1], func=AF.Sqrt, bias=eps6[:, 0:1], scale=1.0 / 128)
            S.vector.reciprocal(out=sm["rs"][:, 0:1], in_=sm["rs"][:, 0:1])
            S.vector.scalar_tensor_tensor(out=Yt[:, h * 128:(h + 1) * 128], in0=T_["o"][:], scalar=sm["rs"][:, 0:1], in1=ngb[:],
                                          op0=ALU.mult, op1=ALU.mult)
        S.vector.tensor_tensor(out=Yt[:], in0=Yt[:], in1=sgt[b][:], op=ALU.mult)
        S.gpsimd.dma_start(out=y[t0:t0 + 128, :], in_=Yt[:])
    S.emit()
    return P.nc
def build_rwkv(S_tok):
    P = Prog()
    S = P.S
    z = P.din("z", [S_tok, 3848])
    vecs = P.din("vecs", [8, 512])
    mu = P.din("mu", [1, 1792])
    w_up = P.din("w_up", [64, 512])
    a_up = P.din("a_up", [64, 512])
    g_up = P.din("g_up", [128, 512])
    y = P.dout("y", [S_tok, 512])
    mub = P.bc_row("mub", mu, 1792)
    vb = [P.bc_row(f"vb{i}", vecs[i:i + 1, :], 512) for i in range(7)]
    w0b, a0b, kkb, kab, rkb, lgb, lbb = vb
    wup = P.sb("wup", [64, 512]); aup = P.sb("aup", [64, 512]); gup = P.sb("gup", [128, 512])
    S.sync.dma_start(out=wup[:], in_=w_up); S.sync.dma_start(out=aup[:], in_=a_up); S.sync.dma_start(out=gup[:], in_=g_up)
    H = P.sb("H", [64, 8, 64])
    S.vector.memset(H[:], 0.0)
    L = 64
    cur = P.rot("cur", [L, 1792]); prev = P.rot("prev", [L, 1792])
    big = {n: P.sb(f"r_{n}", [L, 512]) for n in ("logw", "a", "g", "kk", "kp", "b", "ecl", "ecm", "eni", "rt", "kat", "kt", "bt", "nbt",
                                                   "tmp", "Y", "Yn", "sq")}
    sm = {n: P.sb(f"rs_{n}", [L, 8]) for n in ("ss", "rn", "bs")}
    tw = P.sb("tw", [L, 64]); twT = P.sb("twT", [64, L]); adT = P.sb("adT", [64, L])
    sgd = P.sb("sgd", [L, 128]); sgdT = P.sb("sgdT", [128, L])
    PL = P.sb("PL", [64, 8])
    eps6 = P.sb("eps6", [128, 1])
    S.vector.memset(eps6[:], 1e-6)
    TN = ["rtT", "katT", "ktT", "btT"]
    TT_ = {n: P.rot(n, [64, L]) for n in TN}
    MN = ["AkvT", "BrkT", "nBrbT"]
    M_ = {n: P.rot(n, [L, L]) for n in MN}
    X = P.rot("X", [L, L]); Yq = P.rot("Yq", [L, L]); Pm = P.rot("Pm", [L, L])
    Wsb = P.rot("Wsb", [L, 64]); Usb = P.rot("Usb", [L, 64])
    lns = ln_scratch(P, "rln", D=64, eps=64e-5)
    pT = [S.ps(f"pT{i}", [128, 512]) for i in range(2)]
    pL = S.ps("pL", [128, 512])
    pA = [S.ps(f"pA{i}", [128, 512]) for i in range(2)]
    pI = [S.ps(f"pI{i}", [128, 512]) for i in range(2)]
    pC = S.ps("pC", [128, 512])
    ident, btri, chunkind = P.C("ident", L, L), P.C("tri", L, L), P.C("ones", L, 1)
    bmT_i, bmT_s, bm_s = P.C("tri", L, L), P.C("mT_strict", L, L), P.C("m_strict", L, L)
    B = big
    for t in range(S_tok // L):
        t0 = t * L
        b = t % 2
        Cc, Pp = cur[b], prev[b]
        S.sync.dma_start(out=Cc[:], in_=z[t0:t0 + L, 0:1792])
        if t == 0:
            S.vector.memset(Pp[0:32, :], 0.0)
            S.sync.dma_start(out=Pp[1:L, :], in_=z[0:L - 1, 0:1792])
        else:
            S.sync.dma_start(out=Pp[:], in_=z[t0 - 1:t0 + L - 1, 0:1792])
        S.vector.tensor_tensor(out=Pp[:], in0=Pp[:], in1=Cc[:], op=ALU.subtract)
        S.vector.tensor_tensor(out=Pp[:], in0=Pp[:], in1=mub[0:L, :], op=ALU.mult)
        S.vector.tensor_tensor(out=Cc[:], in0=Cc[:], in1=Pp[:], op=ALU.add)
        r, k, v = Cc[:, 0:512], Cc[:, 512:1024], Cc[:, 1024:1536]
        S.scalar.activation(out=tw[:], in_=Cc[:, 1536:1600], func=AF.Tanh)
        P.transpose(twT[:], tw[:], pT[0][0:64, 0:L], rows=L, cols=64)
        P.transpose(adT[:], Cc[:, 1600:1664], pT[1][0:64, 0:L], rows=L, cols=64, eng="scalar")
        S.scalar.activation(out=sgd[:], in_=Cc[:, 1664:1792], func=AF.Sigmoid)
        P.transpose(sgdT[:], sgd[:], pT[0][:, 0:L], rows=L, cols=128)
        S.tensor.matmul(pL[0:L, :], lhsT=twT[:], rhs=wup[:], start=True, stop=True)
        S.vector.tensor_tensor(out=B["logw"][:], in0=pL[0:L, :], in1=w0b[0:L, :], op=ALU.add)
        S.scalar.activation(out=B["logw"][:], in_=B["logw"][:], func=AF.Sigmoid)
        S.vector.tensor_scalar(out=B["logw"][:], in0=B["logw"][:], scalar1=-0.6065306597126334, scalar2=None, op0=ALU.mult)
        S.tensor.matmul(pL[0:L, :], lhsT=adT[:], rhs=aup[:], start=True, stop=True)
        S.vector.tensor_tensor(out=B["a"][:], in0=pL[0:L, :], in1=a0b[0:L, :], op=ALU.add)
        S.scalar.activation(out=B["a"][:], in_=B["a"][:], func=AF.Sigmoid)
        S.tensor.matmul(pL[0:L, :], lhsT=sgdT[:], rhs=gup[:], start=True, stop=True)
        S.scalar.copy(out=B["g"][:], in_=pL[0:L, :])
        S.vector.tensor_tensor(out=B["kk"][:], in0=k, in1=kkb[0:L, :], op=ALU.mult)
        S.scalar.activation(out=B["sq"][:], in_=B["kk"][:], func=AF.Square)
        S.vector.reduce_sum(out=sm["ss"][:, 0:8], in_=B["sq"][:].rearrange("p (h d) -> p h d", h=8), axis=AX.X)
        S.scalar.activation(out=sm["ss"][:, 0:8], in_=sm["ss"][:, 0:8], func=AF.Sqrt, bias=eps6[0:L, 0:1], scale=1.0)
        S.vector.reciprocal(out=sm["rn"][:, 0:8], in_=sm["ss"][:, 0:8])
        for h in range(8):
            S.vector.tensor_scalar(out=B["kk"][:, h * 64:(h + 1) * 64], in0=B["kk"][:, h * 64:(h + 1) * 64],
                                   scalar1=sm["rn"][:, h:h + 1], scalar2=None, op0=ALU.mult)
        S.vector.scalar_tensor_tensor(out=B["tmp"][:], in0=B["a"][:], scalar=-1.0, in1=kab[0:L, :], op0=ALU.add, op1=ALU.mult)
        S.vector.scalar_tensor_tensor(out=B["kp"][:], in0=B["tmp"][:], scalar=1.0, in1=k, op0=ALU.add, op1=ALU.mult)
        S.vector.tensor_tensor(out=B["b"][:], in0=B["kk"][:], in1=B["a"][:], op=ALU.mult)
        S.tensor.matmul(pL[0:L, :], lhsT=btri, rhs=B["logw"][:], start=True, stop=True)
        S.scalar.activation(out=B["ecl"][:], in_=pL[0:L, :], func=AF.Exp)
        S.scalar.activation(out=B["eni"][:], in_=pL[0:L, :], func=AF.Exp, scale=-1.0)
        S.vector.tensor_tensor(out=B["tmp"][:], in0=pL[0:L, :], in1=B["logw"][:], op=ALU.subtract)
        S.scalar.activation(out=B["ecm"][:], in_=B["tmp"][:], func=AF.Exp)
        S.vector.tensor_tensor(out=B["rt"][:], in0=r, in1=B["ecl"][:], op=ALU.mult)
        S.vector.tensor_tensor(out=B["kat"][:], in0=B["kk"][:], in1=B["ecm"][:], op=ALU.mult)
        S.vector.tensor_tensor(out=B["kt"][:], in0=B["kp"][:], in1=B["eni"][:], op=ALU.mult)
        S.vector.tensor_tensor(out=B["bt"][:], in0=B["b"][:], in1=B["eni"][:], op=ALU.mult)
        S.vector.tensor_scalar(out=B["nbt"][:], in0=B["bt"][:], scalar1=-1.0, scalar2=None, op0=ALU.mult)
        for h in range(8):
            S.tensor.matmul(pL[0:64, h:h + 1], lhsT=B["logw"][:, h * 64:(h + 1) * 64], rhs=chunkind, start=True, stop=True)
        S.scalar.activation(out=PL[:], in_=pL[0:64, 0:8], func=AF.Exp)
        S.vector.tensor_tensor(out=B["tmp"][:], in0=r, in1=B["kp"][:], op=ALU.mult)
        S.vector.tensor_tensor(out=B["tmp"][:], in0=B["tmp"][:], in1=rkb[0:L, :], op=ALU.mult)
        S.vector.reduce_sum(out=sm["bs"][:, 0:8], in_=B["tmp"][:].rearrange("p (h d) -> p h d", h=8), axis=AX.X)
        for h in range(8):
            u = h % 2
            hs = slice(h * 64, (h + 1) * 64)
            T_ = {n: TT_[n][u] for n in TN}
            Mh = {n: M_[n][u] for n in MN}
            P.transpose(T_["rtT"][:], B["rt"][:, hs], pT[0][0:64, 0:L], rows=L, cols=64)
            P.transpose(T_["katT"][:], B["kat"][:, hs], pT[1][0:64, 0:L], rows=L, cols=64, eng="scalar")
            P.transpose(T_["ktT"][:], B["kt"][:, hs], pT[0][0:64, 0:L], rows=L, cols=64)
            P.transpose(T_["btT"][:], B["bt"][:, hs], pT[1][0:64, 0:L], rows=L, cols=64, eng="scalar")
            S.tensor.matmul(pA[0][0:L, 0:0+L], lhsT=T_["btT"][:], rhs=T_["katT"][:], start=True, stop=True)
            S.tensor.matmul(pA[0][0:L, 128:128+L], lhsT=T_["katT"][:], rhs=T_["btT"][:], start=True, stop=True)
            S.tensor.matmul(pA[0][0:L, 256:256+L], lhsT=T_["ktT"][:], rhs=T_["katT"][:], start=True, stop=True)
            S.tensor.matmul(pA[1][0:L, 0:0+L], lhsT=T_["ktT"][:], rhs=T_["rtT"][:], start=True, stop=True)
            S.tensor.matmul(pA[1][0:L, 128:128+L], lhsT=T_["btT"][:], rhs=T_["rtT"][:], start=True, stop=True)
            S.vector.scalar_tensor_tensor(out=X[0][:], in0=pA[0][0:L, 0:0+L], scalar=-1.0, in1=bmT_s, op0=ALU.mult, op1=ALU.mult)
            S.vector.scalar_tensor_tensor(out=Yq[0][:], in0=pA[0][0:L, 128:128+L], scalar=-1.0, in1=bm_s, op0=ALU.mult, op1=ALU.mult)
            S.vector.tensor_tensor(out=Mh["AkvT"][:], in0=pA[0][0:L, 256:256+L], in1=bmT_s, op=ALU.mult)
            S.vector.tensor_tensor(out=Mh["BrkT"][:], in0=pA[1][0:L, 0:0+L], in1=bmT_i, op=ALU.mult)
            S.vector.scalar_tensor_tensor(out=Mh["nBrbT"][:], in0=pA[1][0:L, 128:128+L], scalar=-1.0, in1=bmT_i, op0=ALU.mult, op1=ALU.mult)
            S.vector.tensor_tensor(out=Pm[0][:], in0=X[0][:], in1=ident, op=ALU.add)
            nst = 5
            for n in range(nst):
                a_, c_ = n % 2, (n + 1) % 2
                if n < nst - 1:
                    S.tensor.matmul(pI[0][0:L, 0:0+L], lhsT=Yq[a_][:], rhs=X[a_][:], start=True, stop=True)
                S.tensor.matmul(pI[0][0:L, 128:128+L], lhsT=X[a_][:], rhs=Yq[a_][:], start=True, stop=True)
                if n < nst - 1:
                    S.scalar.copy(out=X[c_][:], in_=pI[0][0:L, 0:0+L])
                S.vector.tensor_copy(out=Yq[c_][:], in_=pI[0][0:L, 128:128+L])
                S.tensor.matmul(pI[1][0:L, 0:0+L], lhsT=Yq[c_][:], rhs=Pm[a_][:], start=True, stop=True)
                S.vector.tensor_tensor(out=Pm[c_][:], in0=Pm[a_][:], in1=pI[1][0:L, 0:0+L], op=ALU.add)
            TT = Pm[nst % 2]
            for c in range(1):
                cs = slice(0, L)
                H0 = H[:, h, :]
                S.tensor.matmul(pC[cs, 0:64], lhsT=T_["katT"][:, cs], rhs=H0, start=True, stop=False)
                S.tensor.matmul(pC[cs, 0:64], lhsT=Mh["AkvT"][cs, cs], rhs=v[cs, hs], start=False, stop=True)
                S.vector.tensor_copy(out=Wsb[u][cs, :], in_=pC[cs, 0:64])
                S.tensor.matmul(pC[cs, 64:128], lhsT=TT[cs, cs], rhs=Wsb[u][cs, :], start=True, stop=True)
                S.vector.tensor_copy(out=Usb[u][cs, :], in_=pC[cs, 64:128])
                S.tensor.matmul(pC[cs, 128:192], lhsT=T_["rtT"][:, cs], rhs=H0, start=True, stop=False)
                S.tensor.matmul(pC[cs, 128:192], lhsT=Mh["BrkT"][cs, cs], rhs=v[cs, hs], start=False, stop=False)
                S.tensor.matmul(pC[cs, 128:192], lhsT=Mh["nBrbT"][cs, cs], rhs=Usb[u][cs, :], start=False, stop=True)
                S.scalar.copy(out=B["Y"][cs, hs], in_=pC[cs, 128:192])
                S.tensor.matmul(pC[0:64, 192:256], lhsT=B["kt"][cs, hs], rhs=v[cs, hs], start=True, stop=False)
                S.tensor.matmul(pC[0:64, 192:256], lhsT=B["nbt"][cs, hs], rhs=Usb[u][cs, :], start=False, stop=True)
                S.vector.tensor_tensor(out=H0, in0=H0, in1=pC[0:64, 192:256], op=ALU.add)
                S.vector.tensor_scalar(out=H0, in0=H0, scalar1=PL[:, h:h + 1], scalar2=None, op0=ALU.mult)
            layernorm(P, B["Y"][:, hs], B["Yn"][:, hs], None, None, lns, D=64, eps=64e-5)
        S.vector.tensor_tensor(out=B["Yn"][:], in0=B["Yn"][:], in1=lgb[0:L, :], op=ALU.mult)
        S.vector.tensor_tensor(out=B["Yn"][:], in0=B["Yn"][:], in1=lbb[0:L, :], op=ALU.add)
        for h in range(8):
            hs = slice(h * 64, (h + 1) * 64)
            S.vector.scalar_tensor_tensor(out=B["Yn"][:, hs], in0=v[:, hs], scalar=sm["bs"][:, h:h + 1], in1=B["Yn"][:, hs],
                                          op0=ALU.mult, op1=ALU.add)
        S.vector.tensor_tensor(out=B["Yn"][:], in0=B["Yn"][:], in1=B["g"][:], op=ALU.mult)
        S.gpsimd.dma_start(out=y[t0:t0 + L, :], in_=B["Yn"][:])
    S.emit()
    return P.nc
TWO_PI = 6.283185307179586


def s5_host_layout(lam_re, lam_im, log_dt, b_re, b_im, c_re, c_im, d_skip, w_glu, b_glu):
    G, Pn = 32, 64
    rows = np.stack([lam_re.reshape(-1), lam_im.reshape(-1), np.repeat(log_dt, Pn)]).astype(np.float32)
    cols = np.stack([lam_re.reshape(16, 128).T, lam_im.reshape(16, 128).T, np.repeat(log_dt, Pn).reshape(16, 128).T], 0).astype(np.float32)
    bpad = np.zeros((2, 4, 128, 512), np.float32)
    for ct in range(4):
        for gl in range(8):
            g = 8 * ct + gl
            bpad[0, ct, gl * 16:(gl + 1) * 16, gl * 64:(gl + 1) * 64] = b_re[g].T
            bpad[1, ct, gl * 16:(gl + 1) * 16, gl * 64:(gl + 1) * 64] = b_im[g].T
    cpad = np.zeros((2, 16, 128, 128), np.float32)
    for st in range(16):
        for gg in range(2):
            g = 2 * st + gg
            gl = g % 8
            cpad[0, st, gg * 64:(gg + 1) * 64, gl * 16:(gl + 1) * 16] = c_re[g].T
            cpad[1, st, gg * 64:(gg + 1) * 64, gl * 16:(gl + 1) * 16] = c_im[g].T
    dcol = np.stack([d_skip.reshape(4, 128).T, b_glu.reshape(4, 128).T], 0).astype(np.float32)
    return {"rows": rows, "cols": cols, "bpad": bpad, "cpad": cpad, "dcol": dcol, "w_glu": np.ascontiguousarray(w_glu, np.float32)}


def build_s5(S_tok):
    P = Prog()
    S = P.S
    z = P.din("z", [S_tok, 2056])
    rows = P.din("rows", [3, 2048])
    cols = P.din("cols", [3, 128, 16])
    bpad = P.din("bpad", [2, 4, 128, 512])
    cpad = P.din("cpad", [2, 16, 128, 128])
    dcol = P.din("dcol", [2, 128, 4])
    w_glu = P.din("w_glu", [512, 512])
    y = P.dout("y", [S_tok, 512])
    I32_ = mybir.dt.int32
    R = lambda n: P.sb(n, [128, 2048])
    lr = P.bc_row("lr", rows[0:1, :], 2048)
    li = P.bc_row("li", rows[1:2, :], 2048)
    dt = P.bc_row("dtr", rows[2:3, :], 2048)
    a_r, th_r, s1, s2, s3, s4 = R("a_r"), R("th_r"), R("s1"), R("s2"), R("s3"), R("s4")
    si = P.sb("si", [128, 2048], I32_)
    Ei_re, Ei_im = R("Ei_re"), R("Ei_im")
    iota_col, iota_row = P.C("iota_col", 128, 1), P.C("iota_row")

    def sincos(turns, o_sin, o_cos, n):
        for off, o in ((0.0, o_sin), (0.25, o_cos)):
            if off:
                S.vector.tensor_scalar(out=s4[:, 0:n], in0=turns, scalar1=off, scalar2=None, op0=ALU.add)
                src = s4[:, 0:n]
            else:
                src = turns
            S.vector.tensor_copy(out=si[:, 0:n], in_=src)
            S.vector.tensor_copy(out=o, in_=si[:, 0:n])
            S.vector.tensor_tensor(out=o, in0=src, in1=o, op=ALU.subtract)
            S.scalar.activation(out=o, in_=o, func=AF.Sin, scale=TWO_PI)

    S.scalar.activation(out=dt[:], in_=dt[:], func=AF.Exp)
    S.vector.tensor_tensor(out=a_r[:], in0=lr[:], in1=dt[:], op=ALU.mult)
    S.vector.tensor_tensor(out=th_r[:], in0=li[:], in1=dt[:], op=ALU.mult)
    S.vector.tensor_scalar(out=th_r[:], in0=th_r[:], scalar1=1.0 / TWO_PI, scalar2=None, op0=ALU.mult)
    sincos(th_r[:], s1[:], s2[:], 2048)
    S.scalar.activation(out=s3[:], in_=a_r[:], func=AF.Exp)
    S.vector.tensor_tensor(out=s1[:], in0=s1[:], in1=s3[:], op=ALU.mult)
    S.vector.tensor_tensor(out=s2[:], in0=s2[:], in1=s3[:], op=ALU.mult)
    S.vector.tensor_scalar(out=s2[:], in0=s2[:], scalar1=-1.0, scalar2=None, op0=ALU.add)
    fr, fi = R("fr"), R("fi")
    S.vector.tensor_tensor(out=s3[:], in0=lr[:], in1=lr[:], op=ALU.mult)
    S.vector.tensor_tensor(out=s4[:], in0=li[:], in1=li[:], op=ALU.mult)
    S.vector.tensor_tensor(out=s3[:], in0=s3[:], in1=s4[:], op=ALU.add)
    S.vector.reciprocal(out=s3[:], in_=s3[:])
    S.vector.tensor_tensor(out=fr[:], in0=s2[:], in1=lr[:], op=ALU.mult)
    S.vector.tensor_tensor(out=s4[:], in0=s1[:], in1=li[:], op=ALU.mult)
    S.vector.tensor_tensor(out=fr[:], in0=fr[:], in1=s4[:], op=ALU.add)
    S.vector.tensor_tensor(out=fr[:], in0=fr[:], in1=s3[:], op=ALU.mult)
    S.vector.tensor_tensor(out=fi[:], in0=s1[:], in1=lr[:], op=ALU.mult)
    S.vector.tensor_tensor(out=s4[:], in0=s2[:], in1=li[:], op=ALU.mult)
    S.vector.tensor_tensor(out=fi[:], in0=fi[:], in1=s4[:], op=ALU.subtract)
    S.vector.tensor_tensor(out=fi[:], in0=fi[:], in1=s3[:], op=ALU.mult)
    bp_re, bp_im = P.sb("bp_re", [128, 4, 512]), P.sb("bp_im", [128, 4, 512])
    for ct in range(4):
        S.sync.dma_start(out=bp_re[:, ct, :], in_=bpad[0, ct])
        S.sync.dma_start(out=bp_im[:, ct, :], in_=bpad[1, ct])
    Bb_re, Bb_im = P.sb("Bb_re", [128, 2048]), P.sb("Bb_im", [128, 2048])
    bpr, bpi = bp_re[:].rearrange("p a b -> p (a b)"), bp_im[:].rearrange("p a b -> p (a b)")
    S.vector.tensor_tensor(out=Bb_re[:], in0=fr[:], in1=bpr, op=ALU.mult)
    S.vector.tensor_tensor(out=s4[:], in0=fi[:], in1=bpi, op=ALU.mult)
    S.vector.tensor_tensor(out=Bb_re[:], in0=Bb_re[:], in1=s4[:], op=ALU.subtract)
    S.vector.tensor_tensor(out=Bb_im[:], in0=fr[:], in1=bpi, op=ALU.mult)
    S.vector.tensor_tensor(out=s4[:], in0=fi[:], in1=bpr, op=ALU.mult)
    S.vector.tensor_tensor(out=Bb_im[:], in0=Bb_im[:], in1=s4[:], op=ALU.add)
    S.vector.tensor_scalar(out=s3[:], in0=th_r[:], scalar1=iota_col, scalar2=None, op0=ALU.mult)
    sincos(s3[:], s1[:], s2[:], 2048)
    S.vector.tensor_scalar(out=s3[:], in0=a_r[:], scalar1=iota_col, scalar2=-1.0, op0=ALU.mult, op1=ALU.mult)
    S.scalar.activation(out=s3[:], in_=s3[:], func=AF.Exp)
    S.vector.tensor_tensor(out=Ei_re[:], in0=s2[:], in1=s3[:], op=ALU.mult)
    S.vector.scalar_tensor_tensor(out=Ei_im[:], in0=s1[:], scalar=-1.0, in1=s3[:], op0=ALU.mult, op1=ALU.mult)
    EL_re, EL_im = fr, fi
    S.vector.tensor_scalar(out=s3[:], in0=th_r[:], scalar1=128.0, scalar2=None, op0=ALU.mult)
    sincos(s3[:], s1[:], s2[:], 2048)
    S.scalar.activation(out=s3[:], in_=a_r[:], func=AF.Exp, scale=128.0)
    S.vector.tensor_tensor(out=EL_re[:], in0=s2[:], in1=s3[:], op=ALU.mult)
    S.vector.tensor_tensor(out=EL_im[:], in0=s1[:], in1=s3[:], op=ALU.mult)
    cl = P.sb("cl", [128, 3, 16])
    for i in range(3):
        S.sync.dma_start(out=cl[:, i, :], in_=cols[i])
    S.scalar.activation(out=cl[:, 2, :], in_=cl[:, 2, :], func=AF.Exp)
    S.vector.tensor_tensor(out=cl[:, 0, :], in0=cl[:, 0, :], in1=cl[:, 2, :], op=ALU.mult)
    S.vector.tensor_tensor(out=cl[:, 1, :], in0=cl[:, 1, :], in1=cl[:, 2, :], op=ALU.mult)
    S.vector.tensor_scalar(out=cl[:, 1, :], in0=cl[:, 1, :], scalar1=1.0 / TWO_PI, scalar2=None, op0=ALU.mult)
    E_re, E_im = P.sb("E_re", [128, 16, 128]), P.sb("E_im", [128, 16, 128])
    for st in range(16):
        S.vector.tensor_scalar(out=s3[:, 0:128], in0=iota_row, scalar1=cl[:, 1, st:st + 1], scalar2=None, op0=ALU.mult)
        sincos(s3[:, 0:128], s1[:, 0:128], s2[:, 0:128], 128)
        S.scalar.activation(out=s3[:, 0:128], in_=iota_row, func=AF.Exp, scale=cl[:, 0, st:st + 1])
        S.vector.tensor_tensor(out=E_re[:, st, :], in0=s2[:, 0:128], in1=s3[:, 0:128], op=ALU.mult)
        S.vector.tensor_tensor(out=E_im[:, st, :], in0=s1[:, 0:128], in1=s3[:, 0:128], op=ALU.mult)
    cp_re, cp_im = P.sb("cp_re", [128, 16, 128]), P.sb("cp_im", [128, 16, 128])
    for st in range(16):
        S.sync.dma_start(out=cp_re[:, st, :], in_=cpad[0, st])
        S.sync.dma_start(out=cp_im[:, st, :], in_=cpad[1, st])
    dc = P.sb("dc", [128, 2, 4])
    for i in range(2):
        S.sync.dma_start(out=dc[:, i, :], in_=dcol[i])
    wg = P.sb("wg", [128, 4, 512])
    for k in range(4):
        S.sync.dma_start(out=wg[:, k, :], in_=w_glu[k * 128:(k + 1) * 128, :])
    Xr_re, Xr_im, Xn_re, Xn_im, Xt = lr[0:1, :], li[0:1, :], dt[0:1, :], th_r[0:1, :], bpr[0:1, :]
    S.vector.memset(Xr_re, 0.0)
    S.vector.memset(Xr_im, 0.0)
    ut = P.rot("ut", [128, 512])
    uT = P.rot("uT", [128, 4, 128])
    V_re, V_im = s1, s2
    x_re, nx_im = s3, a_r
    tA, tB = s4[:, 0:512], s4[:, 512:1024]
    gl = P.sb("gl", [128, 4, 128])
    g1, g2 = P.sb("g1", [128, 128]), P.sb("g2", [128, 128])
    yaT = P.sb("yaT", [128, 4, 128])
    yt = ut
    pT = [S.ps(f"pT{i}", [128, 512]) for i in range(2)]
    pB = [S.ps(f"pB{i}", [128, 512]) for i in range(2)]
    pS_ = [S.ps(f"pS{i}", [128, 512]) for i in range(2)]
    pY = S.ps("pY", [128, 512])
    pX = S.ps("pX", [128, 512])
    ident, tri, ones = P.C("ident"), P.C("tri"), P.C("ones")
    for t in range(S_tok // 128):
        t0 = t * 128
        b = t % 2
        S.sync.dma_start(out=ut[b][:], in_=z[t0:t0 + 128, 0:512])
        for k in range(4):
            P.transpose(uT[b][:, k, :], ut[b][:, k * 128:(k + 1) * 128], pT[k % 2][:, 0:128], eng="vector" if k % 2 == 0 else "scalar")
        for ct in range(4):
            cs = slice(ct * 512, (ct + 1) * 512)
            S.tensor.matmul(pB[0][:], lhsT=uT[b][:, ct, :], rhs=Bb_re[:, cs], start=True, stop=True)
            S.tensor.matmul(pB[1][:], lhsT=uT[b][:, ct, :], rhs=Bb_im[:, cs], start=True, stop=True)
            S.vector.tensor_tensor(out=V_re[:, cs], in0=pB[0][:], in1=Ei_re[:, cs], op=ALU.mult)
            S.vector.tensor_tensor(out=tA, in0=pB[1][:], in1=Ei_im[:, cs], op=ALU.mult)
            S.vector.tensor_tensor(out=V_re[:, cs], in0=V_re[:, cs], in1=tA, op=ALU.subtract)
            S.vector.tensor_tensor(out=V_im[:, cs], in0=pB[0][:], in1=Ei_im[:, cs], op=ALU.mult)
            S.vector.tensor_tensor(out=tB, in0=pB[1][:], in1=Ei_re[:, cs], op=ALU.mult)
            S.vector.tensor_tensor(out=V_im[:, cs], in0=V_im[:, cs], in1=tB, op=ALU.add)
        S.vector.tensor_tensor(out=V_re[0:1, :], in0=V_re[0:1, :], in1=Xr_re, op=ALU.add)
        S.vector.tensor_tensor(out=V_im[0:1, :], in0=V_im[0:1, :], in1=Xr_im, op=ALU.add)
        for q in range(4):
            S.tensor.matmul(pX[:, :], lhsT=ones, rhs=V_re[:, q * 512:(q + 1) * 512], start=True, stop=True)
            S.vector.tensor_copy(out=Xn_re[:, q * 512:(q + 1) * 512], in_=pX[0:1, :])
            S.tensor.matmul(pX[:, :], lhsT=ones, rhs=V_im[:, q * 512:(q + 1) * 512], start=True, stop=True)
            S.vector.tensor_copy(out=Xn_im[:, q * 512:(q + 1) * 512], in_=pX[0:1, :])
        S.vector.tensor_tensor(out=Xr_re, in0=Xn_re, in1=EL_re[0:1, :], op=ALU.mult)
        S.vector.tensor_tensor(out=Xt, in0=Xn_im, in1=EL_im[0:1, :], op=ALU.mult)
        S.vector.tensor_tensor(out=Xr_re, in0=Xr_re, in1=Xt, op=ALU.subtract)
        S.vector.tensor_tensor(out=Xr_im, in0=Xn_re, in1=EL_im[0:1, :], op=ALU.mult)
        S.vector.tensor_tensor(out=Xt, in0=Xn_im, in1=EL_re[0:1, :], op=ALU.mult)
        S.vector.tensor_tensor(out=Xr_im, in0=Xr_im, in1=Xt, op=ALU.add)
        for ft in range(4):
            for q in range(4):
                st = ft * 4 + q
                S.tensor.matmul(pS_[0][:, q * 128:(q + 1) * 128], lhsT=V_re[:, st * 128:(st + 1) * 128], rhs=tri, start=True, stop=True)
                S.tensor.matmul(pS_[1][:, q * 128:(q + 1) * 128], lhsT=V_im[:, st * 128:(st + 1) * 128], rhs=tri, start=True, stop=True)
            cs = slice(ft * 512, (ft + 1) * 512)
            Er = E_re[:, ft * 4:(ft + 1) * 4, :].rearrange("p a b -> p (a b)")
            Em = E_im[:, ft * 4:(ft + 1) * 4, :].rearrange("p a b -> p (a b)")
            S.vector.tensor_tensor(out=x_re[:, cs], in0=pS_[0][:], in1=Er, op=ALU.mult)
            S.vector.tensor_tensor(out=tA, in0=pS_[1][:], in1=Em, op=ALU.mult)
            S.vector.tensor_tensor(out=x_re[:, cs], in0=x_re[:, cs], in1=tA, op=ALU.subtract)
            S.vector.tensor_tensor(out=nx_im[:, cs], in0=pS_[1][:], in1=Er, op=ALU.mult)
            S.vector.tensor_tensor(out=tB, in0=pS_[0][:], in1=Em, op=ALU.mult)
            S.vector.scalar_tensor_tensor(out=nx_im[:, cs], in0=nx_im[:, cs], scalar=-1.0, in1=tB, op0=ALU.mult, op1=ALU.subtract)
            for q in range(4):
                st = ft * 4 + q
                S.tensor.matmul(pY[:, 0:128], lhsT=cp_re[:, st, :], rhs=x_re[:, st * 128:(st + 1) * 128], start=(q == 0), stop=False)
                S.tensor.matmul(pY[:, 0:128], lhsT=cp_im[:, st, :], rhs=nx_im[:, st * 128:(st + 1) * 128], start=False, stop=(q == 3))
            S.vector.scalar_tensor_tensor(out=g1[:], in0=uT[b][:, ft, :], scalar=dc[:, 0, ft:ft + 1], in1=pY[:, 0:128], op0=ALU.mult, op1=ALU.add)
            S.vector.tensor_tensor(out=g2[:], in0=g1[:], in1=g1[:], op=ALU.mult)
            S.vector.tensor_scalar(out=g2[:], in0=g2[:], scalar1=0.044715, scalar2=1.0, op0=ALU.mult, op1=ALU.add)
            S.vector.tensor_tensor(out=g2[:], in0=g2[:], in1=g1[:], op=ALU.mult)
            S.scalar.activation(out=g2[:], in_=g2[:], func=AF.Tanh, scale=0.7978845608028654)
            S.vector.scalar_tensor_tensor(out=g2[:], in0=g2[:], scalar=1.0, in1=g1[:], op0=ALU.add, op1=ALU.mult)
            S.vector.tensor_scalar(out=gl[:, ft, :], in0=g2[:], scalar1=0.5, scalar2=None, op0=ALU.mult)
        for f2 in range(4):
            for ft in range(4):
                S.tensor.matmul(pY[:, 128:256], lhsT=wg[:, ft, f2 * 128:(f2 + 1) * 128], rhs=gl[:, ft, :], start=(ft == 0), stop=(ft == 3))
            S.scalar.activation(out=g1[:], in_=pY[:, 128:256], func=AF.Sigmoid, bias=dc[:, 1, f2:f2 + 1], scale=1.0)
            S.vector.tensor_tensor(out=yaT[:, f2, :], in0=g1[:], in1=gl[:, f2, :], op=ALU.mult)
        for k in range(4):
            P.transpose(yt[b][:, k * 128:(k + 1) * 128], yaT[:, k, :], pT[k % 2][:, 0:128], eng="vector" if k % 2 == 0 else "scalar")
        S.gpsimd.dma_start(out=y[t0:t0 + 128, :], in_=yt[b][:])
    S.emit()
    return P.nc
_PROGS = {}


def _prog(key, builder, *args):
    if key not in _PROGS:
        _PROGS[key] = builder(*args)
    return _PROGS[key]


def _launch(nc, per_core, shared, outname):
    in_maps = []
    for c in range(8):
        d = {"consts": CONST_ARR}
        d.update(shared)
        d.update({k: v[c] for k, v in per_core.items()})
        in_maps.append(d)
    res = run_bass_kernel_spmd(nc, in_maps, core_ids=list(range(8)))
    return [np.asarray(r[outname]) for r in res.results]


def kernel(**inp):
    inp = {k: np.ascontiguousarray(np.asarray(v, dtype=np.float32)) for k, v in inp.items()}
    S_tok = inp["x"].shape[1]
    x = [inp["x"][c] for c in range(8)]
    for layer in range(4):
        i = layer // 2
        lnp = np.ascontiguousarray(np.stack([inp["ln_g"][layer, 0], inp["ln_b"][layer, 0], inp["ln_g"][layer, 1], inp["ln_b"][layer, 1]]))
        if layer % 2 == 0:
            z = _launch(_prog("proj_e", build_proj, S_tok, 2056), {"x": x}, {"w": inp["ev_w_in"][i]}, "z")
            lay = s5_host_layout(inp["s5_lam_re"][i], inp["s5_lam_im"][i], inp["s5_log_dt"][i], inp["s5_b_re"][i], inp["s5_b_im"][i],
                                 inp["s5_c_re"][i], inp["s5_c_im"][i], inp["s5_d"][i], inp["s5_w_glu"][i], inp["s5_b_glu"][i])
            ya = _launch(_prog("s5", build_s5, S_tok), {"z": z}, lay, "y")
            yb = _launch(_prog("mlstm", build_mlstm, S_tok), {"z": z},
                         {"gb": inp["ml_gate_bias"][i][None], "ng": inp["ml_norm_g"][i][None]}, "y")
            x = _launch(_prog("cf_e", build_cf, S_tok, 1, 2816), {"x": x, "ya": ya, "yb": yb},
                        {"w_out": inp["ev_w_out"][i], "lnp": lnp, "wg": inp["ffn_w_gate"][i][None], "wu": inp["ffn_w_up"][i][None],
                         "wd": inp["ffn_w_down"][i][None]}, "xo")
        else:
            z = _launch(_prog("proj_o", build_proj, S_tok, 3848), {"x": x}, {"w": inp["od_w_in"][i]}, "z")
            vecs = np.ascontiguousarray(np.stack([inp["rw_w0"][i], inp["rw_a0"][i], inp["rw_k_k"][i], inp["rw_k_a"][i], inp["rw_r_k"][i],
                                                  inp["rw_ln_g"][i], inp["rw_ln_b"][i], inp["rw_ln_b"][i]]))
            yc = _launch(_prog("rwkv", build_rwkv, S_tok), {"z": z},
                         {"vecs": vecs, "mu": inp["rw_mu"][i][None], "w_up": inp["rw_w_up"][i], "a_up": inp["rw_a_up"][i],
                          "g_up": inp["rw_g_up"][i]}, "y")
            yd = _launch(_prog("gdn", build_gdn, S_tok), {"z": z},
                         {"conv_w": inp["gd_conv"][i], "a_log": inp["gd_a_log"][i][None], "dt_bias": inp["gd_dt_bias"][i][None],
                          "norm_g": inp["gd_norm_g"][i][None]}, "y")
            x = _launch(_prog("cf_o", build_cf, S_tok, 8, 3584), {"x": x, "ya": yc, "yb": yd},
                        {"w_out": inp["od_w_out"][i], "lnp": lnp, "wg": inp["moe_w_gate"][i], "wu": inp["moe_w_up"][i],
                         "wd": inp["moe_w_down"][i], "router": inp["moe_router"][i]}, "xo")
    return np.stack(x).astype(np.float32)
```

```python
import numpy as np
import concourse.bass as bass
import concourse.mybir as mybir
from concourse.bass_utils import run_bass_kernel_spmd

F32 = mybir.dt.float32
BF16 = mybir.dt.bfloat16
I32 = mybir.dt.int32
AF = mybir.ActivationFunctionType
ALU = mybir.AluOpType
AX = mybir.AxisListType

ENGS = ["tensor", "vector", "scalar", "gpsimd", "sync"]
WRITE_KW = ("out", "accum_out", "out_max", "out_indices")
SEM_LIMIT = 30000
N_DMA_SEMS = 12


def _is_ap(x):
    return hasattr(x, "tensor") and hasattr(x, "ap")


class _EngProxy:
    def __init__(self, sched, eng):
        self._s = sched
        self._e = eng

    def __getattr__(self, meth):
        def call(*args, **kwargs):
            return self._s._record(self._e, meth, args, kwargs)
        return call


class Sched:
    def __init__(self, nc, same_engine_sync=True, prefix=""):
        self.nc = nc
        self.prefix = prefix
        self.ops = []
        self.same_engine_sync = same_engine_sync
        self.tensor = _EngProxy(self, "tensor")
        self.vector = _EngProxy(self, "vector")
        self.scalar = _EngProxy(self, "scalar")
        self.gpsimd = _EngProxy(self, "gpsimd")
        self.sync = _EngProxy(self, "sync")
        self._ctx = []

    def sb(self, name, shape, dtype=F32):
        g = self.nc.sbuf_tensor(self.prefix + name, list(shape), dtype)
        t = g.__enter__()
        self._ctx.append(g)
        return t

    def ps(self, name, shape, dtype=F32):
        g = self.nc.psum_tensor(self.prefix + name, list(shape), dtype)
        t = g.__enter__()
        self._ctx.append(g)
        return t

    @staticmethod
    def _box(a):
        name = a.tensor.name
        apl = a.ap
        off = a.offset
        if "DRam" in type(a.tensor).__name__:
            ext = sum(st * (c - 1) for st, c in apl) + 1
            return (name, 0, 1, off, off + ext)
        if "PSum" in type(a.tensor).__name__:
            return (name, 0, 128, 0, 1 << 30)
        row = apl[0][0]
        if row <= 0:
            row = 1 << 30
        p0 = off // row
        f0 = off % row
        ext = sum(st * (c - 1) for st, c in apl[1:]) + 1
        return (name, p0, p0 + apl[0][1], f0, f0 + ext)

    def _record(self, eng, meth, args, kwargs):
        reads, writes = [], []
        extra_r = kwargs.pop("_reads", None)
        extra_w = kwargs.pop("_writes", None)
        for i, a in enumerate(args):
            if _is_ap(a):
                (writes if i == 0 else reads).append(self._box(a))
        for k, a in kwargs.items():
            if _is_ap(a):
                (writes if k in WRITE_KW else reads).append(self._box(a))
        if extra_r:
            reads += [self._box(a) for a in extra_r]
        if extra_w:
            writes += [self._box(a) for a in extra_w]
        for bx in list(reads):
            if bx[4] == (1 << 30) and bx not in writes:
                writes.append(bx)
        is_dma = meth in ("dma_start", "dma_start_transpose", "indirect_dma_start")
        self.ops.append(dict(eng=eng, meth=meth, args=args, kwargs=kwargs,
                             reads=reads, writes=writes, dma=is_dma))
        return len(self.ops) - 1

    def emit(self):
        nc = self.nc
        ops = self.ops
        n = len(ops)
        W = {}
        R = {}
        deps = [None] * n

        def ov(a, b):
            return a[1] < b[2] and b[1] < a[2] and a[3] < b[4] and b[3] < a[4]

        def inside(a, b):
            return a[1] >= b[1] and a[2] <= b[2] and a[3] >= b[3] and a[4] <= b[4]

        for i, op in enumerate(ops):
            d = set()
            for bx in op["reads"]:
                for (b2, j) in W.get(bx[0], ()):
                    if ov(bx, b2):
                        d.add(j)
            for bx in op["writes"]:
                for (b2, j) in W.get(bx[0], ()):
                    if ov(bx, b2):
                        d.add(j)
                for (b2, j) in R.get(bx[0], ()):
                    if ov(bx, b2):
                        d.add(j)
            d.discard(i)
            deps[i] = d
            for bx in op["writes"]:
                nm = bx[0]
                W[nm] = [(b2, j) for (b2, j) in W.get(nm, ()) if not inside(b2, bx)] + [(bx, i)]
                R[nm] = [(b2, j) for (b2, j) in R.get(nm, ()) if not inside(b2, bx)]
            for bx in op["reads"]:
                nm = bx[0]
                lst = [(b2, j) for (b2, j) in R.get(nm, ()) if not (b2 == bx and ops[j]["eng"] == op["eng"] and not ops[j]["dma"])]
                lst.append((bx, i))
                R[nm] = lst
        needed = [False] * n
        for i, op in enumerate(ops):
            keep = set()
            for j in deps[i]:
                pj = ops[j]
                if pj["eng"] == op["eng"] and not pj["dma"]:
                    if op["eng"] == "tensor" and not op["dma"]:
                        continue
                    if op["eng"] == "sync":
                        continue
                    if not self.same_engine_sync and not op["dma"]:
                        continue
                keep.add(j)
            deps[i] = keep
            for j in keep:
                needed[j] = True
        sems = {}
        self._semguards = []

        def new_sem(name):
            return nc.alloc_semaphore(name=self.prefix + name)

        eng_sem = {e: new_sem(f"s_{e}_0") for e in ENGS}
        eng_cnt = {e: 0 for e in ENGS}
        eng_gen = {e: 0 for e in ENGS}
        dma_sems = {e: [new_sem(f"d_{e}_{i}") for i in range(N_DMA_SEMS)] for e in ("sync", "gpsimd", "scalar")}
        dma_cnt = {e: [0] * N_DMA_SEMS for e in dma_sems}
        dma_rr = {e: 0 for e in dma_sems}
        dma_last_tok = {e: [None] * N_DMA_SEMS for e in dma_sems}
        token = [None] * n
        prewait = [None] * n
        for i, op in enumerate(ops):
            e = op["eng"]
            if op["dma"]:
                r = dma_rr[e]
                dma_rr[e] = (r + 1) % N_DMA_SEMS
                prewait[i] = dma_last_tok[e][r]
                dma_cnt[e][r] += 16
                if dma_cnt[e][r] > SEM_LIMIT:
                    dma_sems[e][r] = new_sem(f"d_{e}_{r}_{i}")
                    dma_cnt[e][r] = 16
                    prewait[i] = dma_last_tok[e][r]
                token[i] = (dma_sems[e][r], dma_cnt[e][r])
                dma_last_tok[e][r] = token[i]
            elif needed[i]:
                if eng_cnt[e] >= SEM_LIMIT:
                    eng_gen[e] += 1
                    eng_sem[e] = new_sem(f"s_{e}_{eng_gen[e]}")
                    eng_cnt[e] = 0
                eng_cnt[e] += 1
                token[i] = (eng_sem[e], eng_cnt[e])
        final_tokens = []
        for e in dma_sems:
            for t in dma_last_tok[e]:
                if t is not None:
                    final_tokens.append(t)
        per_eng = {e: [] for e in ENGS}
        for i, op in enumerate(ops):
            per_eng[op["eng"]].append(i)
        self.n_waits = 0
        sched = self

        def run_engine(ename, eobj):
            seen = {}
            def wait(tok):
                s, v = tok
                key = id(s)
                if seen.get(key, 0) >= v:
                    return
                seen[key] = v
                eobj.wait_ge(s, v)
                sched.n_waits += 1
            for i in per_eng[ename]:
                op = ops[i]
                for j in sorted(deps[i]):
                    wait(token[j])
                if prewait[i] is not None:
                    wait(prewait[i])
                ins = getattr(eobj, op["meth"])(*op["args"], **op["kwargs"])
                if token[i] is not None:
                    ins.then_inc(token[i][0], 16 if op["dma"] else 1)
            if ename == "sync":
                for t in final_tokens:
                    wait(t)

        with nc.Block() as block:
            @block.tensor
            def _(e):
                run_engine("tensor", e)

            @block.vector
            def _(e):
                run_engine("vector", e)

            @block.scalar
            def _(e):
                run_engine("scalar", e)

            @block.gpsimd
            def _(e):
                run_engine("gpsimd", e)

            @block.sync
            def _(e):
                run_engine("sync", e)
        return nc
DN_ALPHA = 8 ** 0.25
LN_EPS = 1e-5


def make_consts():
    c = {}
    i = np.arange(128)
    c["ident"] = np.eye(128, dtype=np.float32)
    c["tri"] = (i[:, None] <= i[None, :]).astype(np.float32)
    c["ones"] = np.ones((128, 128), np.float32)
    c["negT"] = np.where(i[:, None] <= i[None, :], 0.0, -30000.0).astype(np.float32)
    c["neg"] = np.where(i[:, None] >= i[None, :], 0.0, -30000.0).astype(np.float32)
    c["mT_strict"] = (i[:, None] < i[None, :]).astype(np.float32)
    c["m_strict"] = (i[:, None] > i[None, :]).astype(np.float32)
    blk = (i[:, None] // 64) == (i[None, :] // 64)
    c["btri"] = ((i[:, None] <= i[None, :]) & blk).astype(np.float32)
    c["bmT_incl"] = ((i[:, None] <= i[None, :]) & blk).astype(np.float32)
    c["bmT_strict"] = ((i[:, None] < i[None, :]) & blk).astype(np.float32)
    c["bm_strict"] = ((i[:, None] > i[None, :]) & blk).astype(np.float32)
    ci = np.zeros((128, 128), np.float32)
    ci[:64, 0] = 1.0
    ci[64:, 1] = 1.0
    c["chunkind"] = ci
    c["iota_row"] = np.tile(i[None, :].astype(np.float32), (128, 1))
    c["iota_col"] = np.tile(i[:, None].astype(np.float32), (1, 128))
    names = list(c.keys())
    arr = np.concatenate([c[k] for k in names], axis=1)
    offs = {k: n * 128 for n, k in enumerate(names)}
    return arr, offs


CONST_ARR, CONST_OFF = make_consts()


class Prog:
    def __init__(self, nc=None, prefix="", io=None):
        self.nc = nc if nc is not None else bass.Bass("TRN2", target_bir_lowering=False)
        self.io = io or {}
        self.S = Sched(self.nc, prefix=prefix)
        self.cd = self.din("consts", CONST_ARR.shape)
        self.csb = self.S.sb("csb", CONST_ARR.shape)
        self.S.sync.dma_start(out=self.csb[:], in_=self.cd)
        self._n = 0

    def din(self, name, shape):
        if name in self.io:
            return self.io[name]
        return self.nc.dram_tensor(name, list(shape), F32, kind="ExternalInput").ap()

    def dout(self, name, shape):
        if name in self.io:
            return self.io[name]
        return self.nc.dram_tensor(name, list(shape), F32, kind="ExternalOutput").ap()

    def C(self, name, rows=128, cols=128):
        o = CONST_OFF[name]
        return self.csb[0:rows, o:o + cols]

    def sb(self, name, shape, dtype=F32):
        return self.S.sb(name, shape, dtype)

    def rot(self, name, shape, n=2):
        return [self.S.sb(f"{name}_{i}", shape) for i in range(n)]

    def bc_row(self, name, dram_row, n):
        t = self.S.sb(name, [128, n])
        self.S.sync.dma_start(out=t[:], in_=dram_row.partition_broadcast(128))
        return t

    def transpose(self, dst, src, ps, rows=128, cols=128, eng="vector"):
        S = self.S
        S.tensor.transpose(out=ps, in_=src, identity=self.C("ident", rows, rows))
        if eng == "vector":
            S.vector.tensor_copy(out=dst, in_=ps)
        else:
            S.scalar.copy(out=dst, in_=ps)


def layernorm(P, src, dst, g_bc, b_bc, scr, D=1024, eps=LN_EPS):
    S = P.S
    n = src.shape[0]
    st = {k: v[0:n, :] for k, v in scr.items()}
    S.vector.reduce_sum(out=st["s1"], in_=src, axis=AX.X)
    S.vector.tensor_scalar(out=st["mean"], in0=st["s1"], scalar1=-1.0 / D, scalar2=None, op0=ALU.mult)
    S.vector.tensor_scalar(out=dst, in0=src, scalar1=st["mean"][:, 0:1], scalar2=None, op0=ALU.add)
    S.scalar.activation(out=st["sq"][:, 0:D], in_=dst, func=AF.Square, accum_out=st["ss"])
    S.scalar.activation(out=st["std"], in_=st["ss"], func=AF.Sqrt, bias=st["eps"][:, 0:1], scale=1.0 / D)
    S.vector.reciprocal(out=st["rstd"], in_=st["std"])
    if g_bc is not None:
        S.vector.scalar_tensor_tensor(out=dst, in0=dst, scalar=st["rstd"][:, 0:1], in1=g_bc, op0=ALU.mult, op1=ALU.mult)
        S.vector.tensor_tensor(out=dst, in0=dst, in1=b_bc, op=ALU.add)
    else:
        S.vector.tensor_scalar(out=dst, in0=dst, scalar1=st["rstd"][:, 0:1], scalar2=None, op0=ALU.mult)


def ln_scratch(P, name, D=1024, eps=LN_EPS):
    st = {k: P.sb(f"{name}_{k}", [128, 1]) for k in ("s1", "mean", "ss", "std", "rstd", "eps")}
    st["sq"] = P.sb(f"{name}_sq", [128, D])
    P.S.vector.memset(st["eps"][:], eps)
    return st


def build_proj(S_tok, C, P=None):
    P = P or Prog()
    S = P.S
    x = P.din("x", [S_tok, 1024])
    w = P.din("w", [1024, C])
    z = P.dout("z", [S_tok, C])
    wsb = P.sb("wsb", [128, 8, C])
    for k in range(8):
        S.sync.dma_start(out=wsb[:, k, :], in_=w[k * 128:(k + 1) * 128, :])
    nt = S_tok // 128
    xt = P.rot("xt", [128, 1024])
    xT = P.rot("xT", [128, 8, 128])
    zt = P.rot("zt", [128, C])
    pst = [S.ps(f"pst{i}", [128, 512]) for i in range(2)]
    psm = [S.ps(f"psm{i}", [128, 512]) for i in range(4)]
    chunks = [(c0, min(512, C - c0)) for c0 in range(0, C, 512)]
    for t in range(nt):
        xs = xt[t % 2]
        S.sync.dma_start(out=xs[:], in_=x[t * 128:(t + 1) * 128, :])
        for k in range(8):
            P.transpose(xT[t % 2][:, k, :], xs[:, k * 128:(k + 1) * 128], pst[k % 2][:, 0:128],
                        eng="vector" if k % 2 == 0 else "scalar")
        for ci, (c0, cw) in enumerate(chunks):
            ps = psm[ci % 4]
            for k in range(8):
                S.tensor.matmul(ps[:, 0:cw], lhsT=xT[t % 2][:, k, :], rhs=wsb[:, k, c0:c0 + cw],
                                start=(k == 0), stop=(k == 7))
            if ci % 2 == 0:
                S.vector.tensor_copy(out=zt[t % 2][:, c0:c0 + cw], in_=ps[:, 0:cw])
            else:
                S.scalar.copy(out=zt[t % 2][:, c0:c0 + cw], in_=ps[:, 0:cw])
        S.gpsimd.dma_start(out=z[t * 128:(t + 1) * 128, :], in_=zt[t % 2][:])
    S.emit()
    return P.nc
def build_cf(S_tok, n_exp, d_ff, P=None):
    P = P or Prog()
    S = P.S
    x = P.din("x", [S_tok, 1024])
    ya = P.din("ya", [S_tok, 512])
    yb = P.din("yb", [S_tok, 512])
    w_out = P.din("w_out", [1024, 1024])
    lnp = P.din("lnp", [4, 1024])
    wg = P.din("wg", [n_exp, 1024, d_ff])
    wu = P.din("wu", [n_exp, 1024, d_ff])
    wd = P.din("wd", [n_exp, d_ff, 1024])
    if n_exp > 1:
        router = P.din("router", [1024, 8])
    xo = P.dout("xo", [S_tok, 1024])
    TB = 512 if S_tok >= 512 else S_tok
    NT = TB // 128
    wo_sb = P.sb("wo_sb", [128, 8, 1024])
    for k in range(8):
        S.sync.dma_start(out=wo_sb[:, k, :], in_=w_out[k * 128:(k + 1) * 128, :])
    g0 = P.bc_row("g0", lnp[0:1, :], 1024)
    b0 = P.bc_row("b0", lnp[1:2, :], 1024)
    g1 = P.bc_row("g1", lnp[2:3, :], 1024)
    b1 = P.bc_row("b1", lnp[3:4, :], 1024)
    if n_exp > 1:
        r_sb = P.sb("r_sb", [128, 8, 8])
        for k in range(8):
            S.sync.dma_start(out=r_sb[:, k, :], in_=router[k * 128:(k + 1) * 128, :])
    lns = ln_scratch(P, "lns")
    xt = P.rot("xt", [128, 1024])
    yt = P.rot("yt", [128, 1024])
    yT = P.rot("yT", [128, 8, 128])
    x1 = P.sb("x1", [128, NT, 1024])
    x1T = P.sb("x1T", [128, 8, TB])
    facc = P.sb("facc", [128, NT, 1024])
    comb = P.sb("comb", [128, NT, 8])
    FC = 256
    nfc = d_ff // FC
    wg_sb = P.rot("wg_sb", [128, 8, FC])
    wu_sb = P.rot("wu_sb", [128, 8, FC])
    wd_sb = P.rot("wd_sb", [128, FC // 128, 1024])
    hT = P.rot("hT", [128, FC // 128, TB])
    sg = P.rot("sg", [128, TB])
    pst = [S.ps(f"pst{i}", [128, 512]) for i in range(2)]
    pg = [S.ps(f"pg{i}", [128, 512]) for i in range(2)]
    pu = [S.ps(f"pu{i}", [128, 512]) for i in range(2)]
    pd = [S.ps(f"pd{i}", [128, 512]) for i in range(2)]
    sm = {k: P.sb(f"sm_{k}", [128, 8]) for k in ("lg", "m1", "mk1", "l2", "m2", "mk2", "d", "g1", "g2", "t")}
    nblk = S_tok // TB
    it = 0
    for blk in range(nblk):
        for tt in range(NT):
            t0 = blk * TB + tt * 128
            xs, ys = xt[tt % 2], yt[tt % 2]
            S.sync.dma_start(out=xs[:], in_=x[t0:t0 + 128, :])
            S.sync.dma_start(out=ys[:, 0:512], in_=ya[t0:t0 + 128, :])
            S.sync.dma_start(out=ys[:, 512:1024], in_=yb[t0:t0 + 128, :])
            for k in range(8):
                P.transpose(yT[tt % 2][:, k, :], ys[:, k * 128:(k + 1) * 128], pst[k % 2][:, 0:128],
                            eng="vector" if k % 2 == 0 else "scalar")
            for half in range(2):
                ps = pd[half]
                for k in range(8):
                    S.tensor.matmul(ps[:], lhsT=yT[tt % 2][:, k, :], rhs=wo_sb[:, k, half * 512:(half + 1) * 512],
                                    start=(k == 0), stop=(k == 7))
                S.vector.scalar_tensor_tensor(out=xs[:, half * 512:(half + 1) * 512], in0=xs[:, half * 512:(half + 1) * 512],
                                              scalar=DN_ALPHA, in1=ps[:], op0=ALU.mult, op1=ALU.add)
            layernorm(P, xs[:], x1[:, tt, :], g0[:], b0[:], lns)
            for k in range(8):
                P.transpose(x1T[:, k, tt * 128:(tt + 1) * 128], x1[:, tt, k * 128:(k + 1) * 128], pst[k % 2][:, 0:128],
                            eng="vector" if k % 2 == 0 else "scalar")
            if n_exp > 1:
                ps = pd[0]
                for k in range(8):
                    S.tensor.matmul(ps[:, 0:8], lhsT=x1T[:, k, tt * 128:(tt + 1) * 128], rhs=r_sb[:, k, :],
                                    start=(k == 0), stop=(k == 7))
                S.vector.tensor_copy(out=sm["lg"][:], in_=ps[:, 0:8])
                S.vector.reduce_max(out=sm["m1"][:, 0:1], in_=sm["lg"][:], axis=AX.X)
                S.vector.tensor_scalar(out=sm["mk1"][:], in0=sm["lg"][:], scalar1=sm["m1"][:, 0:1], scalar2=None, op0=ALU.is_ge)
                S.vector.scalar_tensor_tensor(out=sm["l2"][:], in0=sm["mk1"][:], scalar=-1e30, in1=sm["lg"][:], op0=ALU.mult, op1=ALU.add)
                S.vector.reduce_max(out=sm["m2"][:, 0:1], in_=sm["l2"][:], axis=AX.X)
                S.vector.tensor_scalar(out=sm["mk2"][:], in0=sm["l2"][:], scalar1=sm["m2"][:, 0:1], scalar2=None, op0=ALU.is_ge)
                S.vector.tensor_tensor(out=sm["d"][:, 0:1], in0=sm["m2"][:, 0:1], in1=sm["m1"][:, 0:1], op=ALU.subtract)
                S.scalar.activation(out=sm["t"][:, 0:1], in_=sm["d"][:, 0:1], func=AF.Exp)
                S.vector.tensor_scalar(out=sm["t"][:, 0:1], in0=sm["t"][:, 0:1], scalar1=1.0, scalar2=None, op0=ALU.add)
                S.vector.reciprocal(out=sm["g1"][:, 0:1], in_=sm["t"][:, 0:1])
                S.vector.tensor_scalar(out=sm["g2"][:, 0:1], in0=sm["g1"][:, 0:1], scalar1=-1.0, scalar2=1.0, op0=ALU.mult, op1=ALU.add)
                S.vector.tensor_scalar(out=comb[:, tt, :], in0=sm["mk1"][:], scalar1=sm["g1"][:, 0:1], scalar2=None, op0=ALU.mult)
                S.vector.scalar_tensor_tensor(out=comb[:, tt, :], in0=sm["mk2"][:], scalar=sm["g2"][:, 0:1], in1=comb[:, tt, :],
                                              op0=ALU.mult, op1=ALU.add)
        first = True
        for e in range(n_exp):
            for fc in range(nfc):
                b = it % 2
                it += 1
                f0 = fc * FC
                S.sync.dma_start(out=wg_sb[b][:], in_=wg[e, :, f0:f0 + FC].rearrange("(k p) f -> p k f", p=128))
                S.sync.dma_start(out=wu_sb[b][:], in_=wu[e, :, f0:f0 + FC].rearrange("(k p) f -> p k f", p=128))
                S.sync.dma_start(out=wd_sb[b][:], in_=wd[e, f0:f0 + FC, :].rearrange("(k p) f -> p k f", p=128))
                for ft in range(FC // 128):
                    for k in range(8):
                        S.tensor.matmul(pg[ft % 2][:, 0:TB], lhsT=wg_sb[b][:, k, ft * 128:(ft + 1) * 128], rhs=x1T[:, k, :],
                                        start=(k == 0), stop=(k == 7))
                    for k in range(8):
                        S.tensor.matmul(pu[ft % 2][:, 0:TB], lhsT=wu_sb[b][:, k, ft * 128:(ft + 1) * 128], rhs=x1T[:, k, :],
                                        start=(k == 0), stop=(k == 7))
                    S.scalar.activation(out=sg[ft % 2][:], in_=pg[ft % 2][:, 0:TB], func=AF.Silu)
                    S.vector.tensor_tensor(out=hT[b][:, ft, :], in0=sg[ft % 2][:], in1=pu[ft % 2][:, 0:TB], op=ALU.mult)
                for tt in range(NT):
                    for half in range(2):
                        ps = pd[(tt * 2 + half) % 2]
                        for ft in range(FC // 128):
                            S.tensor.matmul(ps[:], lhsT=hT[b][:, ft, tt * 128:(tt + 1) * 128],
                                            rhs=wd_sb[b][:, ft, half * 512:(half + 1) * 512],
                                            start=(ft == 0), stop=(ft == FC // 128 - 1))
                        fa = facc[:, tt, half * 512:(half + 1) * 512]
                        if n_exp > 1:
                            if first:
                                S.vector.tensor_scalar(out=fa, in0=ps[:], scalar1=comb[:, tt, e:e + 1], scalar2=None, op0=ALU.mult)
                            else:
                                S.vector.scalar_tensor_tensor(out=fa, in0=ps[:], scalar=comb[:, tt, e:e + 1], in1=fa,
                                                              op0=ALU.mult, op1=ALU.add)
                        else:
                            if first:
                                S.vector.tensor_copy(out=fa, in_=ps[:])
                            else:
                                S.vector.tensor_tensor(out=fa, in0=fa, in1=ps[:], op=ALU.add)
                first = False
        for tt in range(NT):
            t0 = blk * TB + tt * 128
            S.vector.scalar_tensor_tensor(out=facc[:, tt, :], in0=x1[:, tt, :], scalar=DN_ALPHA, in1=facc[:, tt, :],
                                          op0=ALU.mult, op1=ALU.add)
            layernorm(P, facc[:, tt, :], x1[:, tt, :], g1[:], b1[:], lns)
            S.gpsimd.dma_start(out=xo[t0:t0 + 128, :], in_=x1[:, tt, :])
    S.emit()
    return P.nc
def build_mlstm(S_tok, P=None):
    P = P or Prog()
    S = P.S
    z = P.din("z", [S_tok, 2056])
    gb = P.din("gb", [1, 8])
    ng = P.din("ng", [1, 512])
    y = P.dout("y", [S_tok, 512])
    gbb = P.bc_row("gbb", gb, 8)
    ngb = P.bc_row("ngb", ng, 512)
    Caug = P.sb("Caug", [64, 4, 129])
    S.vector.memset(Caug[:], 0.0)
    zt = P.rot("zt", [128, 1544])
    vaug = P.rot("vaug", [128, 4, 129])
    for b in range(2):
        S.vector.memset(vaug[b][:], 1.0)
    yt = P.rot("yt", [128, 512])
    sm = {k: P.sb(f"m_{k}", [128, 8]) for k in ("ig", "gf", "e", "lf", "bb", "eb", "es", "ebt", "tmp", "den", "rec")}
    LFtri = P.rot("LFtri", [128, 128])
    Rm = P.rot("Rm", [128, 128])
    ED = P.rot("ED", [128, 128])
    SDT = P.rot("SDT", [128, 128])
    qT = P.rot("qT", [64, 128])
    kT = P.rot("kT", [64, 128])
    qs = P.rot("qs", [128, 64])
    qsT = P.rot("qsT", [64, 128])
    ks = P.rot("ks", [128, 64])
    hh = P.rot("hh", [128, 128])
    sg = P.rot("sg", [128, 512])
    lns = ln_scratch(P, "mln", D=128, eps=1e-6)
    pT = [S.ps(f"pT{i}", [128, 512]) for i in range(2)]
    pD = S.ps("pD", [128, 512])
    pS = S.ps("pS", [128, 512])
    pN = S.ps("pN", [128, 512])
    pC = S.ps("pC", [128, 512])
    pB = S.ps("pB", [128, 512])
    ident, tri, ones, negT = P.C("ident"), P.C("tri"), P.C("ones"), P.C("negT")
    for t in range(S_tok // 128):
        t0 = t * 128
        Z = zt[t % 2]
        S.sync.dma_start(out=Z[:], in_=z[t0:t0 + 128, 512:2056])
        q, k, v, o = Z[:, 0:256], Z[:, 256:512], Z[:, 512:1024], Z[:, 1024:1536]
        S.vector.tensor_tensor(out=sm["ig"][:, 0:4], in0=Z[:, 1536:1540], in1=gbb[:, 0:4], op=ALU.add)
        S.vector.tensor_tensor(out=sm["gf"][:, 0:4], in0=Z[:, 1540:1544], in1=gbb[:, 4:8], op=ALU.add)
        S.scalar.activation(out=sm["e"][:, 0:4], in_=sm["gf"][:, 0:4], func=AF.Exp, scale=-1.0)
        S.vector.tensor_scalar(out=sm["e"][:, 0:4], in0=sm["e"][:, 0:4], scalar1=1.0, scalar2=None, op0=ALU.add)
        S.scalar.activation(out=sm["lf"][:, 0:4], in_=sm["e"][:, 0:4], func=AF.Ln)
        S.vector.tensor_scalar(out=sm["lf"][:, 0:4], in0=sm["lf"][:, 0:4], scalar1=-1.0, scalar2=None, op0=ALU.mult)
        S.tensor.matmul(pB[:, 0:4], lhsT=tri, rhs=sm["lf"][:, 0:4], start=True, stop=True)
        S.tensor.matmul(pB[:, 4:8], lhsT=ones, rhs=sm["lf"][:, 0:4], start=True, stop=True)
        S.vector.tensor_copy(out=sm["bb"][:], in_=pB[:, 0:8])
        S.scalar.activation(out=sm["eb"][:, 0:4], in_=sm["bb"][:, 0:4], func=AF.Exp)
        S.scalar.activation(out=sm["ebt"][:, 0:4], in_=sm["bb"][:, 4:8], func=AF.Exp)
        S.vector.tensor_tensor(out=sm["tmp"][:, 0:4], in0=sm["bb"][:, 4:8], in1=sm["bb"][:, 0:4], op=ALU.subtract)
        S.vector.tensor_tensor(out=sm["tmp"][:, 0:4], in0=sm["tmp"][:, 0:4], in1=sm["ig"][:, 0:4], op=ALU.add)
        S.scalar.activation(out=sm["es"][:, 0:4], in_=sm["tmp"][:, 0:4], func=AF.Exp)
        VA = vaug[t % 2]
        S.vector.tensor_copy(out=VA[:, :, 0:128], in_=v.rearrange("p (h d) -> p h d", h=4))
        S.scalar.activation(out=sg[t % 2][:], in_=o, func=AF.Sigmoid)
        Y = yt[t % 2]
        for h in range(4):
            u = (t * 4 + h) % 2
            S.vector.tensor_scalar(out=LFtri[u][:], in0=tri, scalar1=sm["lf"][:, h:h + 1], scalar2=None, op0=ALU.mult)
            S.vector.scalar_tensor_tensor(out=Rm[u][:], in0=ident, scalar=sm["ig"][:, h:h + 1], in1=LFtri[u][:],
                                          op0=ALU.mult, op1=ALU.subtract)
            S.tensor.matmul(pD[:, 0:128], lhsT=ones, rhs=LFtri[u][:], start=True, stop=False)
            S.tensor.matmul(pD[:, 0:128], lhsT=Rm[u][:], rhs=ones, start=False, stop=False)
            S.tensor.matmul(pD[:, 0:128], lhsT=ident, rhs=negT, start=False, stop=True)
            S.scalar.activation(out=ED[u][:], in_=pD[:, 0:128], func=AF.Exp)
            qh, kh = q[:, h * 64:(h + 1) * 64], k[:, h * 64:(h + 1) * 64]
            P.transpose(qT[u][:], qh, pT[0][0:64, 0:128], rows=128, cols=64)
            P.transpose(kT[u][:], kh, pT[1][0:64, 0:128], rows=128, cols=64, eng="scalar")
            S.vector.tensor_scalar(out=qs[u][:], in0=qh, scalar1=sm["eb"][:, h:h + 1], scalar2=0.125, op0=ALU.mult, op1=ALU.mult)
            P.transpose(qsT[u][:], qs[u][:], pT[0][0:64, 0:128], rows=128, cols=64)
            S.tensor.matmul(pS[:, 0:128], lhsT=kT[u][:], rhs=qT[u][:], start=True, stop=True)
            S.vector.scalar_tensor_tensor(out=SDT[u][:], in0=pS[:, 0:128], scalar=0.125, in1=ED[u][:], op0=ALU.mult, op1=ALU.mult)
            S.tensor.matmul(pN[:, 0:129], lhsT=SDT[u][:], rhs=VA[:, h, :], start=True, stop=False)
            S.tensor.matmul(pN[:, 0:129], lhsT=qsT[u][:], rhs=Caug[:, h, :], start=False, stop=True)
            S.scalar.activation(out=sm["den"][:, 0:1], in_=pN[:, 128:129], func=AF.Abs)
            S.vector.tensor_scalar_max(out=sm["den"][:, 0:1], in0=sm["den"][:, 0:1], scalar1=1.0)
            S.vector.reciprocal(out=sm["rec"][:, 0:1], in_=sm["den"][:, 0:1])
            S.vector.tensor_scalar(out=hh[u][:], in0=pN[:, 0:128], scalar1=sm["rec"][:, 0:1], scalar2=None, op0=ALU.mult)
            S.vector.tensor_scalar(out=ks[u][:], in0=kh, scalar1=sm["es"][:, h:h + 1], scalar2=None, op0=ALU.mult)
            S.tensor.matmul(pC[0:64, 0:129], lhsT=ks[u][:], rhs=VA[:, h, :], start=True, stop=True)
            S.vector.scalar_tensor_tensor(out=Caug[:, h, :], in0=Caug[:, h, :], scalar=sm["ebt"][0:64, h:h + 1], in1=pC[0:64, 0:129],
                                          op0=ALU.mult, op1=ALU.add)
            layernorm(P, hh[u][:], Y[:, h * 128:(h + 1) * 128], None, None, lns, D=128, eps=1e-6)
        S.vector.tensor_tensor(out=Y[:], in0=Y[:], in1=ngb[:], op=ALU.mult)
        S.vector.tensor_tensor(out=Y[:], in0=Y[:], in1=sg[t % 2][:], op=ALU.mult)
        S.gpsimd.dma_start(out=y[t0:t0 + 128, :], in_=Y[:])
    S.emit()
    return P.nc
class _Stop(Exception):
    pass


def build_gdn(S_tok, stop=99, P=None):
    try:
        return _build_gdn(S_tok, stop, P)
    except _Stop as e:
        P = e.args[0]
        P.S.emit()
        return P.nc


def _build_gdn(S_tok, stop, P=None):
    P = P or Prog()
    S = P.S
    z = P.din("z", [S_tok, 3848])
    conv_w = P.din("conv_w", [4, 1536])
    a_log = P.din("a_log", [1, 4])
    dt_bias = P.din("dt_bias", [1, 4])
    norm_g = P.din("norm_g", [1, 128])
    y = P.dout("y", [S_tok, 512])
    cw = [P.bc_row(f"cw{i}", conv_w[i:i + 1, :], 1536) for i in range(4)]
    alb = P.bc_row("alb", a_log, 4)
    dtb = P.bc_row("dtb", dt_bias, 4)
    ngb = P.bc_row("ngb", norm_g, 128)
    ea = P.sb("ea", [128, 4])
    S.scalar.activation(out=ea[:], in_=alb[:], func=AF.Exp)
    state = P.sb("state", [128, 4, 128])
    S.vector.memset(state[:], 0.0)
    xs = [P.rot(f"xs{k}", [128, 1536]) for k in range(4)]
    gt = P.rot("gt", [128, 520])
    acc = P.rot("acc", [128, 1536])
    tmp = P.rot("ctmp", [128, 1536])
    sq = P.sb("sq", [128, 1024])
    yt = P.rot("yt", [128, 512])
    sgt = P.rot("sgt", [128, 512])
    sm = {k: P.sb(f"g_{k}", [128, 8]) for k in ("ss", "rn", "beta", "sp", "g", "dd", "edec", "ek", "edl", "t", "ss2", "rs")}
    eps6 = P.sb("eps6", [128, 1])
    S.vector.memset(eps6[:], 1e-6)
    names = ["Gtri", "nGtri", "GT", "GTs", "G", "Gs", "kb", "kT", "kbT", "qn", "qT", "kn", "rhs_v", "rhs_k", "nwT", "u", "QKT",
             "qd", "qdT", "kd", "o", "osq"]
    B = {n: P.rot(n, [128, 128]) for n in names}
    X = P.rot("X", [128, 128], 2)
    Y = P.rot("Y", [128, 128], 2)
    Pm = P.rot("Pm", [128, 128], 2)
    pT = [S.ps(f"pT{i}", [128, 512]) for i in range(2)]
    pG = S.ps("pG", [128, 512])
    pA = S.ps("pA", [128, 512])
    pI = [S.ps(f"pI{i}", [128, 512]) for i in range(2)]
    pU = S.ps("pU", [128, 512])
    pS = S.ps("pS", [128, 512])
    ident, tri, ones, negT, neg = P.C("ident"), P.C("tri"), P.C("ones"), P.C("negT"), P.C("neg")
    mT_s, m_s = P.C("mT_strict"), P.C("m_strict")
    for t in range(S_tok // 128):
        t0 = t * 128
        b = t % 2
        for k in range(4):
            if t0 - k < 0:
                S.vector.memset(xs[k][b][0:32, :], 0.0)
                S.sync.dma_start(out=xs[k][b][k:128, :], in_=z[0:128 - k, 1792:3328])
            else:
                S.sync.dma_start(out=xs[k][b][:], in_=z[t0 - k:t0 - k + 128, 1792:3328])
        G = gt[b]
        S.sync.dma_start(out=G[:], in_=z[t0:t0 + 128, 3328:3848])
        S.vector.tensor_tensor(out=acc[b][:], in0=xs[0][b][:], in1=cw[3][:], op=ALU.mult)
        for k in (1, 2, 3):
            S.vector.tensor_tensor(out=tmp[b][:], in0=xs[k][b][:], in1=cw[3 - k][:], op=ALU.mult)
            S.vector.tensor_tensor(out=acc[b][:], in0=acc[b][:], in1=tmp[b][:], op=ALU.add)
        A = acc[b]
        if stop == 1:
            raise _Stop(P)
        S.scalar.activation(out=A[:], in_=A[:], func=AF.Silu)
        S.scalar.activation(out=sq[:], in_=A[:, 0:1024], func=AF.Square)
        S.vector.reduce_sum(out=sm["ss"][:, 0:8], in_=sq[:].rearrange("p (h d) -> p h d", h=8), axis=AX.X)
        S.scalar.activation(out=sm["ss"][:, 0:8], in_=sm["ss"][:, 0:8], func=AF.Sqrt, bias=eps6[:, 0:1], scale=1.0)
        S.vector.reciprocal(out=sm["rn"][:, 0:8], in_=sm["ss"][:, 0:8])
        S.scalar.activation(out=sm["beta"][:, 0:4], in_=G[:, 512:516], func=AF.Sigmoid)
        S.vector.tensor_tensor(out=sm["sp"][:, 0:4], in0=G[:, 516:520], in1=dtb[:], op=ALU.add)
        S.scalar.activation(out=sm["sp"][:, 0:4], in_=sm["sp"][:, 0:4], func=AF.Exp)
        S.vector.tensor_scalar(out=sm["sp"][:, 0:4], in0=sm["sp"][:, 0:4], scalar1=1.0, scalar2=None, op0=ALU.add)
        S.scalar.activation(out=sm["sp"][:, 0:4], in_=sm["sp"][:, 0:4], func=AF.Ln)
        S.vector.scalar_tensor_tensor(out=sm["g"][:, 0:4], in0=sm["sp"][:, 0:4], scalar=-1.0, in1=ea[:], op0=ALU.mult, op1=ALU.mult)
        S.tensor.matmul(pS[:, 0:4], lhsT=tri, rhs=sm["g"][:, 0:4], start=True, stop=True)
        S.tensor.matmul(pS[:, 4:8], lhsT=ones, rhs=sm["g"][:, 0:4], start=True, stop=True)
        S.vector.tensor_copy(out=sm["dd"][:], in_=pS[:, 0:8])
        S.scalar.activation(out=sm["edec"][:, 0:4], in_=sm["dd"][:, 0:4], func=AF.Exp)
        S.scalar.activation(out=sm["edl"][:, 0:4], in_=sm["dd"][:, 4:8], func=AF.Exp)
        S.vector.tensor_tensor(out=sm["t"][:, 0:4], in0=sm["dd"][:, 4:8], in1=sm["dd"][:, 0:4], op=ALU.subtract)
        S.scalar.activation(out=sm["ek"][:, 0:4], in_=sm["t"][:, 0:4], func=AF.Exp)
        S.scalar.activation(out=sgt[b][:], in_=G[:, 0:512], func=AF.Silu)
        if stop == 2:
            raise _Stop(P)
        Yt = yt[b]
        for h in range(4):
            u = (t * 4 + h) % 2
            T_ = {n: B[n][u] for n in names}
            qh, kh, vh = A[:, h * 128:(h + 1) * 128], A[:, 512 + h * 128:512 + (h + 1) * 128], A[:, 1024 + h * 128:1024 + (h + 1) * 128]
            S.vector.tensor_scalar(out=T_["Gtri"][:], in0=tri, scalar1=sm["g"][:, h:h + 1], scalar2=None, op0=ALU.mult)
            S.vector.tensor_scalar(out=T_["nGtri"][:], in0=T_["Gtri"][:], scalar1=-1.0, scalar2=None, op0=ALU.mult)
            S.tensor.matmul(pG[:, 0:128], lhsT=ones, rhs=T_["Gtri"][:], start=True, stop=False)
            S.tensor.matmul(pG[:, 0:128], lhsT=T_["nGtri"][:], rhs=ones, start=False, stop=False)
            S.tensor.matmul(pG[:, 0:128], lhsT=ident, rhs=negT, start=False, stop=True)
            S.tensor.matmul(pG[:, 128:256], lhsT=T_["Gtri"][:], rhs=ones, start=True, stop=False)
            S.tensor.matmul(pG[:, 128:256], lhsT=ones, rhs=T_["nGtri"][:], start=False, stop=False)
            S.tensor.matmul(pG[:, 128:256], lhsT=ident, rhs=neg, start=False, stop=True)
            S.scalar.activation(out=T_["GT"][:], in_=pG[:, 0:128], func=AF.Exp)
            S.scalar.activation(out=T_["G"][:], in_=pG[:, 128:256], func=AF.Exp)
            S.vector.tensor_tensor(out=T_["GTs"][:], in0=T_["GT"][:], in1=mT_s, op=ALU.mult)
            S.vector.tensor_tensor(out=T_["Gs"][:], in0=T_["G"][:], in1=m_s, op=ALU.mult)
            if stop == 3:
                raise _Stop(P)
            S.vector.tensor_scalar(out=T_["qn"][:], in0=qh, scalar1=sm["rn"][:, h:h + 1], scalar2=128 ** -0.5, op0=ALU.mult, op1=ALU.mult)
            S.vector.tensor_scalar(out=T_["kn"][:], in0=kh, scalar1=sm["rn"][:, 4 + h:5 + h], scalar2=None, op0=ALU.mult)
            S.vector.tensor_scalar(out=T_["kb"][:], in0=T_["kn"][:], scalar1=sm["beta"][:, h:h + 1], scalar2=None, op0=ALU.mult)
            P.transpose(T_["kT"][:], T_["kn"][:], pT[0][:, 0:128])
            P.transpose(T_["kbT"][:], T_["kb"][:], pT[1][:, 0:128], eng="scalar")
            P.transpose(T_["qT"][:], T_["qn"][:], pT[0][:, 0:128])
            S.vector.tensor_scalar(out=T_["qd"][:], in0=T_["qn"][:], scalar1=sm["edec"][:, h:h + 1], scalar2=None, op0=ALU.mult)
            P.transpose(T_["qdT"][:], T_["qd"][:], pT[1][:, 0:128], eng="scalar")
            S.vector.tensor_scalar(out=T_["kd"][:], in0=T_["kn"][:], scalar1=sm["ek"][:, h:h + 1], scalar2=None, op0=ALU.mult)
            S.vector.tensor_scalar(out=T_["rhs_v"][:], in0=vh, scalar1=sm["beta"][:, h:h + 1], scalar2=None, op0=ALU.mult)
            S.vector.tensor_scalar(out=T_["rhs_k"][:], in0=T_["kb"][:], scalar1=sm["edec"][:, h:h + 1], scalar2=None, op0=ALU.mult)
            if stop == 4:
                raise _Stop(P)
            S.tensor.matmul(pA[:, 0:128], lhsT=T_["kT"][:], rhs=T_["kbT"][:], start=True, stop=True)
            S.tensor.matmul(pA[:, 128:256], lhsT=T_["kbT"][:], rhs=T_["kT"][:], start=True, stop=True)
            S.tensor.matmul(pA[:, 256:384], lhsT=T_["kT"][:], rhs=T_["qT"][:], start=True, stop=True)
            S.vector.scalar_tensor_tensor(out=X[0][:], in0=pA[:, 0:128], scalar=-1.0, in1=T_["GTs"][:], op0=ALU.mult, op1=ALU.mult)
            S.vector.scalar_tensor_tensor(out=Y[0][:], in0=pA[:, 128:256], scalar=-1.0, in1=T_["Gs"][:], op0=ALU.mult, op1=ALU.mult)
            S.vector.tensor_tensor(out=T_["QKT"][:], in0=pA[:, 256:384], in1=T_["GT"][:], op=ALU.mult)
            S.vector.tensor_tensor(out=Pm[0][:], in0=X[0][:], in1=ident, op=ALU.add)
            if stop == 5:
                raise _Stop(P)
            nst = 6
            for n in range(nst):
                a, c = n % 2, (n + 1) % 2
                if n < nst - 1:
                    S.tensor.matmul(pI[0][:, 0:128], lhsT=Y[a][:], rhs=X[a][:], start=True, stop=True)
                S.tensor.matmul(pI[0][:, 128:256], lhsT=X[a][:], rhs=Y[a][:], start=True, stop=True)
                if n < nst - 1:
                    S.scalar.copy(out=X[c][:], in_=pI[0][:, 0:128])
                S.vector.tensor_copy(out=Y[c][:], in_=pI[0][:, 128:256])
                S.tensor.matmul(pI[1][:, 0:128], lhsT=Y[c][:], rhs=Pm[a][:], start=True, stop=True)
                S.vector.tensor_tensor(out=Pm[c][:], in0=Pm[a][:], in1=pI[1][:, 0:128], op=ALU.add)
            TT = Pm[nst % 2]
            if stop == 6:
                raise _Stop(P)
            S.tensor.matmul(pS[:, 128:256], lhsT=T_["rhs_k"][:], rhs=TT[:], start=True, stop=True)
            S.scalar.mul(out=T_["nwT"][:], in_=pS[:, 128:256], mul=-1.0)
            S.tensor.matmul(pU[:, 0:128], lhsT=TT[:], rhs=T_["rhs_v"][:], start=True, stop=False)
            S.tensor.matmul(pU[:, 0:128], lhsT=T_["nwT"][:], rhs=state[:, h, :], start=False, stop=True)
            S.vector.tensor_copy(out=T_["u"][:], in_=pU[:, 0:128])
            S.tensor.matmul(pU[:, 128:256], lhsT=T_["qdT"][:], rhs=state[:, h, :], start=True, stop=False)
            S.tensor.matmul(pU[:, 128:256], lhsT=T_["QKT"][:], rhs=T_["u"][:], start=False, stop=True)
            S.tensor.matmul(pS[:, 256:384], lhsT=T_["kd"][:], rhs=T_["u"][:], start=True, stop=True)
            S.scalar.copy(out=T_["o"][:], in_=pU[:, 128:256])
            S.vector.scalar_tensor_tensor(out=state[:, h, :], in0=state[:, h, :], scalar=sm["edl"][:, h:h + 1], in1=pS[:, 256:384],
                                          op0=ALU.mult, op1=ALU.add)
            if stop == 7:
                raise _Stop(P)
            S.scalar.activation(out=T_["osq"][:], in_=T_["o"][:], func=AF.Square, accum_out=sm["ss2"][:, 0:1])
            S.scalar.activation(out=sm["rs"][:, 0:1], in_=sm["ss2"][:, 0:1], func=AF.Sqrt, bias=eps6[:, 0:1], scale=1.0 / 128)
            S.vector.reciprocal(out=sm["rs"][:, 0:1], in_=sm["rs"][:, 0:1])
            S.vector.scalar_tensor_tensor(out=Yt[:, h * 128:(h + 1) * 128], in0=T_["o"][:], scalar=sm["rs"][:, 0:1], in1=ngb[:],
                                          op0=ALU.mult, op1=ALU.mult)
        S.vector.tensor_tensor(out=Yt[:], in0=Yt[:], in1=sgt[b][:], op=ALU.mult)
        S.gpsimd.dma_start(out=y[t0:t0 + 128, :], in_=Yt[:])
    S.emit()
    return P.nc
def build_rwkv(S_tok, P=None):
    P = P or Prog()
    S = P.S
    z = P.din("z", [S_tok, 3848])
    vecs = P.din("vecs", [8, 512])
    mu = P.din("mu", [1, 1792])
    w_up = P.din("w_up", [64, 512])
    a_up = P.din("a_up", [64, 512])
    g_up = P.din("g_up", [128, 512])
    y = P.dout("y", [S_tok, 512])
    mub = P.bc_row("mub", mu, 1792)
    vb = [P.bc_row(f"vb{i}", vecs[i:i + 1, :], 512) for i in range(7)]
    w0b, a0b, kkb, kab, rkb, lgb, lbb = vb
    wup = P.sb("wup", [64, 512]); aup = P.sb("aup", [64, 512]); gup = P.sb("gup", [128, 512])
    S.sync.dma_start(out=wup[:], in_=w_up); S.sync.dma_start(out=aup[:], in_=a_up); S.sync.dma_start(out=gup[:], in_=g_up)
    H = P.sb("H", [64, 8, 64])
    S.vector.memset(H[:], 0.0)
    L = 64
    cur = P.rot("cur", [L, 1792]); prev = P.rot("prev", [L, 1792])
    big = {n: P.sb(f"r_{n}", [L, 512]) for n in ("logw", "a", "g", "kk", "kp", "b", "ecl", "ecm", "eni", "rt", "kat", "kt", "bt", "nbt",
                                                   "tmp", "Y", "Yn", "sq")}
    sm = {n: P.sb(f"rs_{n}", [L, 8]) for n in ("ss", "rn", "bs")}
    tw = P.sb("tw", [L, 64]); twT = P.sb("twT", [64, L]); adT = P.sb("adT", [64, L])
    sgd = P.sb("sgd", [L, 128]); sgdT = P.sb("sgdT", [128, L])
    PL = P.sb("PL", [64, 8])
    eps6 = P.sb("eps6", [128, 1])
    S.vector.memset(eps6[:], 1e-6)
    TN = ["rtT", "katT", "ktT", "btT"]
    TT_ = {n: P.rot(n, [64, L]) for n in TN}
    MN = ["AkvT", "BrkT", "nBrbT"]
    M_ = {n: P.rot(n, [L, L]) for n in MN}
    X = P.rot("X", [L, L]); Yq = P.rot("Yq", [L, L]); Pm = P.rot("Pm", [L, L])
    Wsb = P.rot("Wsb", [L, 64]); Usb = P.rot("Usb", [L, 64])
    lns = ln_scratch(P, "rln", D=64, eps=64e-5)
    pT = [S.ps(f"pT{i}", [128, 512]) for i in range(2)]
    pL = S.ps("pL", [128, 512])
    pA = [S.ps(f"pA{i}", [128, 512]) for i in range(2)]
    pI = [S.ps(f"pI{i}", [128, 512]) for i in range(2)]
    pC = S.ps("pC", [128, 512])
    ident, btri, chunkind = P.C("ident", L, L), P.C("tri", L, L), P.C("ones", L, 1)
    bmT_i, bmT_s, bm_s = P.C("tri", L, L), P.C("mT_strict", L, L), P.C("m_strict", L, L)
    B = big
    for t in range(S_tok // L):
        t0 = t * L
        b = t % 2
        Cc, Pp = cur[b], prev[b]
        S.sync.dma_start(out=Cc[:], in_=z[t0:t0 + L, 0:1792])
        if t == 0:
            S.vector.memset(Pp[0:32, :], 0.0)
            S.sync.dma_start(out=Pp[1:L, :], in_=z[0:L - 1, 0:1792])
        else:
            S.sync.dma_start(out=Pp[:], in_=z[t0 - 1:t0 + L - 1, 0:1792])
        S.vector.tensor_tensor(out=Pp[:], in0=Pp[:], in1=Cc[:], op=ALU.subtract)
        S.vector.tensor_tensor(out=Pp[:], in0=Pp[:], in1=mub[0:L, :], op=ALU.mult)
        S.vector.tensor_tensor(out=Cc[:], in0=Cc[:], in1=Pp[:], op=ALU.add)
        r, k, v = Cc[:, 0:512], Cc[:, 512:1024], Cc[:, 1024:1536]
        S.scalar.activation(out=tw[:], in_=Cc[:, 1536:1600], func=AF.Tanh)
        P.transpose(twT[:], tw[:], pT[0][0:64, 0:L], rows=L, cols=64)
        P.transpose(adT[:], Cc[:, 1600:1664], pT[1][0:64, 0:L], rows=L, cols=64, eng="scalar")
        S.scalar.activation(out=sgd[:], in_=Cc[:, 1664:1792], func=AF.Sigmoid)
        P.transpose(sgdT[:], sgd[:], pT[0][:, 0:L], rows=L, cols=128)
        S.tensor.matmul(pL[0:L, :], lhsT=twT[:], rhs=wup[:], start=True, stop=True)
        S.vector.tensor_tensor(out=B["logw"][:], in0=pL[0:L, :], in1=w0b[0:L, :], op=ALU.add)
        S.scalar.activation(out=B["logw"][:], in_=B["logw"][:], func=AF.Sigmoid)
        S.vector.tensor_scalar(out=B["logw"][:], in0=B["logw"][:], scalar1=-0.6065306597126334, scalar2=None, op0=ALU.mult)
        S.tensor.matmul(pL[0:L, :], lhsT=adT[:], rhs=aup[:], start=True, stop=True)
        S.vector.tensor_tensor(out=B["a"][:], in0=pL[0:L, :], in1=a0b[0:L, :], op=ALU.add)
        S.scalar.activation(out=B["a"][:], in_=B["a"][:], func=AF.Sigmoid)
        S.tensor.matmul(pL[0:L, :], lhsT=sgdT[:], rhs=gup[:], start=True, stop=True)
        S.scalar.copy(out=B["g"][:], in_=pL[0:L, :])
        S.vector.tensor_tensor(out=B["kk"][:], in0=k, in1=kkb[0:L, :], op=ALU.mult)
        S.scalar.activation(out=B["sq"][:], in_=B["kk"][:], func=AF.Square)
        S.vector.reduce_sum(out=sm["ss"][:, 0:8], in_=B["sq"][:].rearrange("p (h d) -> p h d", h=8), axis=AX.X)
        S.scalar.activation(out=sm["ss"][:, 0:8], in_=sm["ss"][:, 0:8], func=AF.Sqrt, bias=eps6[0:L, 0:1], scale=1.0)
        S.vector.reciprocal(out=sm["rn"][:, 0:8], in_=sm["ss"][:, 0:8])
        for h in range(8):
            S.vector.tensor_scalar(out=B["kk"][:, h * 64:(h + 1) * 64], in0=B["kk"][:, h * 64:(h + 1) * 64],
                                   scalar1=sm["rn"][:, h:h + 1], scalar2=None, op0=ALU.mult)
        S.vector.scalar_tensor_tensor(out=B["tmp"][:], in0=B["a"][:], scalar=-1.0, in1=kab[0:L, :], op0=ALU.add, op1=ALU.mult)
        S.vector.scalar_tensor_tensor(out=B["kp"][:], in0=B["tmp"][:], scalar=1.0, in1=k, op0=ALU.add, op1=ALU.mult)
        S.vector.tensor_tensor(out=B["b"][:], in0=B["kk"][:], in1=B["a"][:], op=ALU.mult)
        S.tensor.matmul(pL[0:L, :], lhsT=btri, rhs=B["logw"][:], start=True, stop=True)
        S.scalar.activation(out=B["ecl"][:], in_=pL[0:L, :], func=AF.Exp)
        S.scalar.activation(out=B["eni"][:], in_=pL[0:L, :], func=AF.Exp, scale=-1.0)
        S.vector.tensor_tensor(out=B["tmp"][:], in0=pL[0:L, :], in1=B["logw"][:], op=ALU.subtract)
        S.scalar.activation(out=B["ecm"][:], in_=B["tmp"][:], func=AF.Exp)
        S.vector.tensor_tensor(out=B["rt"][:], in0=r, in1=B["ecl"][:], op=ALU.mult)
        S.vector.tensor_tensor(out=B["kat"][:], in0=B["kk"][:], in1=B["ecm"][:], op=ALU.mult)
        S.vector.tensor_tensor(out=B["kt"][:], in0=B["kp"][:], in1=B["eni"][:], op=ALU.mult)
        S.vector.tensor_tensor(out=B["bt"][:], in0=B["b"][:], in1=B["eni"][:], op=ALU.mult)
        S.vector.tensor_scalar(out=B["nbt"][:], in0=B["bt"][:], scalar1=-1.0, scalar2=None, op0=ALU.mult)
        for h in range(8):
            S.tensor.matmul(pL[0:64, h:h + 1], lhsT=B["logw"][:, h * 64:(h + 1) * 64], rhs=chunkind, start=True, stop=True)
        S.scalar.activation(out=PL[:], in_=pL[0:64, 0:8], func=AF.Exp)
        S.vector.tensor_tensor(out=B["tmp"][:], in0=r, in1=B["kp"][:], op=ALU.mult)
        S.vector.tensor_tensor(out=B["tmp"][:], in0=B["tmp"][:], in1=rkb[0:L, :], op=ALU.mult)
        S.vector.reduce_sum(out=sm["bs"][:, 0:8], in_=B["tmp"][:].rearrange("p (h d) -> p h d", h=8), axis=AX.X)
        for h in range(8):
            u = h % 2
            hs = slice(h * 64, (h + 1) * 64)
            T_ = {n: TT_[n][u] for n in TN}
            Mh = {n: M_[n][u] for n in MN}
            P.transpose(T_["rtT"][:], B["rt"][:, hs], pT[0][0:64, 0:L], rows=L, cols=64)
            P.transpose(T_["katT"][:], B["kat"][:, hs], pT[1][0:64, 0:L], rows=L, cols=64, eng="scalar")
            P.transpose(T_["ktT"][:], B["kt"][:, hs], pT[0][0:64, 0:L], rows=L, cols=64)
            P.transpose(T_["btT"][:], B["bt"][:, hs], pT[1][0:64, 0:L], rows=L, cols=64, eng="scalar")
            S.tensor.matmul(pA[0][0:L, 0:0+L], lhsT=T_["btT"][:], rhs=T_["katT"][:], start=True, stop=True)
            S.tensor.matmul(pA[0][0:L, 128:128+L], lhsT=T_["katT"][:], rhs=T_["btT"][:], start=True, stop=True)
            S.tensor.matmul(pA[0][0:L, 256:256+L], lhsT=T_["ktT"][:], rhs=T_["katT"][:], start=True, stop=True)
            S.tensor.matmul(pA[1][0:L, 0:0+L], lhsT=T_["ktT"][:], rhs=T_["rtT"][:], start=True, stop=True)
            S.tensor.matmul(pA[1][0:L, 128:128+L], lhsT=T_["btT"][:], rhs=T_["rtT"][:], start=True, stop=True)
            S.vector.scalar_tensor_tensor(out=X[0][:], in0=pA[0][0:L, 0:0+L], scalar=-1.0, in1=bmT_s, op0=ALU.mult, op1=ALU.mult)
            S.vector.scalar_tensor_tensor(out=Yq[0][:], in0=pA[0][0:L, 128:128+L], scalar=-1.0, in1=bm_s, op0=ALU.mult, op1=ALU.mult)
            S.vector.tensor_tensor(out=Mh["AkvT"][:], in0=pA[0][0:L, 256:256+L], in1=bmT_s, op=ALU.mult)
            S.vector.tensor_tensor(out=Mh["BrkT"][:], in0=pA[1][0:L, 0:0+L], in1=bmT_i, op=ALU.mult)
            S.vector.scalar_tensor_tensor(out=Mh["nBrbT"][:], in0=pA[1][0:L, 128:128+L], scalar=-1.0, in1=bmT_i, op0=ALU.mult, op1=ALU.mult)
            S.vector.tensor_tensor(out=Pm[0][:], in0=X[0][:], in1=ident, op=ALU.add)
            nst = 5
            for n in range(nst):
                a_, c_ = n % 2, (n + 1) % 2
                if n < nst - 1:
                    S.tensor.matmul(pI[0][0:L, 0:0+L], lhsT=Yq[a_][:], rhs=X[a_][:], start=True, stop=True)
                S.tensor.matmul(pI[0][0:L, 128:128+L], lhsT=X[a_][:], rhs=Yq[a_][:], start=True, stop=True)
                if n < nst - 1:
                    S.scalar.copy(out=X[c_][:], in_=pI[0][0:L, 0:0+L])
                S.vector.tensor_copy(out=Yq[c_][:], in_=pI[0][0:L, 128:128+L])
                S.tensor.matmul(pI[1][0:L, 0:0+L], lhsT=Yq[c_][:], rhs=Pm[a_][:], start=True, stop=True)
                S.vector.tensor_tensor(out=Pm[c_][:], in0=Pm[a_][:], in1=pI[1][0:L, 0:0+L], op=ALU.add)
            TT = Pm[nst % 2]
            for c in range(1):
                cs = slice(0, L)
                H0 = H[:, h, :]
                S.tensor.matmul(pC[cs, 0:64], lhsT=T_["katT"][:, cs], rhs=H0, start=True, stop=False)
                S.tensor.matmul(pC[cs, 0:64], lhsT=Mh["AkvT"][cs, cs], rhs=v[cs, hs], start=False, stop=True)
                S.vector.tensor_copy(out=Wsb[u][cs, :], in_=pC[cs, 0:64])
                S.tensor.matmul(pC[cs, 64:128], lhsT=TT[cs, cs], rhs=Wsb[u][cs, :], start=True, stop=True)
                S.vector.tensor_copy(out=Usb[u][cs, :], in_=pC[cs, 64:128])
                S.tensor.matmul(pC[cs, 128:192], lhsT=T_["rtT"][:, cs], rhs=H0, start=True, stop=False)
                S.tensor.matmul(pC[cs, 128:192], lhsT=Mh["BrkT"][cs, cs], rhs=v[cs, hs], start=False, stop=False)
                S.tensor.matmul(pC[cs, 128:192], lhsT=Mh["nBrbT"][cs, cs], rhs=Usb[u][cs, :], start=False, stop=True)
                S.scalar.copy(out=B["Y"][cs, hs], in_=pC[cs, 128:192])
                S.tensor.matmul(pC[0:64, 192:256], lhsT=B["kt"][cs, hs], rhs=v[cs, hs], start=True, stop=False)
                S.tensor.matmul(pC[0:64, 192:256], lhsT=B["nbt"][cs, hs], rhs=Usb[u][cs, :], start=False, stop=True)
                S.vector.tensor_tensor(out=H0, in0=H0, in1=pC[0:64, 192:256], op=ALU.add)
                S.vector.tensor_scalar(out=H0, in0=H0, scalar1=PL[:, h:h + 1], scalar2=None, op0=ALU.mult)
            layernorm(P, B["Y"][:, hs], B["Yn"][:, hs], None, None, lns, D=64, eps=64e-5)
        S.vector.tensor_tensor(out=B["Yn"][:], in0=B["Yn"][:], in1=lgb[0:L, :], op=ALU.mult)
        S.vector.tensor_tensor(out=B["Yn"][:], in0=B["Yn"][:], in1=lbb[0:L, :], op=ALU.add)
        for h in range(8):
            hs = slice(h * 64, (h + 1) * 64)
            S.vector.scalar_tensor_tensor(out=B["Yn"][:, hs], in0=v[:, hs], scalar=sm["bs"][:, h:h + 1], in1=B["Yn"][:, hs],
                                          op0=ALU.mult, op1=ALU.add)
        S.vector.tensor_tensor(out=B["Yn"][:], in0=B["Yn"][:], in1=B["g"][:], op=ALU.mult)
        S.gpsimd.dma_start(out=y[t0:t0 + L, :], in_=B["Yn"][:])
    S.emit()
    return P.nc
TWO_PI = 6.283185307179586


def s5_host_layout(lam_re, lam_im, log_dt, b_re, b_im, c_re, c_im, d_skip, w_glu, b_glu):
    G, Pn = 32, 64
    rows = np.stack([lam_re.reshape(-1), lam_im.reshape(-1), np.repeat(log_dt, Pn)]).astype(np.float32)
    cols = np.stack([lam_re.reshape(16, 128).T, lam_im.reshape(16, 128).T, np.repeat(log_dt, Pn).reshape(16, 128).T], 0).astype(np.float32)
    bpad = np.zeros((2, 4, 128, 512), np.float32)
    for ct in range(4):
        for gl in range(8):
            g = 8 * ct + gl
            bpad[0, ct, gl * 16:(gl + 1) * 16, gl * 64:(gl + 1) * 64] = b_re[g].T
            bpad[1, ct, gl * 16:(gl + 1) * 16, gl * 64:(gl + 1) * 64] = b_im[g].T
    cpad = np.zeros((2, 16, 128, 128), np.float32)
    for st in range(16):
        for gg in range(2):
            g = 2 * st + gg
            gl = g % 8
            cpad[0, st, gg * 64:(gg + 1) * 64, gl * 16:(gl + 1) * 16] = c_re[g].T
            cpad[1, st, gg * 64:(gg + 1) * 64, gl * 16:(gl + 1) * 16] = c_im[g].T
    dcol = np.stack([d_skip.reshape(4, 128).T, b_glu.reshape(4, 128).T], 0).astype(np.float32)
    return {"rows": rows, "cols": cols, "bpad": bpad, "cpad": cpad, "dcol": dcol, "w_glu": np.ascontiguousarray(w_glu, np.float32)}


def build_s5(S_tok, P=None):
    P = P or Prog()
    S = P.S
    z = P.din("z", [S_tok, 2056])
    rows = P.din("rows", [3, 2048])
    cols = P.din("cols", [3, 128, 16])
    bpad = P.din("bpad", [2, 4, 128, 512])
    cpad = P.din("cpad", [2, 16, 128, 128])
    dcol = P.din("dcol", [2, 128, 4])
    w_glu = P.din("w_glu", [512, 512])
    y = P.dout("y", [S_tok, 512])
    I32_ = mybir.dt.int32
    R = lambda n: P.sb(n, [128, 2048])
    lr = P.bc_row("lr", rows[0:1, :], 2048)
    li = P.bc_row("li", rows[1:2, :], 2048)
    dt = P.bc_row("dtr", rows[2:3, :], 2048)
    a_r, th_r, s1, s2, s3, s4 = R("a_r"), R("th_r"), R("s1"), R("s2"), R("s3"), R("s4")
    si = P.sb("si", [128, 2048], I32_)
    Ei_re, Ei_im = R("Ei_re"), R("Ei_im")
    iota_col, iota_row = P.C("iota_col", 128, 1), P.C("iota_row")

    def sincos(turns, o_sin, o_cos, n):
        for off, o in ((0.0, o_sin), (0.25, o_cos)):
            if off:
                S.vector.tensor_scalar(out=s4[:, 0:n], in0=turns, scalar1=off, scalar2=None, op0=ALU.add)
                src = s4[:, 0:n]
            else:
                src = turns
            S.vector.tensor_copy(out=si[:, 0:n], in_=src)
            S.vector.tensor_copy(out=o, in_=si[:, 0:n])
            S.vector.tensor_tensor(out=o, in0=src, in1=o, op=ALU.subtract)
            S.scalar.activation(out=o, in_=o, func=AF.Sin, scale=TWO_PI)

    S.scalar.activation(out=dt[:], in_=dt[:], func=AF.Exp)
    S.vector.tensor_tensor(out=a_r[:], in0=lr[:], in1=dt[:], op=ALU.mult)
    S.vector.tensor_tensor(out=th_r[:], in0=li[:], in1=dt[:], op=ALU.mult)
    S.vector.tensor_scalar(out=th_r[:], in0=th_r[:], scalar1=1.0 / TWO_PI, scalar2=None, op0=ALU.mult)
    sincos(th_r[:], s1[:], s2[:], 2048)
    S.scalar.activation(out=s3[:], in_=a_r[:], func=AF.Exp)
    S.vector.tensor_tensor(out=s1[:], in0=s1[:], in1=s3[:], op=ALU.mult)
    S.vector.tensor_tensor(out=s2[:], in0=s2[:], in1=s3[:], op=ALU.mult)
    S.vector.tensor_scalar(out=s2[:], in0=s2[:], scalar1=-1.0, scalar2=None, op0=ALU.add)
    fr, fi = R("fr"), R("fi")
    S.vector.tensor_tensor(out=s3[:], in0=lr[:], in1=lr[:], op=ALU.mult)
    S.vector.tensor_tensor(out=s4[:], in0=li[:], in1=li[:], op=ALU.mult)
    S.vector.tensor_tensor(out=s3[:], in0=s3[:], in1=s4[:], op=ALU.add)
    S.vector.reciprocal(out=s3[:], in_=s3[:])
    S.vector.tensor_tensor(out=fr[:], in0=s2[:], in1=lr[:], op=ALU.mult)
    S.vector.tensor_tensor(out=s4[:], in0=s1[:], in1=li[:], op=ALU.mult)
    S.vector.tensor_tensor(out=fr[:], in0=fr[:], in1=s4[:], op=ALU.add)
    S.vector.tensor_tensor(out=fr[:], in0=fr[:], in1=s3[:], op=ALU.mult)
    S.vector.tensor_tensor(out=fi[:], in0=s1[:], in1=lr[:], op=ALU.mult)
    S.vector.tensor_tensor(out=s4[:], in0=s2[:], in1=li[:], op=ALU.mult)
    S.vector.tensor_tensor(out=fi[:], in0=fi[:], in1=s4[:], op=ALU.subtract)
    S.vector.tensor_tensor(out=fi[:], in0=fi[:], in1=s3[:], op=ALU.mult)
    bp_re, bp_im = P.sb("bp_re", [128, 4, 512]), P.sb("bp_im", [128, 4, 512])
    for ct in range(4):
        S.sync.dma_start(out=bp_re[:, ct, :], in_=bpad[0, ct])
        S.sync.dma_start(out=bp_im[:, ct, :], in_=bpad[1, ct])
    Bb_re, Bb_im = P.sb("Bb_re", [128, 2048]), P.sb("Bb_im", [128, 2048])
    bpr, bpi = bp_re[:].rearrange("p a b -> p (a b)"), bp_im[:].rearrange("p a b -> p (a b)")
    S.vector.tensor_tensor(out=Bb_re[:], in0=fr[:], in1=bpr, op=ALU.mult)
    S.vector.tensor_tensor(out=s4[:], in0=fi[:], in1=bpi, op=ALU.mult)
    S.vector.tensor_tensor(out=Bb_re[:], in0=Bb_re[:], in1=s4[:], op=ALU.subtract)
    S.vector.tensor_tensor(out=Bb_im[:], in0=fr[:], in1=bpi, op=ALU.mult)
    S.vector.tensor_tensor(out=s4[:], in0=fi[:], in1=bpr, op=ALU.mult)
    S.vector.tensor_tensor(out=Bb_im[:], in0=Bb_im[:], in1=s4[:], op=ALU.add)
    S.vector.tensor_scalar(out=s3[:], in0=th_r[:], scalar1=iota_col, scalar2=None, op0=ALU.mult)
    sincos(s3[:], s1[:], s2[:], 2048)
    S.vector.tensor_scalar(out=s3[:], in0=a_r[:], scalar1=iota_col, scalar2=-1.0, op0=ALU.mult, op1=ALU.mult)
    S.scalar.activation(out=s3[:], in_=s3[:], func=AF.Exp)
    S.vector.tensor_tensor(out=Ei_re[:], in0=s2[:], in1=s3[:], op=ALU.mult)
    S.vector.scalar_tensor_tensor(out=Ei_im[:], in0=s1[:], scalar=-1.0, in1=s3[:], op0=ALU.mult, op1=ALU.mult)
    EL_re, EL_im = fr, fi
    S.vector.tensor_scalar(out=s3[:], in0=th_r[:], scalar1=128.0, scalar2=None, op0=ALU.mult)
    sincos(s3[:], s1[:], s2[:], 2048)
    S.scalar.activation(out=s3[:], in_=a_r[:], func=AF.Exp, scale=128.0)
    S.vector.tensor_tensor(out=EL_re[:], in0=s2[:], in1=s3[:], op=ALU.mult)
    S.vector.tensor_tensor(out=EL_im[:], in0=s1[:], in1=s3[:], op=ALU.mult)
    cl = P.sb("cl", [128, 3, 16])
    for i in range(3):
        S.sync.dma_start(out=cl[:, i, :], in_=cols[i])
    S.scalar.activation(out=cl[:, 2, :], in_=cl[:, 2, :], func=AF.Exp)
    S.vector.tensor_tensor(out=cl[:, 0, :], in0=cl[:, 0, :], in1=cl[:, 2, :], op=ALU.mult)
    S.vector.tensor_tensor(out=cl[:, 1, :], in0=cl[:, 1, :], in1=cl[:, 2, :], op=ALU.mult)
    S.vector.tensor_scalar(out=cl[:, 1, :], in0=cl[:, 1, :], scalar1=1.0 / TWO_PI, scalar2=None, op0=ALU.mult)
    E_re, E_im = P.sb("E_re", [128, 16, 128]), P.sb("E_im", [128, 16, 128])
    for st in range(16):
        S.vector.tensor_scalar(out=s3[:, 0:128], in0=iota_row, scalar1=cl[:, 1, st:st + 1], scalar2=None, op0=ALU.mult)
        sincos(s3[:, 0:128], s1[:, 0:128], s2[:, 0:128], 128)
        S.scalar.activation(out=s3[:, 0:128], in_=iota_row, func=AF.Exp, scale=cl[:, 0, st:st + 1])
        S.vector.tensor_tensor(out=E_re[:, st, :], in0=s2[:, 0:128], in1=s3[:, 0:128], op=ALU.mult)
        S.vector.tensor_tensor(out=E_im[:, st, :], in0=s1[:, 0:128], in1=s3[:, 0:128], op=ALU.mult)
    cp_re, cp_im = P.sb("cp_re", [128, 16, 128]), P.sb("cp_im", [128, 16, 128])
    for st in range(16):
        S.sync.dma_start(out=cp_re[:, st, :], in_=cpad[0, st])
        S.sync.dma_start(out=cp_im[:, st, :], in_=cpad[1, st])
    dc = P.sb("dc", [128, 2, 4])
    for i in range(2):
        S.sync.dma_start(out=dc[:, i, :], in_=dcol[i])
    wg = P.sb("wg", [128, 4, 512])
    for k in range(4):
        S.sync.dma_start(out=wg[:, k, :], in_=w_glu[k * 128:(k + 1) * 128, :])
    Xr_re, Xr_im, Xn_re, Xn_im, Xt = lr[0:1, :], li[0:1, :], dt[0:1, :], th_r[0:1, :], bpr[0:1, :]
    S.vector.memset(Xr_re, 0.0)
    S.vector.memset(Xr_im, 0.0)
    ut = P.rot("ut", [128, 512])
    uT = P.rot("uT", [128, 4, 128])
    V_re, V_im = s1, s2
    x_re, nx_im = s3, a_r
    tA, tB = s4[:, 0:512], s4[:, 512:1024]
    gl = P.sb("gl", [128, 4, 128])
    g1, g2 = P.sb("g1", [128, 128]), P.sb("g2", [128, 128])
    yaT = P.sb("yaT", [128, 4, 128])
    yt = ut
    pT = [S.ps(f"pT{i}", [128, 512]) for i in range(2)]
    pB = [S.ps(f"pB{i}", [128, 512]) for i in range(2)]
    pS_ = [S.ps(f"pS{i}", [128, 512]) for i in range(2)]
    pY = S.ps("pY", [128, 512])
    pX = S.ps("pX", [128, 512])
    ident, tri, ones = P.C("ident"), P.C("tri"), P.C("ones")
    for t in range(S_tok // 128):
        t0 = t * 128
        b = t % 2
        S.sync.dma_start(out=ut[b][:], in_=z[t0:t0 + 128, 0:512])
        for k in range(4):
            P.transpose(uT[b][:, k, :], ut[b][:, k * 128:(k + 1) * 128], pT[k % 2][:, 0:128], eng="vector" if k % 2 == 0 else "scalar")
        for ct in range(4):
            cs = slice(ct * 512, (ct + 1) * 512)
            S.tensor.matmul(pB[0][:], lhsT=uT[b][:, ct, :], rhs=Bb_re[:, cs], start=True, stop=True)
            S.tensor.matmul(pB[1][:], lhsT=uT[b][:, ct, :], rhs=Bb_im[:, cs], start=True, stop=True)
            S.vector.tensor_tensor(out=V_re[:, cs], in0=pB[0][:], in1=Ei_re[:, cs], op=ALU.mult)
            S.vector.tensor_tensor(out=tA, in0=pB[1][:], in1=Ei_im[:, cs], op=ALU.mult)
            S.vector.tensor_tensor(out=V_re[:, cs], in0=V_re[:, cs], in1=tA, op=ALU.subtract)
            S.vector.tensor_tensor(out=V_im[:, cs], in0=pB[0][:], in1=Ei_im[:, cs], op=ALU.mult)
            S.vector.tensor_tensor(out=tB, in0=pB[1][:], in1=Ei_re[:, cs], op=ALU.mult)
            S.vector.tensor_tensor(out=V_im[:, cs], in0=V_im[:, cs], in1=tB, op=ALU.add)
        S.vector.tensor_tensor(out=V_re[0:1, :], in0=V_re[0:1, :], in1=Xr_re, op=ALU.add)
        S.vector.tensor_tensor(out=V_im[0:1, :], in0=V_im[0:1, :], in1=Xr_im, op=ALU.add)
        for q in range(4):
            S.tensor.matmul(pX[:, :], lhsT=ones, rhs=V_re[:, q * 512:(q + 1) * 512], start=True, stop=True)
            S.vector.tensor_copy(out=Xn_re[:, q * 512:(q + 1) * 512], in_=pX[0:1, :])
            S.tensor.matmul(pX[:, :], lhsT=ones, rhs=V_im[:, q * 512:(q + 1) * 512], start=True, stop=True)
            S.vector.tensor_copy(out=Xn_im[:, q * 512:(q + 1) * 512], in_=pX[0:1, :])
        S.vector.tensor_tensor(out=Xr_re, in0=Xn_re, in1=EL_re[0:1, :], op=ALU.mult)
        S.vector.tensor_tensor(out=Xt, in0=Xn_im, in1=EL_im[0:1, :], op=ALU.mult)
        S.vector.tensor_tensor(out=Xr_re, in0=Xr_re, in1=Xt, op=ALU.subtract)
        S.vector.tensor_tensor(out=Xr_im, in0=Xn_re, in1=EL_im[0:1, :], op=ALU.mult)
        S.vector.tensor_tensor(out=Xt, in0=Xn_im, in1=EL_re[0:1, :], op=ALU.mult)
        S.vector.tensor_tensor(out=Xr_im, in0=Xr_im, in1=Xt, op=ALU.add)
        for ft in range(4):
            for q in range(4):
                st = ft * 4 + q
                S.tensor.matmul(pS_[0][:, q * 128:(q + 1) * 128], lhsT=V_re[:, st * 128:(st + 1) * 128], rhs=tri, start=True, stop=True)
                S.tensor.matmul(pS_[1][:, q * 128:(q + 1) * 128], lhsT=V_im[:, st * 128:(st + 1) * 128], rhs=tri, start=True, stop=True)
            cs = slice(ft * 512, (ft + 1) * 512)
            Er = E_re[:, ft * 4:(ft + 1) * 4, :].rearrange("p a b -> p (a b)")
            Em = E_im[:, ft * 4:(ft + 1) * 4, :].rearrange("p a b -> p (a b)")
            S.vector.tensor_tensor(out=x_re[:, cs], in0=pS_[0][:], in1=Er, op=ALU.mult)
            S.vector.tensor_tensor(out=tA, in0=pS_[1][:], in1=Em, op=ALU.mult)
            S.vector.tensor_tensor(out=x_re[:, cs], in0=x_re[:, cs], in1=tA, op=ALU.subtract)
            S.vector.tensor_tensor(out=nx_im[:, cs], in0=pS_[1][:], in1=Er, op=ALU.mult)
            S.vector.tensor_tensor(out=tB, in0=pS_[0][:], in1=Em, op=ALU.mult)
            S.vector.scalar_tensor_tensor(out=nx_im[:, cs], in0=nx_im[:, cs], scalar=-1.0, in1=tB, op0=ALU.mult, op1=ALU.subtract)
            for q in range(4):
                st = ft * 4 + q
                S.tensor.matmul(pY[:, 0:128], lhsT=cp_re[:, st, :], rhs=x_re[:, st * 128:(st + 1) * 128], start=(q == 0), stop=False)
                S.tensor.matmul(pY[:, 0:128], lhsT=cp_im[:, st, :], rhs=nx_im[:, st * 128:(st + 1) * 128], start=False, stop=(q == 3))
            S.vector.scalar_tensor_tensor(out=g1[:], in0=uT[b][:, ft, :], scalar=dc[:, 0, ft:ft + 1], in1=pY[:, 0:128], op0=ALU.mult, op1=ALU.add)
            S.vector.tensor_tensor(out=g2[:], in0=g1[:], in1=g1[:], op=ALU.mult)
            S.vector.tensor_scalar(out=g2[:], in0=g2[:], scalar1=0.044715, scalar2=1.0, op0=ALU.mult, op1=ALU.add)
            S.vector.tensor_tensor(out=g2[:], in0=g2[:], in1=g1[:], op=ALU.mult)
            S.scalar.activation(out=g2[:], in_=g2[:], func=AF.Tanh, scale=0.7978845608028654)
            S.vector.scalar_tensor_tensor(out=g2[:], in0=g2[:], scalar=1.0, in1=g1[:], op0=ALU.add, op1=ALU.mult)
            S.vector.tensor_scalar(out=gl[:, ft, :], in0=g2[:], scalar1=0.5, scalar2=None, op0=ALU.mult)
        for f2 in range(4):
            for ft in range(4):
                S.tensor.matmul(pY[:, 128:256], lhsT=wg[:, ft, f2 * 128:(f2 + 1) * 128], rhs=gl[:, ft, :], start=(ft == 0), stop=(ft == 3))
            S.scalar.activation(out=g1[:], in_=pY[:, 128:256], func=AF.Sigmoid, bias=dc[:, 1, f2:f2 + 1], scale=1.0)
            S.vector.tensor_tensor(out=yaT[:, f2, :], in0=g1[:], in1=gl[:, f2, :], op=ALU.mult)
        for k in range(4):
            P.transpose(yt[b][:, k * 128:(k + 1) * 128], yaT[:, k, :], pT[k % 2][:, 0:128], eng="vector" if k % 2 == 0 else "scalar")
        S.gpsimd.dma_start(out=y[t0:t0 + 128, :], in_=yt[b][:])
    S.emit()
    return P.nc
def _stage(nc, idx, fn, *args, io):
    with nc.cleanup_on_exit():
        P = Prog(nc=nc, prefix=f"s{idx}_", io=io)
        fn(*args, P=P)
        nc.all_engine_barrier()


def build_fused(S_tok, shapes):
    nc = bass.Bass("TRN2", target_bir_lowering=False)

    def ein(name):
        return nc.dram_tensor(name, list(shapes[name]), F32, kind="ExternalInput").ap()

    def internal(name, shape):
        return nc.dram_tensor(name, list(shape), F32).ap()

    consts = ein("consts")
    xa = ein("x")
    out = nc.dram_tensor("out", [S_tok, 1024], F32, kind="ExternalOutput").ap()
    sid = 0
    for layer in range(4):
        L = f"L{layer}_"
        xo = out if layer == 3 else internal(f"xact{layer}", [S_tok, 1024])
        ya = internal(f"ya{layer}", [S_tok, 512])
        yb = internal(f"yb{layer}", [S_tok, 512])
        if layer % 2 == 0:
            z = internal(f"z{layer}", [S_tok, 2056])
            _stage(nc, sid, build_proj, S_tok, 2056, io={"consts": consts, "x": xa, "w": ein(L + "w_in"), "z": z}); sid += 1
            io = {"consts": consts, "z": z, "y": ya}
            for k in ("rows", "cols", "bpad", "cpad", "dcol", "w_glu"):
                io[k] = ein(L + k)
            _stage(nc, sid, build_s5, S_tok, io=io); sid += 1
            _stage(nc, sid, build_mlstm, S_tok, io={"consts": consts, "z": z, "y": yb, "gb": ein(L + "gb"), "ng": ein(L + "ng")}); sid += 1
            io = {"consts": consts, "x": xa, "ya": ya, "yb": yb, "xo": xo}
            for k in ("w_out", "lnp", "wg", "wu", "wd"):
                io[k] = ein(L + k)
            _stage(nc, sid, build_cf, S_tok, 1, 2816, io=io); sid += 1
        else:
            z = internal(f"z{layer}", [S_tok, 3848])
            _stage(nc, sid, build_proj, S_tok, 3848, io={"consts": consts, "x": xa, "w": ein(L + "w_in"), "z": z}); sid += 1
            io = {"consts": consts, "z": z, "y": ya}
            for k in ("vecs", "mu", "w_up", "a_up", "g_up"):
                io[k] = ein(L + k)
            _stage(nc, sid, build_rwkv, S_tok, io=io); sid += 1
            io = {"consts": consts, "z": z, "y": yb}
            for k in ("conv_w", "a_log", "dt_bias", "norm_g"):
                io[k] = ein(L + k)
            _stage(nc, sid, build_gdn, S_tok, 99, io=io); sid += 1
            io = {"consts": consts, "x": xa, "ya": ya, "yb": yb, "xo": xo}
            for k in ("w_out", "lnp", "wg", "wu", "wd", "router"):
                io[k] = ein(L + k)
            _stage(nc, sid, build_cf, S_tok, 8, 3584, io=io); sid += 1
        xa = xo
    return nc


def host_inputs(inp):
    W = {"consts": CONST_ARR}
    for layer in range(4):
        i = layer // 2
        L = f"L{layer}_"
        W[L + "lnp"] = np.stack([inp["ln_g"][layer, 0], inp["ln_b"][layer, 0], inp["ln_g"][layer, 1], inp["ln_b"][layer, 1]])
        if layer % 2 == 0:
            W[L + "w_in"] = inp["ev_w_in"][i]
            lay = s5_host_layout(inp["s5_lam_re"][i], inp["s5_lam_im"][i], inp["s5_log_dt"][i], inp["s5_b_re"][i], inp["s5_b_im"][i],
                                 inp["s5_c_re"][i], inp["s5_c_im"][i], inp["s5_d"][i], inp["s5_w_glu"][i], inp["s5_b_glu"][i])
            for k, v in lay.items():
                W[L + k] = v
            W[L + "gb"] = inp["ml_gate_bias"][i][None]
            W[L + "ng"] = inp["ml_norm_g"][i][None]
            W[L + "w_out"] = inp["ev_w_out"][i]
            W[L + "wg"] = inp["ffn_w_gate"][i][None]
            W[L + "wu"] = inp["ffn_w_up"][i][None]
            W[L + "wd"] = inp["ffn_w_down"][i][None]
        else:
            W[L + "w_in"] = inp["od_w_in"][i]
            W[L + "vecs"] = np.stack([inp["rw_w0"][i], inp["rw_a0"][i], inp["rw_k_k"][i], inp["rw_k_a"][i], inp["rw_r_k"][i],
                                      inp["rw_ln_g"][i], inp["rw_ln_b"][i], inp["rw_ln_b"][i]])
            W[L + "mu"] = inp["rw_mu"][i][None]
            W[L + "w_up"] = inp["rw_w_up"][i]
            W[L + "a_up"] = inp["rw_a_up"][i]
            W[L + "g_up"] = inp["rw_g_up"][i]
            W[L + "conv_w"] = inp["gd_conv"][i]
            W[L + "a_log"] = inp["gd_a_log"][i][None]
            W[L + "dt_bias"] = inp["gd_dt_bias"][i][None]
            W[L + "norm_g"] = inp["gd_norm_g"][i][None]
            W[L + "w_out"] = inp["od_w_out"][i]
            W[L + "wg"] = inp["moe_w_gate"][i]
            W[L + "wu"] = inp["moe_w_up"][i]
            W[L + "wd"] = inp["moe_w_down"][i]
            W[L + "router"] = inp["moe_router"][i]
    return {k: np.ascontiguousarray(v, dtype=np.float32) for k, v in W.items()}


def kernel(_n_cores=8, **inp):
    inp = {k: np.asarray(v, dtype=np.float32) for k, v in inp.items()}
    S_tok = inp["x"].shape[1]
    W = host_inputs(inp)
    shapes = {k: v.shape for k, v in W.items()}
    shapes["x"] = (S_tok, 1024)
    nc = build_fused(S_tok, shapes)
    in_maps = []
    for c in range(_n_cores):
        d = dict(W)
        d["x"] = np.ascontiguousarray(inp["x"][c])
        in_maps.append(d)
    res = run_bass_kernel_spmd(nc, in_maps, core_ids=list(range(_n_cores)))
    return np.stack([np.asarray(r["out"]) for r in res.results]).astype(np.float32)
```

```python
import numpy as np
import concourse.bass as bass
import concourse.mybir as mybir
from concourse.bass_utils import run_bass_kernel_spmd

F32 = mybir.dt.float32
BF16 = mybir.dt.bfloat16
I32 = mybir.dt.int32
AF = mybir.ActivationFunctionType
ALU = mybir.AluOpType
AX = mybir.AxisListType

ENGS = ["tensor", "vector", "scalar", "gpsimd", "sync"]
WRITE_KW = ("out", "accum_out", "out_max", "out_indices")
SEM_LIMIT = 30000
N_DMA_SEMS = 12


def _is_ap(x):
    return hasattr(x, "tensor") and hasattr(x, "ap")


class _EngProxy:
    def __init__(self, sched, eng):
        self._s = sched
        self._e = eng

    def __getattr__(self, meth):
        def call(*args, **kwargs):
            return self._s._record(self._e, meth, args, kwargs)
        return call


class Sched:
    def __init__(self, nc, same_engine_sync=True, prefix=""):
        self.nc = nc
        self.prefix = prefix
        self.ops = []
        self.same_engine_sync = same_engine_sync
        self.tensor = _EngProxy(self, "tensor")
        self.vector = _EngProxy(self, "vector")
        self.scalar = _EngProxy(self, "scalar")
        self.gpsimd = _EngProxy(self, "gpsimd")
        self.sync = _EngProxy(self, "sync")
        self._ctx = []

    def sb(self, name, shape, dtype=F32):
        g = self.nc.sbuf_tensor(self.prefix + name, list(shape), dtype)
        t = g.__enter__()
        self._ctx.append(g)
        return t

    def ps(self, name, shape, dtype=F32):
        g = self.nc.psum_tensor(self.prefix + name, list(shape), dtype)
        t = g.__enter__()
        self._ctx.append(g)
        return t

    @staticmethod
    def _box(a):
        name = a.tensor.name
        apl = a.ap
        off = a.offset
        if "DRam" in type(a.tensor).__name__:
            ext = sum(st * (c - 1) for st, c in apl) + 1
            return (name, 0, 1, off, off + ext)
        if "PSum" in type(a.tensor).__name__:
            return (name, 0, 128, 0, 1 << 30)
        row = apl[0][0]
        if row <= 0:
            row = 1 << 30
        p0 = off // row
        f0 = off % row
        ext = sum(st * (c - 1) for st, c in apl[1:]) + 1
        return (name, p0, p0 + apl[0][1], f0, f0 + ext)

    def _record(self, eng, meth, args, kwargs):
        reads, writes = [], []
        extra_r = kwargs.pop("_reads", None)
        extra_w = kwargs.pop("_writes", None)
        for i, a in enumerate(args):
            if _is_ap(a):
                (writes if i == 0 else reads).append(self._box(a))
        for k, a in kwargs.items():
            if _is_ap(a):
                (writes if k in WRITE_KW else reads).append(self._box(a))
        if extra_r:
            reads += [self._box(a) for a in extra_r]
        if extra_w:
            writes += [self._box(a) for a in extra_w]
        for bx in list(reads):
            if bx[4] == (1 << 30) and bx not in writes:
                writes.append(bx)
        is_dma = meth in ("dma_start", "dma_start_transpose", "indirect_dma_start")
        self.ops.append(dict(eng=eng, meth=meth, args=args, kwargs=kwargs,
                             reads=reads, writes=writes, dma=is_dma))
        return len(self.ops) - 1

    def emit(self):
        nc = self.nc
        ops = self.ops
        n = len(ops)
        W = {}
        R = {}
        deps = [None] * n

        def ov(a, b):
            return a[1] < b[2] and b[1] < a[2] and a[3] < b[4] and b[3] < a[4]

        def inside(a, b):
            return a[1] >= b[1] and a[2] <= b[2] and a[3] >= b[3] and a[4] <= b[4]

        for i, op in enumerate(ops):
            d = set()
            for bx in op["reads"]:
                for (b2, j) in W.get(bx[0], ()):
                    if ov(bx, b2):
                        d.add(j)
            for bx in op["writes"]:
                for (b2, j) in W.get(bx[0], ()):
                    if ov(bx, b2):
                        d.add(j)
                for (b2, j) in R.get(bx[0], ()):
                    if ov(bx, b2):
                        d.add(j)
            d.discard(i)
            deps[i] = d
            for bx in op["writes"]:
                nm = bx[0]
                W[nm] = [(b2, j) for (b2, j) in W.get(nm, ()) if not inside(b2, bx)] + [(bx, i)]
                R[nm] = [(b2, j) for (b2, j) in R.get(nm, ()) if not inside(b2, bx)]
            for bx in op["reads"]:
                nm = bx[0]
                lst = [(b2, j) for (b2, j) in R.get(nm, ()) if not (b2 == bx and ops[j]["eng"] == op["eng"] and not ops[j]["dma"])]
                lst.append((bx, i))
                R[nm] = lst
        needed = [False] * n
        for i, op in enumerate(ops):
            keep = set()
            for j in deps[i]:
                pj = ops[j]
                if pj["eng"] == op["eng"] and not pj["dma"]:
                    if op["eng"] == "tensor" and not op["dma"]:
                        continue
                    if op["eng"] == "sync":
                        continue
                    if not self.same_engine_sync and not op["dma"]:
                        continue
                keep.add(j)
            deps[i] = keep
            for j in keep:
                needed[j] = True
        sems = {}
        self._semguards = []

        def new_sem(name):
            return nc.alloc_semaphore(name=self.prefix + name)

        eng_sem = {e: new_sem(f"s_{e}_0") for e in ENGS}
        eng_cnt = {e: 0 for e in ENGS}
        eng_gen = {e: 0 for e in ENGS}
        dma_sems = {e: [new_sem(f"d_{e}_{i}") for i in range(N_DMA_SEMS)] for e in ("sync", "gpsimd", "scalar")}
        dma_cnt = {e: [0] * N_DMA_SEMS for e in dma_sems}
        dma_rr = {e: 0 for e in dma_sems}
        dma_last_tok = {e: [None] * N_DMA_SEMS for e in dma_sems}
        token = [None] * n
        prewait = [None] * n
        for i, op in enumerate(ops):
            e = op["eng"]
            if op["dma"]:
                r = dma_rr[e]
                dma_rr[e] = (r + 1) % N_DMA_SEMS
                prewait[i] = dma_last_tok[e][r]
                dma_cnt[e][r] += 16
                if dma_cnt[e][r] > SEM_LIMIT:
                    dma_sems[e][r] = new_sem(f"d_{e}_{r}_{i}")
                    dma_cnt[e][r] = 16
                    prewait[i] = dma_last_tok[e][r]
                token[i] = (dma_sems[e][r], dma_cnt[e][r])
                dma_last_tok[e][r] = token[i]
            elif needed[i]:
                if eng_cnt[e] >= SEM_LIMIT:
                    eng_gen[e] += 1
                    eng_sem[e] = new_sem(f"s_{e}_{eng_gen[e]}")
                    eng_cnt[e] = 0
                eng_cnt[e] += 1
                token[i] = (eng_sem[e], eng_cnt[e])
        final_tokens = []
        for e in dma_sems:
            for t in dma_last_tok[e]:
                if t is not None:
                    final_tokens.append(t)
        per_eng = {e: [] for e in ENGS}
        for i, op in enumerate(ops):
            per_eng[op["eng"]].append(i)
        self.n_waits = 0
        sched = self

        def run_engine(ename, eobj):
            seen = {}
            def wait(tok):
                s, v = tok
                key = id(s)
                if seen.get(key, 0) >= v:
                    return
                seen[key] = v
                eobj.wait_ge(s, v)
                sched.n_waits += 1
            for i in per_eng[ename]:
                op = ops[i]
                for j in sorted(deps[i]):
                    wait(token[j])
                if prewait[i] is not None:
                    wait(prewait[i])
                ins = getattr(eobj, op["meth"])(*op["args"], **op["kwargs"])
                if token[i] is not None:
                    ins.then_inc(token[i][0], 16 if op["dma"] else 1)
            if ename == "sync":
                for t in final_tokens:
                    wait(t)

        with nc.Block() as block:
            @block.tensor
            def _(e):
                run_engine("tensor", e)

            @block.vector
            def _(e):
                run_engine("vector", e)

            @block.scalar
            def _(e):
                run_engine("scalar", e)

            @block.gpsimd
            def _(e):
                run_engine("gpsimd", e)

            @block.sync
            def _(e):
                run_engine("sync", e)
        return nc
DN_ALPHA = 8 ** 0.25
LN_EPS = 1e-5


def make_consts():
    c = {}
    i = np.arange(128)
    c["ident"] = np.eye(128, dtype=np.float32)
    c["tri"] = (i[:, None] <= i[None, :]).astype(np.float32)
    c["ones"] = np.ones((128, 128), np.float32)
    c["negT"] = np.where(i[:, None] <= i[None, :], 0.0, -30000.0).astype(np.float32)
    c["neg"] = np.where(i[:, None] >= i[None, :], 0.0, -30000.0).astype(np.float32)
    c["mT_strict"] = (i[:, None] < i[None, :]).astype(np.float32)
    c["m_strict"] = (i[:, None] > i[None, :]).astype(np.float32)
    blk = (i[:, None] // 64) == (i[None, :] // 64)
    c["btri"] = ((i[:, None] <= i[None, :]) & blk).astype(np.float32)
    c["bmT_incl"] = ((i[:, None] <= i[None, :]) & blk).astype(np.float32)
    c["bmT_strict"] = ((i[:, None] < i[None, :]) & blk).astype(np.float32)
    c["bm_strict"] = ((i[:, None] > i[None, :]) & blk).astype(np.float32)
    ci = np.zeros((128, 128), np.float32)
    ci[:64, 0] = 1.0
    ci[64:, 1] = 1.0
    c["chunkind"] = ci
    c["iota_row"] = np.tile(i[None, :].astype(np.float32), (128, 1))
    c["iota_col"] = np.tile(i[:, None].astype(np.float32), (1, 128))
    names = list(c.keys())
    arr = np.concatenate([c[k] for k in names], axis=1)
    offs = {k: n * 128 for n, k in enumerate(names)}
    return arr, offs


CONST_ARR, CONST_OFF = make_consts()


class Prog:
    def __init__(self, nc=None, prefix="", io=None):
        self.nc = nc if nc is not None else bass.Bass("TRN2", target_bir_lowering=False)
        self.io = io or {}
        self.S = Sched(self.nc, prefix=prefix)
        self.cd = self.din("consts", CONST_ARR.shape)
        self.csb = self.S.sb("csb", CONST_ARR.shape)
        self.S.sync.dma_start(out=self.csb[:], in_=self.cd)
        self._n = 0

    def din(self, name, shape):
        if name in self.io:
            return self.io[name]
        return self.nc.dram_tensor(name, list(shape), F32, kind="ExternalInput").ap()

    def dout(self, name, shape):
        if name in self.io:
            return self.io[name]
        return self.nc.dram_tensor(name, list(shape), F32, kind="ExternalOutput").ap()

    def C(self, name, rows=128, cols=128):
        o = CONST_OFF[name]
        return self.csb[0:rows, o:o + cols]

    def sb(self, name, shape, dtype=F32):
        return self.S.sb(name, shape, dtype)

    def rot(self, name, shape, n=2):
        return [self.S.sb(f"{name}_{i}", shape) for i in range(n)]

    def bc_row(self, name, dram_row, n):
        t = self.S.sb(name, [128, n])
        self.S.sync.dma_start(out=t[:], in_=dram_row.partition_broadcast(128))
        return t

    def transpose(self, dst, src, ps, rows=128, cols=128, eng="vector"):
        S = self.S
        S.tensor.transpose(out=ps, in_=src, identity=self.C("ident", rows, rows))
        if eng == "vector":
            S.vector.tensor_copy(out=dst, in_=ps)
        else:
            S.scalar.copy(out=dst, in_=ps)


def layernorm(P, src, dst, g_bc, b_bc, scr, D=1024, eps=LN_EPS):
    S = P.S
    n = src.shape[0]
    st = {k: v[0:n, :] for k, v in scr.items()}
    S.vector.reduce_sum(out=st["s1"], in_=src, axis=AX.X)
    S.vector.tensor_scalar(out=st["mean"], in0=st["s1"], scalar1=-1.0 / D, scalar2=None, op0=ALU.mult)
    S.vector.tensor_scalar(out=dst, in0=src, scalar1=st["mean"][:, 0:1], scalar2=None, op0=ALU.add)
    S.scalar.activation(out=st["sq"][:, 0:D], in_=dst, func=AF.Square, accum_out=st["ss"])
    S.scalar.activation(out=st["std"], in_=st["ss"], func=AF.Sqrt, bias=st["eps"][:, 0:1], scale=1.0 / D)
    S.vector.reciprocal(out=st["rstd"], in_=st["std"])
    if g_bc is not None:
        S.vector.scalar_tensor_tensor(out=dst, in0=dst, scalar=st["rstd"][:, 0:1], in1=g_bc, op0=ALU.mult, op1=ALU.mult)
        S.vector.tensor_tensor(out=dst, in0=dst, in1=b_bc, op=ALU.add)
    else:
        S.vector.tensor_scalar(out=dst, in0=dst, scalar1=st["rstd"][:, 0:1], scalar2=None, op0=ALU.mult)


def ln_scratch(P, name, D=1024, eps=LN_EPS):
    st = {k: P.sb(f"{name}_{k}", [128, 1]) for k in ("s1", "mean", "ss", "std", "rstd", "eps")}
    st["sq"] = P.sb(f"{name}_sq", [128, D])
    P.S.vector.memset(st["eps"][:], eps)
    return st


def build_proj(S_tok, C, P=None):
    P = P or Prog()
    S = P.S
    x = P.din("x", [S_tok, 1024])
    w = P.din("w", [1024, C])
    z = P.dout("z", [S_tok, C])
    wsb = P.sb("wsb", [128, 8, C])
    for k in range(8):
        S.sync.dma_start(out=wsb[:, k, :], in_=w[k * 128:(k + 1) * 128, :])
    nt = S_tok // 128
    xt = P.rot("xt", [128, 1024])
    xT = P.rot("xT", [128, 8, 128])
    zt = P.rot("zt", [128, C])
    pst = [S.ps(f"pst{i}", [128, 512]) for i in range(2)]
    psm = [S.ps(f"psm{i}", [128, 512]) for i in range(4)]
    chunks = [(c0, min(512, C - c0)) for c0 in range(0, C, 512)]
    for t in range(nt):
        xs = xt[t % 2]
        S.sync.dma_start(out=xs[:], in_=x[t * 128:(t + 1) * 128, :])
        for k in range(8):
            P.transpose(xT[t % 2][:, k, :], xs[:, k * 128:(k + 1) * 128], pst[k % 2][:, 0:128],
                        eng="vector" if k % 2 == 0 else "scalar")
        for ci, (c0, cw) in enumerate(chunks):
            ps = psm[ci % 4]
            for k in range(8):
                S.tensor.matmul(ps[:, 0:cw], lhsT=xT[t % 2][:, k, :], rhs=wsb[:, k, c0:c0 + cw],
                                start=(k == 0), stop=(k == 7))
            if ci % 2 == 0:
                S.vector.tensor_copy(out=zt[t % 2][:, c0:c0 + cw], in_=ps[:, 0:cw])
            else:
                S.scalar.copy(out=zt[t % 2][:, c0:c0 + cw], in_=ps[:, 0:cw])
        S.gpsimd.dma_start(out=z[t * 128:(t + 1) * 128, :], in_=zt[t % 2][:])
    S.emit()
    return P.nc
def build_cf(S_tok, n_exp, d_ff, P=None):
    P = P or Prog()
    S = P.S
    x = P.din("x", [S_tok, 1024])
    ya = P.din("ya", [S_tok, 512])
    yb = P.din("yb", [S_tok, 512])
    w_out = P.din("w_out", [1024, 1024])
    lnp = P.din("lnp", [4, 1024])
    wg = P.din("wg", [n_exp, 1024, d_ff])
    wu = P.din("wu", [n_exp, 1024, d_ff])
    wd = P.din("wd", [n_exp, d_ff, 1024])
    if n_exp > 1:
        router = P.din("router", [1024, 8])
    xo = P.dout("xo", [S_tok, 1024])
    TB = 1024 if S_tok >= 1024 else S_tok
    HB = min(512, TB)
    NT = TB // 128
    wo_sb = P.sb("wo_sb", [128, 8, 1024], BF16)
    for k in range(8):
        S.gpsimd.dma_start(out=wo_sb[:, k, :], in_=w_out[k * 128:(k + 1) * 128, :])
    g0 = P.bc_row("g0", lnp[0:1, :], 1024)
    b0 = P.bc_row("b0", lnp[1:2, :], 1024)
    g1 = P.bc_row("g1", lnp[2:3, :], 1024)
    b1 = P.bc_row("b1", lnp[3:4, :], 1024)
    if n_exp > 1:
        r_sb = P.sb("r_sb", [128, 8, 8])
        for k in range(8):
            S.sync.dma_start(out=r_sb[:, k, :], in_=router[k * 128:(k + 1) * 128, :])
    lns = ln_scratch(P, "lns")
    xt = P.rot("xt", [128, 1024])
    yt = P.rot("yt", [128, 1024])
    yT = [P.sb(f"yT_{i}", [128, 8, 128], BF16) for i in range(2)]
    x1t = P.rot("x1t", [128, 1024])
    x1T = P.sb("x1T", [128, 8, TB], BF16)
    x1Tf = P.rot("x1Tf", [128, 8, 128])
    facc = P.sb("facc", [128, NT, 1024])
    comb = P.sb("comb", [128, NT, 8])
    FC = 512
    chunks = [(f0, min(FC, d_ff - f0)) for f0 in range(0, d_ff, FC)]
    wg_sb = [P.sb(f"wg_sb_{i}", [128, 8, FC], BF16) for i in range(2)]
    wu_sb = [P.sb(f"wu_sb_{i}", [128, 8, FC], BF16) for i in range(2)]
    wd_sb = [P.sb(f"wd_sb_{i}", [128, FC // 128, 1024], BF16) for i in range(2)]
    hT = [P.sb(f"hT_{i}", [128, FC // 128, TB], BF16) for i in range(2)]
    sg = P.rot("sg", [128, HB])
    pst = [S.ps(f"pst{i}", [128, 512]) for i in range(2)]
    pg = [S.ps(f"pg{i}", [128, 512]) for i in range(2)]
    pu = [S.ps(f"pu{i}", [128, 512]) for i in range(2)]
    pd = [S.ps(f"pd{i}", [128, 512]) for i in range(2)]
    sm = {k: P.sb(f"sm_{k}", [128, 8]) for k in ("lg", "m1", "mk1", "l2", "m2", "mk2", "d", "g1", "g2", "t")}
    nblk = S_tok // TB
    it = 0
    for blk in range(nblk):
        for tt in range(NT):
            t0 = blk * TB + tt * 128
            xs, ys = xt[tt % 2], yt[tt % 2]
            S.sync.dma_start(out=xs[:], in_=x[t0:t0 + 128, :])
            S.sync.dma_start(out=ys[:, 0:512], in_=ya[t0:t0 + 128, :])
            S.sync.dma_start(out=ys[:, 512:1024], in_=yb[t0:t0 + 128, :])
            for k in range(8):
                P.transpose(yT[tt % 2][:, k, :], ys[:, k * 128:(k + 1) * 128], pst[k % 2][:, 0:128],
                            eng="vector" if k % 2 == 0 else "scalar")
            for half in range(2):
                ps = pd[half]
                for k in range(8):
                    S.tensor.matmul(ps[:], lhsT=yT[tt % 2][:, k, :], rhs=wo_sb[:, k, half * 512:(half + 1) * 512],
                                    start=(k == 0), stop=(k == 7))
                S.vector.scalar_tensor_tensor(out=xs[:, half * 512:(half + 1) * 512], in0=xs[:, half * 512:(half + 1) * 512],
                                              scalar=DN_ALPHA, in1=ps[:], op0=ALU.mult, op1=ALU.add)
            X1 = x1t[tt % 2]
            layernorm(P, xs[:], X1[:], g0[:], b0[:], lns)
            S.scalar.mul(out=facc[:, tt, :], in_=X1[:], mul=DN_ALPHA)
            for k in range(8):
                S.tensor.transpose(out=pst[k % 2][:, 0:128], in_=X1[:, k * 128:(k + 1) * 128], identity=P.C("ident"))
                if k % 2 == 0:
                    S.vector.tensor_copy(out=x1T[:, k, tt * 128:(tt + 1) * 128], in_=pst[k % 2][:, 0:128])
                    if n_exp > 1:
                        S.scalar.copy(out=x1Tf[tt % 2][:, k, :], in_=pst[k % 2][:, 0:128])
                else:
                    S.scalar.copy(out=x1T[:, k, tt * 128:(tt + 1) * 128], in_=pst[k % 2][:, 0:128])
                    if n_exp > 1:
                        S.vector.tensor_copy(out=x1Tf[tt % 2][:, k, :], in_=pst[k % 2][:, 0:128])
            if n_exp > 1:
                ps = pd[0]
                for k in range(8):
                    S.tensor.matmul(ps[:, 0:8], lhsT=x1Tf[tt % 2][:, k, :], rhs=r_sb[:, k, :],
                                    start=(k == 0), stop=(k == 7))
                S.vector.tensor_copy(out=sm["lg"][:], in_=ps[:, 0:8])
                S.vector.reduce_max(out=sm["m1"][:, 0:1], in_=sm["lg"][:], axis=AX.X)
                S.vector.tensor_scalar(out=sm["mk1"][:], in0=sm["lg"][:], scalar1=sm["m1"][:, 0:1], scalar2=None, op0=ALU.is_ge)
                S.vector.scalar_tensor_tensor(out=sm["l2"][:], in0=sm["mk1"][:], scalar=-1e30, in1=sm["lg"][:], op0=ALU.mult, op1=ALU.add)
                S.vector.reduce_max(out=sm["m2"][:, 0:1], in_=sm["l2"][:], axis=AX.X)
                S.vector.tensor_scalar(out=sm["mk2"][:], in0=sm["l2"][:], scalar1=sm["m2"][:, 0:1], scalar2=None, op0=ALU.is_ge)
                S.vector.tensor_tensor(out=sm["d"][:, 0:1], in0=sm["m2"][:, 0:1], in1=sm["m1"][:, 0:1], op=ALU.subtract)
                S.scalar.activation(out=sm["t"][:, 0:1], in_=sm["d"][:, 0:1], func=AF.Exp)
                S.vector.tensor_scalar(out=sm["t"][:, 0:1], in0=sm["t"][:, 0:1], scalar1=1.0, scalar2=None, op0=ALU.add)
                S.vector.reciprocal(out=sm["g1"][:, 0:1], in_=sm["t"][:, 0:1])
                S.vector.tensor_scalar(out=sm["g2"][:, 0:1], in0=sm["g1"][:, 0:1], scalar1=-1.0, scalar2=1.0, op0=ALU.mult, op1=ALU.add)
                S.vector.tensor_scalar(out=comb[:, tt, :], in0=sm["mk1"][:], scalar1=sm["g1"][:, 0:1], scalar2=None, op0=ALU.mult)
                S.vector.scalar_tensor_tensor(out=comb[:, tt, :], in0=sm["mk2"][:], scalar=sm["g2"][:, 0:1], in1=comb[:, tt, :],
                                              op0=ALU.mult, op1=ALU.add)
        for e in range(n_exp):
            for (f0, fcw) in chunks:
                b = it % 2
                it += 1
                nft = fcw // 128
                S.gpsimd.dma_start(out=wg_sb[b][:, :, 0:fcw], in_=wg[e, :, f0:f0 + fcw].rearrange("(k p) f -> p k f", p=128))
                S.gpsimd.dma_start(out=wu_sb[b][:, :, 0:fcw], in_=wu[e, :, f0:f0 + fcw].rearrange("(k p) f -> p k f", p=128))
                S.gpsimd.dma_start(out=wd_sb[b][:, 0:nft, :], in_=wd[e, f0:f0 + fcw, :].rearrange("(k p) f -> p k f", p=128))
                for hb in range(TB // HB):
                    hsl = slice(hb * HB, (hb + 1) * HB)
                    for ft in range(nft):
                        for k in range(8):
                            S.tensor.matmul(pg[ft % 2][:, 0:HB], lhsT=wg_sb[b][:, k, ft * 128:(ft + 1) * 128], rhs=x1T[:, k, hsl],
                                            start=(k == 0), stop=(k == 7))
                        for k in range(8):
                            S.tensor.matmul(pu[ft % 2][:, 0:HB], lhsT=wu_sb[b][:, k, ft * 128:(ft + 1) * 128], rhs=x1T[:, k, hsl],
                                            start=(k == 0), stop=(k == 7))
                        S.scalar.activation(out=sg[ft % 2][:], in_=pg[ft % 2][:, 0:HB], func=AF.Silu)
                        S.vector.tensor_tensor(out=hT[b][:, ft, hsl], in0=sg[ft % 2][:], in1=pu[ft % 2][:, 0:HB], op=ALU.mult)
                for tt in range(NT):
                    for half in range(2):
                        ps = pd[(tt * 2 + half) % 2]
                        for ft in range(nft):
                            S.tensor.matmul(ps[:], lhsT=hT[b][:, ft, tt * 128:(tt + 1) * 128],
                                            rhs=wd_sb[b][:, ft, half * 512:(half + 1) * 512],
                                            start=(ft == 0), stop=(ft == nft - 1))
                        fa = facc[:, tt, half * 512:(half + 1) * 512]
                        if n_exp > 1:
                            S.vector.scalar_tensor_tensor(out=fa, in0=ps[:], scalar=comb[:, tt, e:e + 1], in1=fa,
                                                          op0=ALU.mult, op1=ALU.add)
                        else:
                            S.vector.tensor_tensor(out=fa, in0=fa, in1=ps[:], op=ALU.add)
        for tt in range(NT):
            t0 = blk * TB + tt * 128
            layernorm(P, facc[:, tt, :], xt[tt % 2][:], g1[:], b1[:], lns)
            S.sync.dma_start(out=xo[t0:t0 + 128, :], in_=xt[tt % 2][:])
    S.emit()
    return P.nc
def build_mlstm(S_tok, P=None):
    P = P or Prog()
    S = P.S
    z = P.din("z", [S_tok, 2056])
    gb = P.din("gb", [1, 8])
    ng = P.din("ng", [1, 512])
    y = P.dout("y", [S_tok, 512])
    gbb = P.bc_row("gbb", gb, 8)
    ngb = P.bc_row("ngb", ng, 512)
    Caug = P.sb("Caug", [64, 4, 129])
    S.vector.memset(Caug[:], 0.0)
    zt = P.rot("zt", [128, 1544])
    vaug = P.rot("vaug", [128, 4, 129])
    for b in range(2):
        S.vector.memset(vaug[b][:], 1.0)
    yt = P.rot("yt", [128, 512])
    sm = {k: P.sb(f"m_{k}", [128, 8]) for k in ("ig", "gf", "e", "lf", "bb", "eb", "es", "ebt", "tmp", "den", "rec")}
    LFtri = P.rot("LFtri", [128, 128])
    Rm = P.rot("Rm", [128, 128])
    ED = P.rot("ED", [128, 128])
    SDT = P.rot("SDT", [128, 128])
    qT = P.rot("qT", [64, 128])
    kT = P.rot("kT", [64, 128])
    qs = P.rot("qs", [128, 64])
    qsT = P.rot("qsT", [64, 128])
    ks = P.rot("ks", [128, 64])
    hh = P.rot("hh", [128, 128])
    sg = P.rot("sg", [128, 512])
    lns = ln_scratch(P, "mln", D=128, eps=1e-6)
    pT = [S.ps(f"pT{i}", [128, 512]) for i in range(2)]
    pD = S.ps("pD", [128, 512])
    pS = S.ps("pS", [128, 512])
    pN = S.ps("pN", [128, 512])
    pC = S.ps("pC", [128, 512])
    pB = S.ps("pB", [128, 512])
    ident, tri, ones, negT = P.C("ident"), P.C("tri"), P.C("ones"), P.C("negT")
    for t in range(S_tok // 128):
        t0 = t * 128
        Z = zt[t % 2]
        S.sync.dma_start(out=Z[:], in_=z[t0:t0 + 128, 512:2056])
        q, k, v, o = Z[:, 0:256], Z[:, 256:512], Z[:, 512:1024], Z[:, 1024:1536]
        S.vector.tensor_tensor(out=sm["ig"][:, 0:4], in0=Z[:, 1536:1540], in1=gbb[:, 0:4], op=ALU.add)
        S.vector.tensor_tensor(out=sm["gf"][:, 0:4], in0=Z[:, 1540:1544], in1=gbb[:, 4:8], op=ALU.add)
        S.scalar.activation(out=sm["e"][:, 0:4], in_=sm["gf"][:, 0:4], func=AF.Exp, scale=-1.0)
        S.vector.tensor_scalar(out=sm["e"][:, 0:4], in0=sm["e"][:, 0:4], scalar1=1.0, scalar2=None, op0=ALU.add)
        S.scalar.activation(out=sm["lf"][:, 0:4], in_=sm["e"][:, 0:4], func=AF.Ln)
        S.vector.tensor_scalar(out=sm["lf"][:, 0:4], in0=sm["lf"][:, 0:4], scalar1=-1.0, scalar2=None, op0=ALU.mult)
        S.tensor.matmul(pB[:, 0:4], lhsT=tri, rhs=sm["lf"][:, 0:4], start=True, stop=True)
        S.tensor.matmul(pB[:, 4:8], lhsT=ones, rhs=sm["lf"][:, 0:4], start=True, stop=True)
        S.vector.tensor_copy(out=sm["bb"][:], in_=pB[:, 0:8])
        S.scalar.activation(out=sm["eb"][:, 0:4], in_=sm["bb"][:, 0:4], func=AF.Exp)
        S.scalar.activation(out=sm["ebt"][:, 0:4], in_=sm["bb"][:, 4:8], func=AF.Exp)
        S.vector.tensor_tensor(out=sm["tmp"][:, 0:4], in0=sm["bb"][:, 4:8], in1=sm["bb"][:, 0:4], op=ALU.subtract)
        S.vector.tensor_tensor(out=sm["tmp"][:, 0:4], in0=sm["tmp"][:, 0:4], in1=sm["ig"][:, 0:4], op=ALU.add)
        S.scalar.activation(out=sm["es"][:, 0:4], in_=sm["tmp"][:, 0:4], func=AF.Exp)
        VA = vaug[t % 2]
        S.vector.tensor_copy(out=VA[:, :, 0:128], in_=v.rearrange("p (h d) -> p h d", h=4))
        S.scalar.activation(out=sg[t % 2][:], in_=o, func=AF.Sigmoid)
        Y = yt[t % 2]
        for h in range(4):
            u = (t * 4 + h) % 2
            S.vector.tensor_scalar(out=LFtri[u][:], in0=tri, scalar1=sm["lf"][:, h:h + 1], scalar2=None, op0=ALU.mult)
            S.vector.scalar_tensor_tensor(out=Rm[u][:], in0=ident, scalar=sm["ig"][:, h:h + 1], in1=LFtri[u][:],
                                          op0=ALU.mult, op1=ALU.subtract)
            S.tensor.matmul(pD[:, 0:128], lhsT=ones, rhs=LFtri[u][:], start=True, stop=False)
            S.tensor.matmul(pD[:, 0:128], lhsT=Rm[u][:], rhs=ones, start=False, stop=False)
            S.tensor.matmul(pD[:, 0:128], lhsT=ident, rhs=negT, start=False, stop=True)
            S.scalar.activation(out=ED[u][:], in_=pD[:, 0:128], func=AF.Exp)
            qh, kh = q[:, h * 64:(h + 1) * 64], k[:, h * 64:(h + 1) * 64]
            P.transpose(qT[u][:], qh, pT[0][0:64, 0:128], rows=128, cols=64)
            P.transpose(kT[u][:], kh, pT[1][0:64, 0:128], rows=128, cols=64, eng="scalar")
            S.vector.tensor_scalar(out=qs[u][:], in0=qh, scalar1=sm["eb"][:, h:h + 1], scalar2=0.125, op0=ALU.mult, op1=ALU.mult)
            P.transpose(qsT[u][:], qs[u][:], pT[0][0:64, 0:128], rows=128, cols=64)
            S.tensor.matmul(pS[:, 0:128], lhsT=kT[u][:], rhs=qT[u][:], start=True, stop=True)
            S.vector.scalar_tensor_tensor(out=SDT[u][:], in0=pS[:, 0:128], scalar=0.125, in1=ED[u][:], op0=ALU.mult, op1=ALU.mult)
            S.tensor.matmul(pN[:, 0:129], lhsT=SDT[u][:], rhs=VA[:, h, :], start=True, stop=False)
            S.tensor.matmul(pN[:, 0:129], lhsT=qsT[u][:], rhs=Caug[:, h, :], start=False, stop=True)
            S.scalar.activation(out=sm["den"][:, 0:1], in_=pN[:, 128:129], func=AF.Abs)
            S.vector.tensor_scalar_max(out=sm["den"][:, 0:1], in0=sm["den"][:, 0:1], scalar1=1.0)
            S.vector.reciprocal(out=sm["rec"][:, 0:1], in_=sm["den"][:, 0:1])
            S.vector.tensor_scalar(out=hh[u][:], in0=pN[:, 0:128], scalar1=sm["rec"][:, 0:1], scalar2=None, op0=ALU.mult)
            S.vector.tensor_scalar(out=ks[u][:], in0=kh, scalar1=sm["es"][:, h:h + 1], scalar2=None, op0=ALU.mult)
            S.tensor.matmul(pC[0:64, 0:129], lhsT=ks[u][:], rhs=VA[:, h, :], start=True, stop=True)
            S.vector.scalar_tensor_tensor(out=Caug[:, h, :], in0=Caug[:, h, :], scalar=sm["ebt"][0:64, h:h + 1], in1=pC[0:64, 0:129],
                                          op0=ALU.mult, op1=ALU.add)
            layernorm(P, hh[u][:], Y[:, h * 128:(h + 1) * 128], None, None, lns, D=128, eps=1e-6)
        S.vector.tensor_tensor(out=Y[:], in0=Y[:], in1=ngb[:], op=ALU.mult)
        S.vector.tensor_tensor(out=Y[:], in0=Y[:], in1=sg[t % 2][:], op=ALU.mult)
        S.gpsimd.dma_start(out=y[t0:t0 + 128, :], in_=Y[:])
    S.emit()
    return P.nc
class _Stop(Exception):
    pass


def build_gdn(S_tok, stop=99, P=None):
    try:
        return _build_gdn(S_tok, stop, P)
    except _Stop as e:
        P = e.args[0]
        P.S.emit()
        return P.nc


def _build_gdn(S_tok, stop, P=None):
    P = P or Prog()
    S = P.S
    z = P.din("z", [S_tok, 3848])
    conv_w = P.din("conv_w", [4, 1536])
    a_log = P.din("a_log", [1, 4])
    dt_bias = P.din("dt_bias", [1, 4])
    norm_g = P.din("norm_g", [1, 128])
    y = P.dout("y", [S_tok, 512])
    cw = [P.bc_row(f"cw{i}", conv_w[i:i + 1, :], 1536) for i in range(4)]
    alb = P.bc_row("alb", a_log, 4)
    dtb = P.bc_row("dtb", dt_bias, 4)
    ngb = P.bc_row("ngb", norm_g, 128)
    ea = P.sb("ea", [128, 4])
    S.scalar.activation(out=ea[:], in_=alb[:], func=AF.Exp)
    state = P.sb("state", [128, 4, 128])
    S.vector.memset(state[:], 0.0)
    xs = [P.rot(f"xs{k}", [128, 1536]) for k in range(4)]
    gt = P.rot("gt", [128, 520])
    acc = P.rot("acc", [128, 1536])
    tmp = P.rot("ctmp", [128, 1536])
    sq = P.sb("sq", [128, 1024])
    yt = P.rot("yt", [128, 512])
    sgt = P.rot("sgt", [128, 512])
    sm = {k: P.sb(f"g_{k}", [128, 8]) for k in ("ss", "rn", "beta", "sp", "g", "dd", "edec", "ek", "edl", "t", "ss2", "rs")}
    eps6 = P.sb("eps6", [128, 1])
    S.vector.memset(eps6[:], 1e-6)
    names = ["Gtri", "nGtri", "GT", "GTs", "G", "Gs", "kb", "kT", "kbT", "qn", "qT", "kn", "rhs_v", "rhs_k", "nwT", "u", "QKT",
             "qd", "qdT", "kd", "o", "osq"]
    B = {n: P.rot(n, [128, 128]) for n in names}
    X = P.rot("X", [128, 128], 2)
    Y = P.rot("Y", [128, 128], 2)
    Pm = P.rot("Pm", [128, 128], 2)
    pT = [S.ps(f"pT{i}", [128, 512]) for i in range(2)]
    pG = S.ps("pG", [128, 512])
    pA = S.ps("pA", [128, 512])
    pI = [S.ps(f"pI{i}", [128, 512]) for i in range(2)]
    pU = S.ps("pU", [128, 512])
    pS = S.ps("pS", [128, 512])
    ident, tri, ones, negT, neg = P.C("ident"), P.C("tri"), P.C("ones"), P.C("negT"), P.C("neg")
    mT_s, m_s = P.C("mT_strict"), P.C("m_strict")
    for t in range(S_tok // 128):
        t0 = t * 128
        b = t % 2
        for k in range(4):
            if t0 - k < 0:
                S.vector.memset(xs[k][b][0:32, :], 0.0)
                S.sync.dma_start(out=xs[k][b][k:128, :], in_=z[0:128 - k, 1792:3328])
            else:
                S.sync.dma_start(out=xs[k][b][:], in_=z[t0 - k:t0 - k + 128, 1792:3328])
        G = gt[b]
        S.sync.dma_start(out=G[:], in_=z[t0:t0 + 128, 3328:3848])
        S.vector.tensor_tensor(out=acc[b][:], in0=xs[0][b][:], in1=cw[3][:], op=ALU.mult)
        for k in (1, 2, 3):
            S.vector.tensor_tensor(out=xs[k][b][:], in0=xs[k][b][:], in1=cw[3 - k][:], op=ALU.mult)
            S.vector.tensor_tensor(out=acc[b][:], in0=acc[b][:], in1=xs[k][b][:], op=ALU.add)
        A = acc[b]
        if stop == 1:
            raise _Stop(P)
        S.scalar.activation(out=A[:], in_=A[:], func=AF.Silu)
        S.scalar.activation(out=sq[:], in_=A[:, 0:1024], func=AF.Square)
        S.vector.reduce_sum(out=sm["ss"][:, 0:8], in_=sq[:].rearrange("p (h d) -> p h d", h=8), axis=AX.X)
        S.scalar.activation(out=sm["ss"][:, 0:8], in_=sm["ss"][:, 0:8], func=AF.Sqrt, bias=eps6[:, 0:1], scale=1.0)
        S.vector.reciprocal(out=sm["rn"][:, 0:8], in_=sm["ss"][:, 0:8])
        S.scalar.activation(out=sm["beta"][:, 0:4], in_=G[:, 512:516], func=AF.Sigmoid)
        S.vector.tensor_tensor(out=sm["sp"][:, 0:4], in0=G[:, 516:520], in1=dtb[:], op=ALU.add)
        S.scalar.activation(out=sm["sp"][:, 0:4], in_=sm["sp"][:, 0:4], func=AF.Exp)
        S.vector.tensor_scalar(out=sm["sp"][:, 0:4], in0=sm["sp"][:, 0:4], scalar1=1.0, scalar2=None, op0=ALU.add)
        S.scalar.activation(out=sm["sp"][:, 0:4], in_=sm["sp"][:, 0:4], func=AF.Ln)
        S.vector.scalar_tensor_tensor(out=sm["g"][:, 0:4], in0=sm["sp"][:, 0:4], scalar=-1.0, in1=ea[:], op0=ALU.mult, op1=ALU.mult)
        S.tensor.matmul(pS[:, 0:4], lhsT=tri, rhs=sm["g"][:, 0:4], start=True, stop=True)
        S.tensor.matmul(pS[:, 4:8], lhsT=ones, rhs=sm["g"][:, 0:4], start=True, stop=True)
        S.vector.tensor_copy(out=sm["dd"][:], in_=pS[:, 0:8])
        S.scalar.activation(out=sm["edec"][:, 0:4], in_=sm["dd"][:, 0:4], func=AF.Exp)
        S.scalar.activation(out=sm["edl"][:, 0:4], in_=sm["dd"][:, 4:8], func=AF.Exp)
        S.vector.tensor_tensor(out=sm["t"][:, 0:4], in0=sm["dd"][:, 4:8], in1=sm["dd"][:, 0:4], op=ALU.subtract)
        S.scalar.activation(out=sm["ek"][:, 0:4], in_=sm["t"][:, 0:4], func=AF.Exp)
        S.scalar.activation(out=sgt[b][:], in_=G[:, 0:512], func=AF.Silu)
        if stop == 2:
            raise _Stop(P)
        Yt = yt[b]
        for h in range(4):
            u = (t * 4 + h) % 2
            T_ = {n: B[n][u] for n in names}
            qh, kh, vh = A[:, h * 128:(h + 1) * 128], A[:, 512 + h * 128:512 + (h + 1) * 128], A[:, 1024 + h * 128:1024 + (h + 1) * 128]
            S.vector.tensor_scalar(out=T_["Gtri"][:], in0=tri, scalar1=sm["g"][:, h:h + 1], scalar2=None, op0=ALU.mult)
            S.vector.tensor_scalar(out=T_["nGtri"][:], in0=T_["Gtri"][:], scalar1=-1.0, scalar2=None, op0=ALU.mult)
            S.tensor.matmul(pG[:, 0:128], lhsT=ones, rhs=T_["Gtri"][:], start=True, stop=False)
            S.tensor.matmul(pG[:, 0:128], lhsT=T_["nGtri"][:], rhs=ones, start=False, stop=False)
            S.tensor.matmul(pG[:, 0:128], lhsT=ident, rhs=negT, start=False, stop=True)
            S.tensor.matmul(pG[:, 128:256], lhsT=T_["Gtri"][:], rhs=ones, start=True, stop=False)
            S.tensor.matmul(pG[:, 128:256], lhsT=ones, rhs=T_["nGtri"][:], start=False, stop=False)
            S.tensor.matmul(pG[:, 128:256], lhsT=ident, rhs=neg, start=False, stop=True)
            S.scalar.activation(out=T_["GT"][:], in_=pG[:, 0:128], func=AF.Exp)
            S.scalar.activation(out=T_["G"][:], in_=pG[:, 128:256], func=AF.Exp)
            S.vector.tensor_tensor(out=T_["GTs"][:], in0=T_["GT"][:], in1=mT_s, op=ALU.mult)
            S.vector.tensor_tensor(out=T_["Gs"][:], in0=T_["G"][:], in1=m_s, op=ALU.mult)
            if stop == 3:
                raise _Stop(P)
            S.vector.tensor_scalar(out=T_["qn"][:], in0=qh, scalar1=sm["rn"][:, h:h + 1], scalar2=128 ** -0.5, op0=ALU.mult, op1=ALU.mult)
            S.vector.tensor_scalar(out=T_["kn"][:], in0=kh, scalar1=sm["rn"][:, 4 + h:5 + h], scalar2=None, op0=ALU.mult)
            S.vector.tensor_scalar(out=T_["kb"][:], in0=T_["kn"][:], scalar1=sm["beta"][:, h:h + 1], scalar2=None, op0=ALU.mult)
            P.transpose(T_["kT"][:], T_["kn"][:], pT[0][:, 0:128])
            P.transpose(T_["kbT"][:], T_["kb"][:], pT[1][:, 0:128], eng="scalar")
            P.transpose(T_["qT"][:], T_["qn"][:], pT[0][:, 0:128])
            S.vector.tensor_scalar(out=T_["qd"][:], in0=T_["qn"][:], scalar1=sm["edec"][:, h:h + 1], scalar2=None, op0=ALU.mult)
            P.transpose(T_["qdT"][:], T_["qd"][:], pT[1][:, 0:128], eng="scalar")
            S.vector.tensor_scalar(out=T_["kd"][:], in0=T_["kn"][:], scalar1=sm["ek"][:, h:h + 1], scalar2=None, op0=ALU.mult)
            S.vector.tensor_scalar(out=T_["rhs_v"][:], in0=vh, scalar1=sm["beta"][:, h:h + 1], scalar2=None, op0=ALU.mult)
            S.vector.tensor_scalar(out=T_["rhs_k"][:], in0=T_["kb"][:], scalar1=sm["edec"][:, h:h + 1], scalar2=None, op0=ALU.mult)
            if stop == 4:
                raise _Stop(P)
            S.tensor.matmul(pA[:, 0:128], lhsT=T_["kT"][:], rhs=T_["kbT"][:], start=True, stop=True)
            S.tensor.matmul(pA[:, 128:256], lhsT=T_["kbT"][:], rhs=T_["kT"][:], start=True, stop=True)
            S.tensor.matmul(pA[:, 256:384], lhsT=T_["kT"][:], rhs=T_["qT"][:], start=True, stop=True)
            S.vector.scalar_tensor_tensor(out=X[0][:], in0=pA[:, 0:128], scalar=-1.0, in1=T_["GTs"][:], op0=ALU.mult, op1=ALU.mult)
            S.vector.scalar_tensor_tensor(out=Y[0][:], in0=pA[:, 128:256], scalar=-1.0, in1=T_["Gs"][:], op0=ALU.mult, op1=ALU.mult)
            S.vector.tensor_tensor(out=T_["QKT"][:], in0=pA[:, 256:384], in1=T_["GT"][:], op=ALU.mult)
            S.vector.tensor_tensor(out=Pm[0][:], in0=X[0][:], in1=ident, op=ALU.add)
            if stop == 5:
                raise _Stop(P)
            nst = 6
            for n in range(nst):
                a, c = n % 2, (n + 1) % 2
                if n < nst - 1:
                    S.tensor.matmul(pI[0][:, 0:128], lhsT=Y[a][:], rhs=X[a][:], start=True, stop=True)
                S.tensor.matmul(pI[0][:, 128:256], lhsT=X[a][:], rhs=Y[a][:], start=True, stop=True)
                if n < nst - 1:
                    S.scalar.copy(out=X[c][:], in_=pI[0][:, 0:128])
                S.vector.tensor_copy(out=Y[c][:], in_=pI[0][:, 128:256])
                S.tensor.matmul(pI[1][:, 0:128], lhsT=Y[c][:], rhs=Pm[a][:], start=True, stop=True)
                S.vector.tensor_tensor(out=Pm[c][:], in0=Pm[a][:], in1=pI[1][:, 0:128], op=ALU.add)
            TT = Pm[nst % 2]
            if stop == 6:
                raise _Stop(P)
            S.tensor.matmul(pS[:, 128:256], lhsT=T_["rhs_k"][:], rhs=TT[:], start=True, stop=True)
            S.scalar.mul(out=T_["nwT"][:], in_=pS[:, 128:256], mul=-1.0)
            S.tensor.matmul(pU[:, 0:128], lhsT=TT[:], rhs=T_["rhs_v"][:], start=True, stop=False)
            S.tensor.matmul(pU[:, 0:128], lhsT=T_["nwT"][:], rhs=state[:, h, :], start=False, stop=True)
            S.vector.tensor_copy(out=T_["u"][:], in_=pU[:, 0:128])
            S.tensor.matmul(pU[:, 128:256], lhsT=T_["qdT"][:], rhs=state[:, h, :], start=True, stop=False)
            S.tensor.matmul(pU[:, 128:256], lhsT=T_["QKT"][:], rhs=T_["u"][:], start=False, stop=True)
            S.tensor.matmul(pS[:, 256:384], lhsT=T_["kd"][:], rhs=T_["u"][:], start=True, stop=True)
            S.scalar.copy(out=T_["o"][:], in_=pU[:, 128:256])
            S.vector.scalar_tensor_tensor(out=state[:, h, :], in0=state[:, h, :], scalar=sm["edl"][:, h:h + 1], in1=pS[:, 256:384],
                                          op0=ALU.mult, op1=ALU.add)
            if stop == 7:
                raise _Stop(P)
            S.scalar.activation(out=T_["osq"][:], in_=T_["o"][:], func=AF.Square, accum_out=sm["ss2"][:, 0:1])
            S.scalar.activation(out=sm["rs"][:, 0:1], in_=sm["ss2"][:, 0:1], func=AF.Sqrt, bias=eps6[:, 0:1], scale=1.0 / 128)
            S.vector.reciprocal(out=sm["rs"][:, 0:1], in_=sm["rs"][:, 0:1])
            S.vector.scalar_tensor_tensor(out=Yt[:, h * 128:(h + 1) * 128], in0=T_["o"][:], scalar=sm["rs"][:, 0:1], in1=ngb[:],
                                          op0=ALU.mult, op1=ALU.mult)
        S.vector.tensor_tensor(out=Yt[:], in0=Yt[:], in1=sgt[b][:], op=ALU.mult)
        S.gpsimd.dma_start(out=y[t0:t0 + 128, :], in_=Yt[:])
    S.emit()
    return P.nc
def build_rwkv(S_tok, P=None):
    P = P or Prog()
    S = P.S
    z = P.din("z", [S_tok, 3848])
    vecs = P.din("vecs", [8, 512])
    mu = P.din("mu", [1, 1792])
    w_up = P.din("w_up", [64, 512])
    a_up = P.din("a_up", [64, 512])
    g_up = P.din("g_up", [128, 512])
    y = P.dout("y", [S_tok, 512])
    mub = P.bc_row("mub", mu, 1792)
    vb = [P.bc_row(f"vb{i}", vecs[i:i + 1, :], 512) for i in range(7)]
    w0b, a0b, kkb, kab, rkb, lgb, lbb = vb
    wup = P.sb("wup", [64, 512]); aup = P.sb("aup", [64, 512]); gup = P.sb("gup", [128, 512])
    S.sync.dma_start(out=wup[:], in_=w_up); S.sync.dma_start(out=aup[:], in_=a_up); S.sync.dma_start(out=gup[:], in_=g_up)
    H = P.sb("H", [64, 8, 64])
    S.vector.memset(H[:], 0.0)
    L = 64
    cur = P.rot("cur", [L, 1792]); prev = P.rot("prev", [L, 1792])
    bigs = [{n: P.sb(f"r{i}_{n}", [L, 512]) for n in ("logw", "a", "g", "kk", "kp", "b", "ecl", "ecm", "eni", "rt", "kat", "kt", "bt", "nbt",
                                                        "tmp", "Y", "Yn", "sq")} for i in range(2)]
    sm = {n: P.sb(f"rs_{n}", [L, 8]) for n in ("ss", "rn", "bs")}
    tw = P.sb("tw", [L, 64]); twT = P.sb("twT", [64, L]); adT = P.sb("adT", [64, L])
    sgd = P.sb("sgd", [L, 128]); sgdT = P.sb("sgdT", [128, L])
    PL = P.sb("PL", [64, 8])
    eps6 = P.sb("eps6", [128, 1])
    S.vector.memset(eps6[:], 1e-6)
    TN = ["rtT", "katT", "ktT", "btT"]
    TT_ = {n: P.rot(n, [64, L]) for n in TN}
    MN = ["AkvT", "BrkT", "nBrbT"]
    M_ = {n: P.rot(n, [L, L]) for n in MN}
    X2 = [P.rot(f"X{u}", [L, L]) for u in range(2)]; Y2 = [P.rot(f"Yq{u}", [L, L]) for u in range(2)]; P2 = [P.rot(f"Pm{u}", [L, L]) for u in range(2)]
    Wsb = P.rot("Wsb", [L, 64]); Usb = P.rot("Usb", [L, 64])
    lns2 = [ln_scratch(P, f"rln{u}", D=64, eps=64e-5) for u in range(2)]
    pT = [S.ps(f"pT{i}", [128, 512]) for i in range(2)]
    pL = S.ps("pL", [128, 512])
    pA = [S.ps(f"pA{i}", [128, 512]) for i in range(2)]
    pI = [S.ps(f"pI{i}", [128, 512]) for i in range(2)]
    pC = S.ps("pC", [128, 512])
    ident, btri, chunkind = P.C("ident", L, L), P.C("tri", L, L), P.C("ones", L, 1)
    bmT_i, bmT_s, bm_s = P.C("tri", L, L), P.C("mT_strict", L, L), P.C("m_strict", L, L)
    for t in range(S_tok // L):
        t0 = t * L
        b = t % 2
        B = bigs[b]
        Cc, Pp = cur[b], prev[b]
        S.sync.dma_start(out=Cc[:], in_=z[t0:t0 + L, 0:1792])
        if t == 0:
            S.vector.memset(Pp[0:32, :], 0.0)
            S.sync.dma_start(out=Pp[1:L, :], in_=z[0:L - 1, 0:1792])
        else:
            S.sync.dma_start(out=Pp[:], in_=z[t0 - 1:t0 + L - 1, 0:1792])
        S.vector.tensor_tensor(out=Pp[:], in0=Pp[:], in1=Cc[:], op=ALU.subtract)
        S.vector.tensor_tensor(out=Pp[:], in0=Pp[:], in1=mub[0:L, :], op=ALU.mult)
        S.vector.tensor_tensor(out=Cc[:], in0=Cc[:], in1=Pp[:], op=ALU.add)
        r, k, v = Cc[:, 0:512], Cc[:, 512:1024], Cc[:, 1024:1536]
        S.scalar.activation(out=tw[:], in_=Cc[:, 1536:1600], func=AF.Tanh)
        P.transpose(twT[:], tw[:], pT[0][0:64, 0:L], rows=L, cols=64)
        P.transpose(adT[:], Cc[:, 1600:1664], pT[1][0:64, 0:L], rows=L, cols=64, eng="scalar")
        S.scalar.activation(out=sgd[:], in_=Cc[:, 1664:1792], func=AF.Sigmoid)
        P.transpose(sgdT[:], sgd[:], pT[0][:, 0:L], rows=L, cols=128)
        S.tensor.matmul(pL[0:L, :], lhsT=twT[:], rhs=wup[:], start=True, stop=True)
        S.vector.tensor_tensor(out=B["logw"][:], in0=pL[0:L, :], in1=w0b[0:L, :], op=ALU.add)
        S.scalar.activation(out=B["logw"][:], in_=B["logw"][:], func=AF.Sigmoid)
        S.vector.tensor_scalar(out=B["logw"][:], in0=B["logw"][:], scalar1=-0.6065306597126334, scalar2=None, op0=ALU.mult)
        S.tensor.matmul(pL[0:L, :], lhsT=adT[:], rhs=aup[:], start=True, stop=True)
        S.vector.tensor_tensor(out=B["a"][:], in0=pL[0:L, :], in1=a0b[0:L, :], op=ALU.add)
        S.scalar.activation(out=B["a"][:], in_=B["a"][:], func=AF.Sigmoid)
        S.tensor.matmul(pL[0:L, :], lhsT=sgdT[:], rhs=gup[:], start=True, stop=True)
        S.scalar.copy(out=B["g"][:], in_=pL[0:L, :])
        S.vector.tensor_tensor(out=B["kk"][:], in0=k, in1=kkb[0:L, :], op=ALU.mult)
        S.scalar.activation(out=B["sq"][:], in_=B["kk"][:], func=AF.Square)
        S.vector.reduce_sum(out=sm["ss"][:, 0:8], in_=B["sq"][:].rearrange("p (h d) -> p h d", h=8), axis=AX.X)
        S.scalar.activation(out=sm["ss"][:, 0:8], in_=sm["ss"][:, 0:8], func=AF.Sqrt, bias=eps6[0:L, 0:1], scale=1.0)
        S.vector.reciprocal(out=sm["rn"][:, 0:8], in_=sm["ss"][:, 0:8])
        for h in range(8):
            S.vector.tensor_scalar(out=B["kk"][:, h * 64:(h + 1) * 64], in0=B["kk"][:, h * 64:(h + 1) * 64],
                                   scalar1=sm["rn"][:, h:h + 1], scalar2=None, op0=ALU.mult)
        S.vector.scalar_tensor_tensor(out=B["tmp"][:], in0=B["a"][:], scalar=-1.0, in1=kab[0:L, :], op0=ALU.add, op1=ALU.mult)
        S.vector.scalar_tensor_tensor(out=B["kp"][:], in0=B["tmp"][:], scalar=1.0, in1=k, op0=ALU.add, op1=ALU.mult)
        S.vector.tensor_tensor(out=B["b"][:], in0=B["kk"][:], in1=B["a"][:], op=ALU.mult)
        S.tensor.matmul(pL[0:L, :], lhsT=btri, rhs=B["logw"][:], start=True, stop=True)
        S.scalar.activation(out=B["ecl"][:], in_=pL[0:L, :], func=AF.Exp)
        S.scalar.activation(out=B["eni"][:], in_=pL[0:L, :], func=AF.Exp, scale=-1.0)
        S.vector.tensor_tensor(out=B["tmp"][:], in0=pL[0:L, :], in1=B["logw"][:], op=ALU.subtract)
        S.scalar.activation(out=B["ecm"][:], in_=B["tmp"][:], func=AF.Exp)
        S.vector.tensor_tensor(out=B["rt"][:], in0=r, in1=B["ecl"][:], op=ALU.mult)
        S.vector.tensor_tensor(out=B["kat"][:], in0=B["kk"][:], in1=B["ecm"][:], op=ALU.mult)
        S.vector.tensor_tensor(out=B["kt"][:], in0=B["kp"][:], in1=B["eni"][:], op=ALU.mult)
        S.vector.tensor_tensor(out=B["bt"][:], in0=B["b"][:], in1=B["eni"][:], op=ALU.mult)
        S.vector.tensor_scalar(out=B["nbt"][:], in0=B["bt"][:], scalar1=-1.0, scalar2=None, op0=ALU.mult)
        for h in range(8):
            S.tensor.matmul(pL[0:64, h:h + 1], lhsT=B["logw"][:, h * 64:(h + 1) * 64], rhs=chunkind, start=True, stop=True)
        S.scalar.activation(out=PL[:], in_=pL[0:64, 0:8], func=AF.Exp)
        S.vector.tensor_tensor(out=B["tmp"][:], in0=r, in1=B["kp"][:], op=ALU.mult)
        S.vector.tensor_tensor(out=B["tmp"][:], in0=B["tmp"][:], in1=rkb[0:L, :], op=ALU.mult)
        S.vector.reduce_sum(out=sm["bs"][:, 0:8], in_=B["tmp"][:].rearrange("p (h d) -> p h d", h=8), axis=AX.X)
        def head_body(h, u):
            hs = slice(h * 64, (h + 1) * 64)
            T_ = {n: TT_[n][u] for n in TN}
            Mh = {n: M_[n][u] for n in MN}
            pTu, pAu, pIu = pT[u], pA[u], pI[u]
            c0 = u * 256
            Xu, Yu, Pu = X2[u], Y2[u], P2[u]
            P.transpose(T_["rtT"][:], B["rt"][:, hs], pTu[0:64, 0:L], rows=L, cols=64)
            yield
            P.transpose(T_["katT"][:], B["kat"][:, hs], pTu[0:64, 0:L], rows=L, cols=64, eng="scalar")
            yield
            P.transpose(T_["ktT"][:], B["kt"][:, hs], pTu[0:64, 0:L], rows=L, cols=64)
            yield
            P.transpose(T_["btT"][:], B["bt"][:, hs], pTu[0:64, 0:L], rows=L, cols=64, eng="scalar")
            yield
            S.tensor.matmul(pAu[0:L, 0:64], lhsT=T_["btT"][:], rhs=T_["katT"][:], start=True, stop=True)
            S.tensor.matmul(pAu[0:L, 64:128], lhsT=T_["katT"][:], rhs=T_["btT"][:], start=True, stop=True)
            S.tensor.matmul(pAu[0:L, 128:192], lhsT=T_["ktT"][:], rhs=T_["katT"][:], start=True, stop=True)
            S.tensor.matmul(pAu[0:L, 192:256], lhsT=T_["ktT"][:], rhs=T_["rtT"][:], start=True, stop=True)
            S.tensor.matmul(pAu[0:L, 256:320], lhsT=T_["btT"][:], rhs=T_["rtT"][:], start=True, stop=True)
            yield
            S.vector.scalar_tensor_tensor(out=Xu[0][:], in0=pAu[0:L, 0:64], scalar=-1.0, in1=bmT_s, op0=ALU.mult, op1=ALU.mult)
            S.vector.scalar_tensor_tensor(out=Yu[0][:], in0=pAu[0:L, 64:128], scalar=-1.0, in1=bm_s, op0=ALU.mult, op1=ALU.mult)
            S.vector.tensor_tensor(out=Mh["AkvT"][:], in0=pAu[0:L, 128:192], in1=bmT_s, op=ALU.mult)
            S.vector.tensor_tensor(out=Mh["BrkT"][:], in0=pAu[0:L, 192:256], in1=bmT_i, op=ALU.mult)
            S.vector.scalar_tensor_tensor(out=Mh["nBrbT"][:], in0=pAu[0:L, 256:320], scalar=-1.0, in1=bmT_i, op0=ALU.mult, op1=ALU.mult)
            S.vector.tensor_tensor(out=Pu[0][:], in0=Xu[0][:], in1=ident, op=ALU.add)
            yield
            nst = 5
            for n in range(nst):
                a_, c_ = n % 2, (n + 1) % 2
                if n < nst - 1:
                    S.tensor.matmul(pIu[0:L, 0:64], lhsT=Yu[a_][:], rhs=Xu[a_][:], start=True, stop=True)
                S.tensor.matmul(pIu[0:L, 64:128], lhsT=Xu[a_][:], rhs=Yu[a_][:], start=True, stop=True)
                yield
                if n < nst - 1:
                    S.scalar.copy(out=Xu[c_][:], in_=pIu[0:L, 0:64])
                S.vector.tensor_copy(out=Yu[c_][:], in_=pIu[0:L, 64:128])
                yield
                S.tensor.matmul(pIu[0:L, 128:192], lhsT=Yu[c_][:], rhs=Pu[a_][:], start=True, stop=True)
                yield
                S.vector.tensor_tensor(out=Pu[c_][:], in0=Pu[a_][:], in1=pIu[0:L, 128:192], op=ALU.add)
                yield
            TT = Pu[nst % 2]
            cs = slice(0, L)
            H0 = H[:, h, :]
            S.tensor.matmul(pC[cs, c0:c0 + 64], lhsT=T_["katT"][:, cs], rhs=H0, start=True, stop=False)
            S.tensor.matmul(pC[cs, c0:c0 + 64], lhsT=Mh["AkvT"][cs, cs], rhs=v[cs, hs], start=False, stop=True)
            yield
            S.vector.tensor_copy(out=Wsb[u][cs, :], in_=pC[cs, c0:c0 + 64])
            yield
            S.tensor.matmul(pC[cs, c0 + 64:c0 + 128], lhsT=TT[cs, cs], rhs=Wsb[u][cs, :], start=True, stop=True)
            yield
            S.vector.tensor_copy(out=Usb[u][cs, :], in_=pC[cs, c0 + 64:c0 + 128])
            yield
            S.tensor.matmul(pC[cs, c0 + 128:c0 + 192], lhsT=T_["rtT"][:, cs], rhs=H0, start=True, stop=False)
            S.tensor.matmul(pC[cs, c0 + 128:c0 + 192], lhsT=Mh["BrkT"][cs, cs], rhs=v[cs, hs], start=False, stop=False)
            S.tensor.matmul(pC[cs, c0 + 128:c0 + 192], lhsT=Mh["nBrbT"][cs, cs], rhs=Usb[u][cs, :], start=False, stop=True)
            S.tensor.matmul(pC[0:64, c0 + 192:c0 + 256], lhsT=B["kt"][cs, hs], rhs=v[cs, hs], start=True, stop=False)
            S.tensor.matmul(pC[0:64, c0 + 192:c0 + 256], lhsT=B["nbt"][cs, hs], rhs=Usb[u][cs, :], start=False, stop=True)
            yield
            S.scalar.copy(out=B["Y"][cs, hs], in_=pC[cs, c0 + 128:c0 + 192])
            S.vector.tensor_tensor(out=H0, in0=H0, in1=pC[0:64, c0 + 192:c0 + 256], op=ALU.add)
            S.vector.tensor_scalar(out=H0, in0=H0, scalar1=PL[:, h:h + 1], scalar2=None, op0=ALU.mult)
            yield
            layernorm(P, B["Y"][:, hs], B["Yn"][:, hs], None, None, lns2[u], D=64, eps=64e-5)

        for hp in range(4):
            gens = [head_body(2 * hp, 0), head_body(2 * hp + 1, 1)]
            while gens:
                for g_ in list(gens):
                    try:
                        next(g_)
                    except StopIteration:
                        gens.remove(g_)
        S.vector.tensor_tensor(out=B["Yn"][:], in0=B["Yn"][:], in1=lgb[0:L, :], op=ALU.mult)
        S.vector.tensor_tensor(out=B["Yn"][:], in0=B["Yn"][:], in1=lbb[0:L, :], op=ALU.add)
        for h in range(8):
            hs = slice(h * 64, (h + 1) * 64)
            S.vector.scalar_tensor_tensor(out=B["Yn"][:, hs], in0=v[:, hs], scalar=sm["bs"][:, h:h + 1], in1=B["Yn"][:, hs],
                                          op0=ALU.mult, op1=ALU.add)
        S.vector.tensor_tensor(out=B["Yn"][:], in0=B["Yn"][:], in1=B["g"][:], op=ALU.mult)
        S.gpsimd.dma_start(out=y[t0:t0 + L, :], in_=B["Yn"][:])
    S.emit()
    return P.nc
TWO_PI = 6.283185307179586


def s5_host_layout(lam_re, lam_im, log_dt, b_re, b_im, c_re, c_im, d_skip, w_glu, b_glu):
    G, Pn = 32, 64
    rows = np.stack([lam_re.reshape(-1), lam_im.reshape(-1), np.repeat(log_dt, Pn)]).astype(np.float32)
    cols = np.stack([lam_re.reshape(16, 128).T, lam_im.reshape(16, 128).T, np.repeat(log_dt, Pn).reshape(16, 128).T], 0).astype(np.float32)
    bpad = np.zeros((2, 4, 128, 512), np.float32)
    for ct in range(4):
        for gl in range(8):
            g = 8 * ct + gl
            bpad[0, ct, gl * 16:(gl + 1) * 16, gl * 64:(gl + 1) * 64] = b_re[g].T
            bpad[1, ct, gl * 16:(gl + 1) * 16, gl * 64:(gl + 1) * 64] = b_im[g].T
    cpad = np.zeros((2, 16, 128, 128), np.float32)
    for st in range(16):
        for gg in range(2):
            g = 2 * st + gg
            gl = g % 8
            cpad[0, st, gg * 64:(gg + 1) * 64, gl * 16:(gl + 1) * 16] = c_re[g].T
            cpad[1, st, gg * 64:(gg + 1) * 64, gl * 16:(gl + 1) * 16] = c_im[g].T
    dcol = np.stack([d_skip.reshape(4, 128).T, b_glu.reshape(4, 128).T], 0).astype(np.float32)
    return {"rows": rows, "cols": cols, "bpad": bpad, "cpad": cpad, "dcol": dcol, "w_glu": np.ascontiguousarray(w_glu, np.float32)}


def build_s5(S_tok, P=None):
    P = P or Prog()
    S = P.S
    z = P.din("z", [S_tok, 2056])
    rows = P.din("rows", [3, 2048])
    cols = P.din("cols", [3, 128, 16])
    bpad = P.din("bpad", [2, 4, 128, 512])
    cpad = P.din("cpad", [2, 16, 128, 128])
    dcol = P.din("dcol", [2, 128, 4])
    w_glu = P.din("w_glu", [512, 512])
    y = P.dout("y", [S_tok, 512])
    I32_ = mybir.dt.int32
    R = lambda n: P.sb(n, [128, 2048])
    lr = P.bc_row("lr", rows[0:1, :], 2048)
    li = P.bc_row("li", rows[1:2, :], 2048)
    dt = P.bc_row("dtr", rows[2:3, :], 2048)
    a_r, th_r, s1, s2, s3, s4 = R("a_r"), R("th_r"), R("s1"), R("s2"), R("s3"), R("s4")
    si = P.sb("si", [128, 2048], I32_)
    Ei_re, Ei_im = R("Ei_re"), R("Ei_im")
    iota_col, iota_row = P.C("iota_col", 128, 1), P.C("iota_row")

    def sincos(turns, o_sin, o_cos, n):
        for off, o in ((0.0, o_sin), (0.25, o_cos)):
            if off:
                S.vector.tensor_scalar(out=s4[:, 0:n], in0=turns, scalar1=off, scalar2=None, op0=ALU.add)
                src = s4[:, 0:n]
            else:
                src = turns
            S.vector.tensor_copy(out=si[:, 0:n], in_=src)
            S.vector.tensor_copy(out=o, in_=si[:, 0:n])
            S.vector.tensor_tensor(out=o, in0=src, in1=o, op=ALU.subtract)
            S.scalar.activation(out=o, in_=o, func=AF.Sin, scale=TWO_PI)

    S.scalar.activation(out=dt[:], in_=dt[:], func=AF.Exp)
    S.vector.tensor_tensor(out=a_r[:], in0=lr[:], in1=dt[:], op=ALU.mult)
    S.vector.tensor_tensor(out=th_r[:], in0=li[:], in1=dt[:], op=ALU.mult)
    S.vector.tensor_scalar(out=th_r[:], in0=th_r[:], scalar1=1.0 / TWO_PI, scalar2=None, op0=ALU.mult)
    sincos(th_r[:], s1[:], s2[:], 2048)
    S.scalar.activation(out=s3[:], in_=a_r[:], func=AF.Exp)
    S.vector.tensor_tensor(out=s1[:], in0=s1[:], in1=s3[:], op=ALU.mult)
    S.vector.tensor_tensor(out=s2[:], in0=s2[:], in1=s3[:], op=ALU.mult)
    S.vector.tensor_scalar(out=s2[:], in0=s2[:], scalar1=-1.0, scalar2=None, op0=ALU.add)
    fr, fi = R("fr"), R("fi")
    S.vector.tensor_tensor(out=s3[:], in0=lr[:], in1=lr[:], op=ALU.mult)
    S.vector.tensor_tensor(out=s4[:], in0=li[:], in1=li[:], op=ALU.mult)
    S.vector.tensor_tensor(out=s3[:], in0=s3[:], in1=s4[:], op=ALU.add)
    S.vector.reciprocal(out=s3[:], in_=s3[:])
    S.vector.tensor_tensor(out=fr[:], in0=s2[:], in1=lr[:], op=ALU.mult)
    S.vector.tensor_tensor(out=s4[:], in0=s1[:], in1=li[:], op=ALU.mult)
    S.vector.tensor_tensor(out=fr[:], in0=fr[:], in1=s4[:], op=ALU.add)
    S.vector.tensor_tensor(out=fr[:], in0=fr[:], in1=s3[:], op=ALU.mult)
    S.vector.tensor_tensor(out=fi[:], in0=s1[:], in1=lr[:], op=ALU.mult)
    S.vector.tensor_tensor(out=s4[:], in0=s2[:], in1=li[:], op=ALU.mult)
    S.vector.tensor_tensor(out=fi[:], in0=fi[:], in1=s4[:], op=ALU.subtract)
    S.vector.tensor_tensor(out=fi[:], in0=fi[:], in1=s3[:], op=ALU.mult)
    bp_re, bp_im = P.sb("bp_re", [128, 4, 512]), P.sb("bp_im", [128, 4, 512])
    for ct in range(4):
        S.sync.dma_start(out=bp_re[:, ct, :], in_=bpad[0, ct])
        S.sync.dma_start(out=bp_im[:, ct, :], in_=bpad[1, ct])
    Bb_re, Bb_im = P.sb("Bb_re", [128, 2048]), P.sb("Bb_im", [128, 2048])
    bpr, bpi = bp_re[:].rearrange("p a b -> p (a b)"), bp_im[:].rearrange("p a b -> p (a b)")
    S.vector.tensor_tensor(out=Bb_re[:], in0=fr[:], in1=bpr, op=ALU.mult)
    S.vector.tensor_tensor(out=s4[:], in0=fi[:], in1=bpi, op=ALU.mult)
    S.vector.tensor_tensor(out=Bb_re[:], in0=Bb_re[:], in1=s4[:], op=ALU.subtract)
    S.vector.tensor_tensor(out=Bb_im[:], in0=fr[:], in1=bpi, op=ALU.mult)
    S.vector.tensor_tensor(out=s4[:], in0=fi[:], in1=bpr, op=ALU.mult)
    S.vector.tensor_tensor(out=Bb_im[:], in0=Bb_im[:], in1=s4[:], op=ALU.add)
    S.vector.tensor_scalar(out=s3[:], in0=th_r[:], scalar1=iota_col, scalar2=None, op0=ALU.mult)
    sincos(s3[:], s1[:], s2[:], 2048)
    S.vector.tensor_scalar(out=s3[:], in0=a_r[:], scalar1=iota_col, scalar2=-1.0, op0=ALU.mult, op1=ALU.mult)
    S.scalar.activation(out=s3[:], in_=s3[:], func=AF.Exp)
    S.vector.tensor_tensor(out=Ei_re[:], in0=s2[:], in1=s3[:], op=ALU.mult)
    S.vector.scalar_tensor_tensor(out=Ei_im[:], in0=s1[:], scalar=-1.0, in1=s3[:], op0=ALU.mult, op1=ALU.mult)
    EL_re, EL_im = fr, fi
    S.vector.tensor_scalar(out=s3[:], in0=th_r[:], scalar1=128.0, scalar2=None, op0=ALU.mult)
    sincos(s3[:], s1[:], s2[:], 2048)
    S.scalar.activation(out=s3[:], in_=a_r[:], func=AF.Exp, scale=128.0)
    S.vector.tensor_tensor(out=EL_re[:], in0=s2[:], in1=s3[:], op=ALU.mult)
    S.vector.tensor_tensor(out=EL_im[:], in0=s1[:], in1=s3[:], op=ALU.mult)
    cl = P.sb("cl", [128, 3, 16])
    for i in range(3):
        S.sync.dma_start(out=cl[:, i, :], in_=cols[i])
    S.scalar.activation(out=cl[:, 2, :], in_=cl[:, 2, :], func=AF.Exp)
    S.vector.tensor_tensor(out=cl[:, 0, :], in0=cl[:, 0, :], in1=cl[:, 2, :], op=ALU.mult)
    S.vector.tensor_tensor(out=cl[:, 1, :], in0=cl[:, 1, :], in1=cl[:, 2, :], op=ALU.mult)
    S.vector.tensor_scalar(out=cl[:, 1, :], in0=cl[:, 1, :], scalar1=1.0 / TWO_PI, scalar2=None, op0=ALU.mult)
    E_re, E_im = P.sb("E_re", [128, 16, 128]), P.sb("E_im", [128, 16, 128])
    for st in range(16):
        S.vector.tensor_scalar(out=s3[:, 0:128], in0=iota_row, scalar1=cl[:, 1, st:st + 1], scalar2=None, op0=ALU.mult)
        sincos(s3[:, 0:128], s1[:, 0:128], s2[:, 0:128], 128)
        S.scalar.activation(out=s3[:, 0:128], in_=iota_row, func=AF.Exp, scale=cl[:, 0, st:st + 1])
        S.vector.tensor_tensor(out=E_re[:, st, :], in0=s2[:, 0:128], in1=s3[:, 0:128], op=ALU.mult)
        S.vector.tensor_tensor(out=E_im[:, st, :], in0=s1[:, 0:128], in1=s3[:, 0:128], op=ALU.mult)
    cp_re, cp_im = P.sb("cp_re", [128, 16, 128]), P.sb("cp_im", [128, 16, 128])
    for st in range(16):
        S.sync.dma_start(out=cp_re[:, st, :], in_=cpad[0, st])
        S.sync.dma_start(out=cp_im[:, st, :], in_=cpad[1, st])
    dc = P.sb("dc", [128, 2, 4])
    for i in range(2):
        S.sync.dma_start(out=dc[:, i, :], in_=dcol[i])
    wg = P.sb("wg", [128, 4, 512])
    for k in range(4):
        S.sync.dma_start(out=wg[:, k, :], in_=w_glu[k * 128:(k + 1) * 128, :])
    Xr_re, Xr_im, Xn_re, Xn_im, Xt = lr[0:1, :], li[0:1, :], dt[0:1, :], th_r[0:1, :], bpr[0:1, :]
    S.vector.memset(Xr_re, 0.0)
    S.vector.memset(Xr_im, 0.0)
    ut = P.rot("ut", [128, 512])
    uT = P.rot("uT", [128, 4, 128])
    V_re, V_im = s1, s2
    x_re, nx_im = s3, a_r
    tA, tB = s4[:, 0:512], s4[:, 512:1024]
    gl = P.sb("gl", [128, 4, 128])
    g1, g2 = P.sb("g1", [128, 128]), P.sb("g2", [128, 128])
    yaT = P.sb("yaT", [128, 4, 128])
    yt = ut
    pT = [S.ps(f"pT{i}", [128, 512]) for i in range(2)]
    pB = [S.ps(f"pB{i}", [128, 512]) for i in range(2)]
    pS_ = [S.ps(f"pS{i}", [128, 512]) for i in range(2)]
    pY = S.ps("pY", [128, 512])
    pX = S.ps("pX", [128, 512])
    ident, tri, ones = P.C("ident"), P.C("tri"), P.C("ones")
    for t in range(S_tok // 128):
        t0 = t * 128
        b = t % 2
        S.sync.dma_start(out=ut[b][:], in_=z[t0:t0 + 128, 0:512])
        for k in range(4):
            P.transpose(uT[b][:, k, :], ut[b][:, k * 128:(k + 1) * 128], pT[k % 2][:, 0:128], eng="vector" if k % 2 == 0 else "scalar")
        for ct in range(4):
            cs = slice(ct * 512, (ct + 1) * 512)
            S.tensor.matmul(pB[0][:], lhsT=uT[b][:, ct, :], rhs=Bb_re[:, cs], start=True, stop=True)
            S.tensor.matmul(pB[1][:], lhsT=uT[b][:, ct, :], rhs=Bb_im[:, cs], start=True, stop=True)
            S.vector.tensor_tensor(out=V_re[:, cs], in0=pB[0][:], in1=Ei_re[:, cs], op=ALU.mult)
            S.vector.tensor_tensor(out=tA, in0=pB[1][:], in1=Ei_im[:, cs], op=ALU.mult)
            S.vector.tensor_tensor(out=V_re[:, cs], in0=V_re[:, cs], in1=tA, op=ALU.subtract)
            S.vector.tensor_tensor(out=V_im[:, cs], in0=pB[0][:], in1=Ei_im[:, cs], op=ALU.mult)
            S.vector.tensor_tensor(out=tB, in0=pB[1][:], in1=Ei_re[:, cs], op=ALU.mult)
            S.vector.tensor_tensor(out=V_im[:, cs], in0=V_im[:, cs], in1=tB, op=ALU.add)
        S.vector.tensor_tensor(out=V_re[0:1, :], in0=V_re[0:1, :], in1=Xr_re, op=ALU.add)
        S.vector.tensor_tensor(out=V_im[0:1, :], in0=V_im[0:1, :], in1=Xr_im, op=ALU.add)
        for q in range(4):
            S.tensor.matmul(pX[:, :], lhsT=ones, rhs=V_re[:, q * 512:(q + 1) * 512], start=True, stop=True)
            S.vector.tensor_copy(out=Xn_re[:, q * 512:(q + 1) * 512], in_=pX[0:1, :])
            S.tensor.matmul(pX[:, :], lhsT=ones, rhs=V_im[:, q * 512:(q + 1) * 512], start=True, stop=True)
            S.vector.tensor_copy(out=Xn_im[:, q * 512:(q + 1) * 512], in_=pX[0:1, :])
        S.vector.tensor_tensor(out=Xr_re, in0=Xn_re, in1=EL_re[0:1, :], op=ALU.mult)
        S.vector.tensor_tensor(out=Xt, in0=Xn_im, in1=EL_im[0:1, :], op=ALU.mult)
        S.vector.tensor_tensor(out=Xr_re, in0=Xr_re, in1=Xt, op=ALU.subtract)
        S.vector.tensor_tensor(out=Xr_im, in0=Xn_re, in1=EL_im[0:1, :], op=ALU.mult)
        S.vector.tensor_tensor(out=Xt, in0=Xn_im, in1=EL_re[0:1, :], op=ALU.mult)
        S.vector.tensor_tensor(out=Xr_im, in0=Xr_im, in1=Xt, op=ALU.add)
        for ft in range(4):
            for q in range(4):
                st = ft * 4 + q
                S.tensor.matmul(pS_[0][:, q * 128:(q + 1) * 128], lhsT=V_re[:, st * 128:(st + 1) * 128], rhs=tri, start=True, stop=True)
                S.tensor.matmul(pS_[1][:, q * 128:(q + 1) * 128], lhsT=V_im[:, st * 128:(st + 1) * 128], rhs=tri, start=True, stop=True)
            cs = slice(ft * 512, (ft + 1) * 512)
            Er = E_re[:, ft * 4:(ft + 1) * 4, :].rearrange("p a b -> p (a b)")
            Em = E_im[:, ft * 4:(ft + 1) * 4, :].rearrange("p a b -> p (a b)")
            S.vector.tensor_tensor(out=x_re[:, cs], in0=pS_[0][:], in1=Er, op=ALU.mult)
            S.vector.tensor_tensor(out=tA, in0=pS_[1][:], in1=Em, op=ALU.mult)
            S.vector.tensor_tensor(out=x_re[:, cs], in0=x_re[:, cs], in1=tA, op=ALU.subtract)
            S.vector.tensor_tensor(out=nx_im[:, cs], in0=pS_[1][:], in1=Er, op=ALU.mult)
            S.vector.tensor_tensor(out=tB, in0=pS_[0][:], in1=Em, op=ALU.mult)
            S.vector.scalar_tensor_tensor(out=nx_im[:, cs], in0=nx_im[:, cs], scalar=-1.0, in1=tB, op0=ALU.mult, op1=ALU.subtract)
            for q in range(4):
                st = ft * 4 + q
                S.tensor.matmul(pY[:, 0:128], lhsT=cp_re[:, st, :], rhs=x_re[:, st * 128:(st + 1) * 128], start=(q == 0), stop=False)
                S.tensor.matmul(pY[:, 0:128], lhsT=cp_im[:, st, :], rhs=nx_im[:, st * 128:(st + 1) * 128], start=False, stop=(q == 3))
            S.vector.scalar_tensor_tensor(out=g1[:], in0=uT[b][:, ft, :], scalar=dc[:, 0, ft:ft + 1], in1=pY[:, 0:128], op0=ALU.mult, op1=ALU.add)
            S.vector.tensor_tensor(out=g2[:], in0=g1[:], in1=g1[:], op=ALU.mult)
            S.vector.tensor_scalar(out=g2[:], in0=g2[:], scalar1=0.044715, scalar2=1.0, op0=ALU.mult, op1=ALU.add)
            S.vector.tensor_tensor(out=g2[:], in0=g2[:], in1=g1[:], op=ALU.mult)
            S.scalar.activation(out=g2[:], in_=g2[:], func=AF.Tanh, scale=0.7978845608028654)
            S.vector.scalar_tensor_tensor(out=g2[:], in0=g2[:], scalar=1.0, in1=g1[:], op0=ALU.add, op1=ALU.mult)
            S.vector.tensor_scalar(out=gl[:, ft, :], in0=g2[:], scalar1=0.5, scalar2=None, op0=ALU.mult)
        for f2 in range(4):
            for ft in range(4):
                S.tensor.matmul(pY[:, 128:256], lhsT=wg[:, ft, f2 * 128:(f2 + 1) * 128], rhs=gl[:, ft, :], start=(ft == 0), stop=(ft == 3))
            S.scalar.activation(out=g1[:], in_=pY[:, 128:256], func=AF.Sigmoid, bias=dc[:, 1, f2:f2 + 1], scale=1.0)
            S.vector.tensor_tensor(out=yaT[:, f2, :], in0=g1[:], in1=gl[:, f2, :], op=ALU.mult)
        for k in range(4):
            P.transpose(yt[b][:, k * 128:(k + 1) * 128], yaT[:, k, :], pT[k % 2][:, 0:128], eng="vector" if k % 2 == 0 else "scalar")
        S.gpsimd.dma_start(out=y[t0:t0 + 128, :], in_=yt[b][:])
    S.emit()
    return P.nc
def _stage(nc, idx, fn, *args, io):
    with nc.cleanup_on_exit():
        P = Prog(nc=nc, prefix=f"s{idx}_", io=io)
        fn(*args, P=P)
        nc.all_engine_barrier()


def build_fused(S_tok, shapes):
    nc = bass.Bass("TRN2", target_bir_lowering=False)

    def ein(name):
        return nc.dram_tensor(name, list(shapes[name]), F32, kind="ExternalInput").ap()

    def internal(name, shape):
        return nc.dram_tensor(name, list(shape), F32).ap()

    consts = ein("consts")
    xa = ein("x")
    out = nc.dram_tensor("out", [S_tok, 1024], F32, kind="ExternalOutput").ap()
    sid = 0
    for layer in range(4):
        L = f"L{layer}_"
        xo = out if layer == 3 else internal(f"xact{layer}", [S_tok, 1024])
        ya = internal(f"ya{layer}", [S_tok, 512])
        yb = internal(f"yb{layer}", [S_tok, 512])
        if layer % 2 == 0:
            z = internal(f"z{layer}", [S_tok, 2056])
            _stage(nc, sid, build_proj, S_tok, 2056, io={"consts": consts, "x": xa, "w": ein(L + "w_in"), "z": z}); sid += 1
            io = {"consts": consts, "z": z, "y": ya}
            for k in ("rows", "cols", "bpad", "cpad", "dcol", "w_glu"):
                io[k] = ein(L + k)
            _stage(nc, sid, build_s5, S_tok, io=io); sid += 1
            _stage(nc, sid, build_mlstm, S_tok, io={"consts": consts, "z": z, "y": yb, "gb": ein(L + "gb"), "ng": ein(L + "ng")}); sid += 1
            io = {"consts": consts, "x": xa, "ya": ya, "yb": yb, "xo": xo}
            for k in ("w_out", "lnp", "wg", "wu", "wd"):
                io[k] = ein(L + k)
            _stage(nc, sid, build_cf, S_tok, 1, 2816, io=io); sid += 1
        else:
            z = internal(f"z{layer}", [S_tok, 3848])
            _stage(nc, sid, build_proj, S_tok, 3848, io={"consts": consts, "x": xa, "w": ein(L + "w_in"), "z": z}); sid += 1
            io = {"consts": consts, "z": z, "y": ya}
            for k in ("vecs", "mu", "w_up", "a_up", "g_up"):
                io[k] = ein(L + k)
            _stage(nc, sid, build_rwkv, S_tok, io=io); sid += 1
            io = {"consts": consts, "z": z, "y": yb}
            for k in ("conv_w", "a_log", "dt_bias", "norm_g"):
                io[k] = ein(L + k)
            _stage(nc, sid, build_gdn, S_tok, 99, io=io); sid += 1
            io = {"consts": consts, "x": xa, "ya": ya, "yb": yb, "xo": xo}
            for k in ("w_out", "lnp", "wg", "wu", "wd", "router"):
                io[k] = ein(L + k)
            _stage(nc, sid, build_cf, S_tok, 8, 3584, io=io); sid += 1
        xa = xo
    return nc


def host_inputs(inp):
    W = {"consts": CONST_ARR}
    for layer in range(4):
        i = layer // 2
        L = f"L{layer}_"
        W[L + "lnp"] = np.stack([inp["ln_g"][layer, 0], inp["ln_b"][layer, 0], inp["ln_g"][layer, 1], inp["ln_b"][layer, 1]])
        if layer % 2 == 0:
            W[L + "w_in"] = inp["ev_w_in"][i]
            lay = s5_host_layout(inp["s5_lam_re"][i], inp["s5_lam_im"][i], inp["s5_log_dt"][i], inp["s5_b_re"][i], inp["s5_b_im"][i],
                                 inp["s5_c_re"][i], inp["s5_c_im"][i], inp["s5_d"][i], inp["s5_w_glu"][i], inp["s5_b_glu"][i])
            for k, v in lay.items():
                W[L + k] = v
            W[L + "gb"] = inp["ml_gate_bias"][i][None]
            W[L + "ng"] = inp["ml_norm_g"][i][None]
            W[L + "w_out"] = inp["ev_w_out"][i]
            W[L + "wg"] = inp["ffn_w_gate"][i][None]
            W[L + "wu"] = inp["ffn_w_up"][i][None]
            W[L + "wd"] = inp["ffn_w_down"][i][None]
        else:
            W[L + "w_in"] = inp["od_w_in"][i]
            W[L + "vecs"] = np.stack([inp["rw_w0"][i], inp["rw_a0"][i], inp["rw_k_k"][i], inp["rw_k_a"][i], inp["rw_r_k"][i],
                                      inp["rw_ln_g"][i], inp["rw_ln_b"][i], inp["rw_ln_b"][i]])
            W[L + "mu"] = inp["rw_mu"][i][None]
            W[L + "w_up"] = inp["rw_w_up"][i]
            W[L + "a_up"] = inp["rw_a_up"][i]
            W[L + "g_up"] = inp["rw_g_up"][i]
            W[L + "conv_w"] = inp["gd_conv"][i]
            W[L + "a_log"] = inp["gd_a_log"][i][None]
            W[L + "dt_bias"] = inp["gd_dt_bias"][i][None]
            W[L + "norm_g"] = inp["gd_norm_g"][i][None]
            W[L + "w_out"] = inp["od_w_out"][i]
            W[L + "wg"] = inp["moe_w_gate"][i]
            W[L + "wu"] = inp["moe_w_up"][i]
            W[L + "wd"] = inp["moe_w_down"][i]
            W[L + "router"] = inp["moe_router"][i]
    return {k: np.ascontiguousarray(v, dtype=np.float32) for k, v in W.items()}


def kernel(_n_cores=8, **inp):
    inp = {k: np.asarray(v, dtype=np.float32) for k, v in inp.items()}
    S_tok = inp["x"].shape[1]
    W = host_inputs(inp)
    shapes = {k: v.shape for k, v in W.items()}
    shapes["x"] = (S_tok, 1024)
    nc = build_fused(S_tok, shapes)
    in_maps = []
    for c in range(_n_cores):
        d = dict(W)
        d["x"] = np.ascontiguousarray(inp["x"][c])
        in_maps.append(d)
    res = run_bass_kernel_spmd(nc, in_maps, core_ids=list(range(_n_cores)))
    return np.stack([np.asarray(r["out"]) for r in res.results]).astype(np.float32)
```

```python
import numpy as np
import concourse.bass as bass
import concourse.mybir as mybir
from concourse.bass_utils import run_bass_kernel_spmd

F32 = mybir.dt.float32
BF16 = mybir.dt.bfloat16
I32 = mybir.dt.int32
AF = mybir.ActivationFunctionType
ALU = mybir.AluOpType
AX = mybir.AxisListType

ENGS = ["tensor", "vector", "scalar", "gpsimd", "sync"]
WRITE_KW = ("out", "accum_out", "out_max", "out_indices")
SEM_LIMIT = 30000
N_DMA_SEMS = 12


def _is_ap(x):
    return hasattr(x, "tensor") and hasattr(x, "ap")


class _EngProxy:
    def __init__(self, sched, eng):
        self._s = sched
        self._e = eng

    def __getattr__(self, meth):
        def call(*args, **kwargs):
            return self._s._record(self._e, meth, args, kwargs)
        return call


class Sched:
    def __init__(self, nc, same_engine_sync=True, prefix=""):
        self.nc = nc
        self.prefix = prefix
        self.ops = []
        self.same_engine_sync = same_engine_sync
        self.tensor = _EngProxy(self, "tensor")
        self.vector = _EngProxy(self, "vector")
        self.scalar = _EngProxy(self, "scalar")
        self.gpsimd = _EngProxy(self, "gpsimd")
        self.sync = _EngProxy(self, "sync")
        self._ctx = []

    def sb(self, name, shape, dtype=F32):
        g = self.nc.sbuf_tensor(self.prefix + name, list(shape), dtype)
        t = g.__enter__()
        self._ctx.append(g)
        return t

    def ps(self, name, shape, dtype=F32):
        g = self.nc.psum_tensor(self.prefix + name, list(shape), dtype)
        t = g.__enter__()
        self._ctx.append(g)
        return t

    @staticmethod
    def _box(a):
        name = a.tensor.name
        apl = a.ap
        off = a.offset
        if "DRam" in type(a.tensor).__name__:
            ext = sum(st * (c - 1) for st, c in apl) + 1
            return (name, 0, 1, off, off + ext)
        if "PSum" in type(a.tensor).__name__:
            return (name, 0, 128, 0, 1 << 30)
        row = apl[0][0]
        if row <= 0:
            row = 1 << 30
        p0 = off // row
        f0 = off % row
        ext = sum(st * (c - 1) for st, c in apl[1:]) + 1
        return (name, p0, p0 + apl[0][1], f0, f0 + ext)

    def _record(self, eng, meth, args, kwargs):
        reads, writes = [], []
        extra_r = kwargs.pop("_reads", None)
        extra_w = kwargs.pop("_writes", None)
        for i, a in enumerate(args):
            if _is_ap(a):
                (writes if i == 0 else reads).append(self._box(a))
        for k, a in kwargs.items():
            if _is_ap(a):
                (writes if k in WRITE_KW else reads).append(self._box(a))
        if extra_r:
            reads += [self._box(a) for a in extra_r]
        if extra_w:
            writes += [self._box(a) for a in extra_w]
        for bx in list(reads):
            if bx[4] == (1 << 30) and bx not in writes:
                writes.append(bx)
        is_dma = meth in ("dma_start", "dma_start_transpose", "indirect_dma_start")
        self.ops.append(dict(eng=eng, meth=meth, args=args, kwargs=kwargs,
                             reads=reads, writes=writes, dma=is_dma))
        return len(self.ops) - 1

    def emit(self):
        nc = self.nc
        ops = self.ops
        n = len(ops)
        W = {}
        R = {}
        deps = [None] * n

        def ov(a, b):
            return a[1] < b[2] and b[1] < a[2] and a[3] < b[4] and b[3] < a[4]

        def inside(a, b):
            return a[1] >= b[1] and a[2] <= b[2] and a[3] >= b[3] and a[4] <= b[4]

        for i, op in enumerate(ops):
            d = set()
            for bx in op["reads"]:
                for (b2, j) in W.get(bx[0], ()):
                    if ov(bx, b2):
                        d.add(j)
            for bx in op["writes"]:
                for (b2, j) in W.get(bx[0], ()):
                    if ov(bx, b2):
                        d.add(j)
                for (b2, j) in R.get(bx[0], ()):
                    if ov(bx, b2):
                        d.add(j)
            d.discard(i)
            deps[i] = d
            for bx in op["writes"]:
                nm = bx[0]
                W[nm] = [(b2, j) for (b2, j) in W.get(nm, ()) if not inside(b2, bx)] + [(bx, i)]
                R[nm] = [(b2, j) for (b2, j) in R.get(nm, ()) if not inside(b2, bx)]
            for bx in op["reads"]:
                nm = bx[0]
                lst = [(b2, j) for (b2, j) in R.get(nm, ()) if not (b2 == bx and ops[j]["eng"] == op["eng"] and not ops[j]["dma"])]
                lst.append((bx, i))
                R[nm] = lst
        needed = [False] * n
        for i, op in enumerate(ops):
            keep = set()
            for j in deps[i]:
                pj = ops[j]
                if pj["eng"] == op["eng"] and not pj["dma"]:
                    if op["eng"] == "tensor" and not op["dma"]:
                        continue
                    if op["eng"] == "sync":
                        continue
                    if not self.same_engine_sync and not op["dma"]:
                        continue
                keep.add(j)
            deps[i] = keep
            for j in keep:
                needed[j] = True
        sems = {}
        self._semguards = []

        def new_sem(name):
            return nc.alloc_semaphore(name=self.prefix + name)

        eng_sem = {e: new_sem(f"s_{e}_0") for e in ENGS}
        eng_cnt = {e: 0 for e in ENGS}
        eng_gen = {e: 0 for e in ENGS}
        dma_sems = {e: [new_sem(f"d_{e}_{i}") for i in range(N_DMA_SEMS)] for e in ("sync", "gpsimd", "scalar")}
        dma_cnt = {e: [0] * N_DMA_SEMS for e in dma_sems}
        dma_rr = {e: 0 for e in dma_sems}
        dma_last_tok = {e: [None] * N_DMA_SEMS for e in dma_sems}
        token = [None] * n
        prewait = [None] * n
        for i, op in enumerate(ops):
            e = op["eng"]
            if op["dma"]:
                r = dma_rr[e]
                dma_rr[e] = (r + 1) % N_DMA_SEMS
                prewait[i] = dma_last_tok[e][r]
                dma_cnt[e][r] += 16
                if dma_cnt[e][r] > SEM_LIMIT:
                    dma_sems[e][r] = new_sem(f"d_{e}_{r}_{i}")
                    dma_cnt[e][r] = 16
                    prewait[i] = dma_last_tok[e][r]
                token[i] = (dma_sems[e][r], dma_cnt[e][r])
                dma_last_tok[e][r] = token[i]
            elif needed[i]:
                if eng_cnt[e] >= SEM_LIMIT:
                    eng_gen[e] += 1
                    eng_sem[e] = new_sem(f"s_{e}_{eng_gen[e]}")
                    eng_cnt[e] = 0
                eng_cnt[e] += 1
                token[i] = (eng_sem[e], eng_cnt[e])
        final_tokens = []
        for e in dma_sems:
            for t in dma_last_tok[e]:
                if t is not None:
                    final_tokens.append(t)
        per_eng = {e: [] for e in ENGS}
        for i, op in enumerate(ops):
            per_eng[op["eng"]].append(i)
        self.n_waits = 0
        sched = self

        def run_engine(ename, eobj):
            seen = {}
            def wait(tok):
                s, v = tok
                key = id(s)
                if seen.get(key, 0) >= v:
                    return
                seen[key] = v
                eobj.wait_ge(s, v)
                sched.n_waits += 1
            for i in per_eng[ename]:
                op = ops[i]
                for j in sorted(deps[i]):
                    wait(token[j])
                if prewait[i] is not None:
                    wait(prewait[i])
                ins = getattr(eobj, op["meth"])(*op["args"], **op["kwargs"])
                if token[i] is not None:
                    ins.then_inc(token[i][0], 16 if op["dma"] else 1)
            if ename == "sync":
                for t in final_tokens:
                    wait(t)

        with nc.Block() as block:
            @block.tensor
            def _(e):
                run_engine("tensor", e)

            @block.vector
            def _(e):
                run_engine("vector", e)

            @block.scalar
            def _(e):
                run_engine("scalar", e)

            @block.gpsimd
            def _(e):
                run_engine("gpsimd", e)

            @block.sync
            def _(e):
                run_engine("sync", e)
        return nc
DN_ALPHA = 8 ** 0.25
LN_EPS = 1e-5


def make_consts():
    c = {}
    i = np.arange(128)
    c["ident"] = np.eye(128, dtype=np.float32)
    c["tri"] = (i[:, None] <= i[None, :]).astype(np.float32)
    c["ones"] = np.ones((128, 128), np.float32)
    c["negT"] = np.where(i[:, None] <= i[None, :], 0.0, -30000.0).astype(np.float32)
    c["neg"] = np.where(i[:, None] >= i[None, :], 0.0, -30000.0).astype(np.float32)
    c["mT_strict"] = (i[:, None] < i[None, :]).astype(np.float32)
    c["m_strict"] = (i[:, None] > i[None, :]).astype(np.float32)
    blk = (i[:, None] // 64) == (i[None, :] // 64)
    c["btri"] = ((i[:, None] <= i[None, :]) & blk).astype(np.float32)
    c["bmT_incl"] = ((i[:, None] <= i[None, :]) & blk).astype(np.float32)
    c["bmT_strict"] = ((i[:, None] < i[None, :]) & blk).astype(np.float32)
    c["bm_strict"] = ((i[:, None] > i[None, :]) & blk).astype(np.float32)
    ci = np.zeros((128, 128), np.float32)
    ci[:64, 0] = 1.0
    ci[64:, 1] = 1.0
    c["chunkind"] = ci
    c["iota_row"] = np.tile(i[None, :].astype(np.float32), (128, 1))
    c["iota_col"] = np.tile(i[:, None].astype(np.float32), (1, 128))
    names = list(c.keys())
    arr = np.concatenate([c[k] for k in names], axis=1)
    offs = {k: n * 128 for n, k in enumerate(names)}
    return arr, offs


CONST_ARR, CONST_OFF = make_consts()


class Prog:
    def __init__(self, nc=None, prefix="", io=None):
        self.nc = nc if nc is not None else bass.Bass("TRN2", target_bir_lowering=False)
        self.io = io or {}
        self.S = Sched(self.nc, prefix=prefix)
        self.cd = self.din("consts", CONST_ARR.shape)
        self.csb = self.S.sb("csb", CONST_ARR.shape)
        self.S.sync.dma_start(out=self.csb[:], in_=self.cd)
        self._n = 0

    def din(self, name, shape):
        if name in self.io:
            return self.io[name]
        return self.nc.dram_tensor(name, list(shape), F32, kind="ExternalInput").ap()

    def dout(self, name, shape):
        if name in self.io:
            return self.io[name]
        return self.nc.dram_tensor(name, list(shape), F32, kind="ExternalOutput").ap()

    def C(self, name, rows=128, cols=128):
        o = CONST_OFF[name]
        return self.csb[0:rows, o:o + cols]

    def sb(self, name, shape, dtype=F32):
        return self.S.sb(name, shape, dtype)

    def rot(self, name, shape, n=2):
        return [self.S.sb(f"{name}_{i}", shape) for i in range(n)]

    def bc_row(self, name, dram_row, n):
        t = self.S.sb(name, [128, n])
        self.S.sync.dma_start(out=t[:], in_=dram_row.partition_broadcast(128))
        return t

    def transpose(self, dst, src, ps, rows=128, cols=128, eng="vector"):
        S = self.S
        S.tensor.transpose(out=ps, in_=src, identity=self.C("ident", rows, rows))
        if eng == "vector":
            S.vector.tensor_copy(out=dst, in_=ps)
        else:
            S.scalar.copy(out=dst, in_=ps)


def layernorm(P, src, dst, g_bc, b_bc, scr, D=1024, eps=LN_EPS):
    S = P.S
    n = src.shape[0]
    st = {k: v[0:n, :] for k, v in scr.items()}
    S.vector.reduce_sum(out=st["s1"], in_=src, axis=AX.X)
    S.vector.tensor_scalar(out=st["mean"], in0=st["s1"], scalar1=-1.0 / D, scalar2=None, op0=ALU.mult)
    S.vector.tensor_scalar(out=dst, in0=src, scalar1=st["mean"][:, 0:1], scalar2=None, op0=ALU.add)
    S.scalar.activation(out=st["sq"][:, 0:D], in_=dst, func=AF.Square, accum_out=st["ss"])
    S.scalar.activation(out=st["std"], in_=st["ss"], func=AF.Sqrt, bias=st["eps"][:, 0:1], scale=1.0 / D)
    S.vector.reciprocal(out=st["rstd"], in_=st["std"])
    if g_bc is not None:
        S.vector.scalar_tensor_tensor(out=dst, in0=dst, scalar=st["rstd"][:, 0:1], in1=g_bc, op0=ALU.mult, op1=ALU.mult)
        S.vector.tensor_tensor(out=dst, in0=dst, in1=b_bc, op=ALU.add)
    else:
        S.vector.tensor_scalar(out=dst, in0=dst, scalar1=st["rstd"][:, 0:1], scalar2=None, op0=ALU.mult)


def ln_scratch(P, name, D=1024, eps=LN_EPS):
    st = {k: P.sb(f"{name}_{k}", [128, 1]) for k in ("s1", "mean", "ss", "std", "rstd", "eps")}
    st["sq"] = P.sb(f"{name}_sq", [128, D])
    P.S.vector.memset(st["eps"][:], eps)
    return st


def build_proj(S_tok, C, P=None):
    P = P or Prog()
    S = P.S
    x = P.din("x", [S_tok, 1024])
    w = P.din("w", [1024, C])
    z = P.dout("z", [S_tok, C])
    wsb = P.sb("wsb", [128, 8, C], BF16)
    for k in range(8):
        S.gpsimd.dma_start(out=wsb[:, k, :], in_=w[k * 128:(k + 1) * 128, :])
    nt = S_tok // 128
    xt = P.rot("xt", [128, 1024])
    xT = [P.sb(f"xT_{i}", [128, 8, 128], BF16) for i in range(2)]
    zt = P.rot("zt", [128, C])
    pst = [S.ps(f"pst{i}", [128, 512]) for i in range(2)]
    psm = [S.ps(f"psm{i}", [128, 512]) for i in range(4)]
    chunks = [(c0, min(512, C - c0)) for c0 in range(0, C, 512)]
    for t in range(nt):
        xs = xt[t % 2]
        S.sync.dma_start(out=xs[:], in_=x[t * 128:(t + 1) * 128, :])
        for k in range(8):
            P.transpose(xT[t % 2][:, k, :], xs[:, k * 128:(k + 1) * 128], pst[k % 2][:, 0:128],
                        eng="vector" if k % 2 == 0 else "scalar")
        for ci, (c0, cw) in enumerate(chunks):
            ps = psm[ci % 4]
            for k in range(8):
                S.tensor.matmul(ps[:, 0:cw], lhsT=xT[t % 2][:, k, :], rhs=wsb[:, k, c0:c0 + cw],
                                start=(k == 0), stop=(k == 7))
            if ci % 2 == 0:
                S.vector.tensor_copy(out=zt[t % 2][:, c0:c0 + cw], in_=ps[:, 0:cw])
            else:
                S.scalar.copy(out=zt[t % 2][:, c0:c0 + cw], in_=ps[:, 0:cw])
        S.sync.dma_start(out=z[t * 128:(t + 1) * 128, :], in_=zt[t % 2][:])
    S.emit()
    return P.nc
def build_cf(S_tok, n_exp, d_ff, P=None):
    P = P or Prog()
    S = P.S
    x = P.din("x", [S_tok, 1024])
    ya = P.din("ya", [S_tok, 512])
    yb = P.din("yb", [S_tok, 512])
    w_out = P.din("w_out", [1024, 1024])
    lnp = P.din("lnp", [4, 1024])
    wg = P.din("wg", [n_exp, 1024, d_ff])
    wu = P.din("wu", [n_exp, 1024, d_ff])
    wd = P.din("wd", [n_exp, d_ff, 1024])
    if n_exp > 1:
        router = P.din("router", [1024, 8])
    xo = P.dout("xo", [S_tok, 1024])
    TB = 1024 if S_tok >= 1024 else S_tok
    HB = min(512, TB)
    NT = TB // 128
    wo_sb = P.sb("wo_sb", [128, 8, 1024], BF16)
    for k in range(8):
        S.gpsimd.dma_start(out=wo_sb[:, k, :], in_=w_out[k * 128:(k + 1) * 128, :])
    g0 = P.bc_row("g0", lnp[0:1, :], 1024)
    b0 = P.bc_row("b0", lnp[1:2, :], 1024)
    g1 = P.bc_row("g1", lnp[2:3, :], 1024)
    b1 = P.bc_row("b1", lnp[3:4, :], 1024)
    if n_exp > 1:
        r_sb = P.sb("r_sb", [128, 8, 8])
        for k in range(8):
            S.sync.dma_start(out=r_sb[:, k, :], in_=router[k * 128:(k + 1) * 128, :])
    lns = ln_scratch(P, "lns")
    xt = P.rot("xt", [128, 1024])
    yt = P.rot("yt", [128, 1024])
    yT = [P.sb(f"yT_{i}", [128, 8, 128], BF16) for i in range(2)]
    x1t = P.rot("x1t", [128, 1024])
    x1T = P.sb("x1T", [128, 8, TB], BF16)
    x1Tf = P.rot("x1Tf", [128, 8, 128])
    facc = P.sb("facc", [128, NT, 1024])
    comb = P.sb("comb", [128, NT, 8])
    FC = 512
    chunks = [(f0, min(FC, d_ff - f0)) for f0 in range(0, d_ff, FC)]
    wg_sb = [P.sb(f"wg_sb_{i}", [128, 8, FC], BF16) for i in range(2)]
    wu_sb = [P.sb(f"wu_sb_{i}", [128, 8, FC], BF16) for i in range(2)]
    wd_sb = [P.sb(f"wd_sb_{i}", [128, FC // 128, 1024], BF16) for i in range(2)]
    hT = [P.sb(f"hT_{i}", [128, FC // 128, TB], BF16) for i in range(2)]
    sg = P.rot("sg", [128, HB])
    pst = [S.ps(f"pst{i}", [128, 512]) for i in range(2)]
    pg = [S.ps(f"pg{i}", [128, 512]) for i in range(2)]
    pu = [S.ps(f"pu{i}", [128, 512]) for i in range(2)]
    pd = [S.ps(f"pd{i}", [128, 512]) for i in range(2)]
    sm = {k: P.sb(f"sm_{k}", [128, 8]) for k in ("lg", "m1", "mk1", "l2", "m2", "mk2", "d", "g1", "g2", "t")}
    nblk = S_tok // TB
    it = 0
    for blk in range(nblk):
        for tt in range(NT):
            t0 = blk * TB + tt * 128
            xs, ys = xt[tt % 2], yt[tt % 2]
            S.sync.dma_start(out=xs[:], in_=x[t0:t0 + 128, :])
            S.sync.dma_start(out=ys[:, 0:512], in_=ya[t0:t0 + 128, :])
            S.sync.dma_start(out=ys[:, 512:1024], in_=yb[t0:t0 + 128, :])
            for k in range(8):
                P.transpose(yT[tt % 2][:, k, :], ys[:, k * 128:(k + 1) * 128], pst[k % 2][:, 0:128],
                            eng="vector" if k % 2 == 0 else "scalar")
            for half in range(2):
                ps = pd[half]
                for k in range(8):
                    S.tensor.matmul(ps[:], lhsT=yT[tt % 2][:, k, :], rhs=wo_sb[:, k, half * 512:(half + 1) * 512],
                                    start=(k == 0), stop=(k == 7))
                S.vector.scalar_tensor_tensor(out=xs[:, half * 512:(half + 1) * 512], in0=xs[:, half * 512:(half + 1) * 512],
                                              scalar=DN_ALPHA, in1=ps[:], op0=ALU.mult, op1=ALU.add)
            X1 = x1t[tt % 2]
            layernorm(P, xs[:], X1[:], g0[:], b0[:], lns)
            S.scalar.mul(out=facc[:, tt, :], in_=X1[:], mul=DN_ALPHA)
            for k in range(8):
                S.tensor.transpose(out=pst[k % 2][:, 0:128], in_=X1[:, k * 128:(k + 1) * 128], identity=P.C("ident"))
                if k % 2 == 0:
                    S.vector.tensor_copy(out=x1T[:, k, tt * 128:(tt + 1) * 128], in_=pst[k % 2][:, 0:128])
                    if n_exp > 1:
                        S.scalar.copy(out=x1Tf[tt % 2][:, k, :], in_=pst[k % 2][:, 0:128])
                else:
                    S.scalar.copy(out=x1T[:, k, tt * 128:(tt + 1) * 128], in_=pst[k % 2][:, 0:128])
                    if n_exp > 1:
                        S.vector.tensor_copy(out=x1Tf[tt % 2][:, k, :], in_=pst[k % 2][:, 0:128])
            if n_exp > 1:
                ps = pd[0]
                for k in range(8):
                    S.tensor.matmul(ps[:, 0:8], lhsT=x1Tf[tt % 2][:, k, :], rhs=r_sb[:, k, :],
                                    start=(k == 0), stop=(k == 7))
                S.vector.tensor_copy(out=sm["lg"][:], in_=ps[:, 0:8])
                S.vector.reduce_max(out=sm["m1"][:, 0:1], in_=sm["lg"][:], axis=AX.X)
                S.vector.tensor_scalar(out=sm["mk1"][:], in0=sm["lg"][:], scalar1=sm["m1"][:, 0:1], scalar2=None, op0=ALU.is_ge)
                S.vector.scalar_tensor_tensor(out=sm["l2"][:], in0=sm["mk1"][:], scalar=-1e30, in1=sm["lg"][:], op0=ALU.mult, op1=ALU.add)
                S.vector.reduce_max(out=sm["m2"][:, 0:1], in_=sm["l2"][:], axis=AX.X)
                S.vector.tensor_scalar(out=sm["mk2"][:], in0=sm["l2"][:], scalar1=sm["m2"][:, 0:1], scalar2=None, op0=ALU.is_ge)
                S.vector.tensor_tensor(out=sm["d"][:, 0:1], in0=sm["m2"][:, 0:1], in1=sm["m1"][:, 0:1], op=ALU.subtract)
                S.scalar.activation(out=sm["t"][:, 0:1], in_=sm["d"][:, 0:1], func=AF.Exp)
                S.vector.tensor_scalar(out=sm["t"][:, 0:1], in0=sm["t"][:, 0:1], scalar1=1.0, scalar2=None, op0=ALU.add)
                S.vector.reciprocal(out=sm["g1"][:, 0:1], in_=sm["t"][:, 0:1])
                S.vector.tensor_scalar(out=sm["g2"][:, 0:1], in0=sm["g1"][:, 0:1], scalar1=-1.0, scalar2=1.0, op0=ALU.mult, op1=ALU.add)
                S.vector.tensor_scalar(out=comb[:, tt, :], in0=sm["mk1"][:], scalar1=sm["g1"][:, 0:1], scalar2=None, op0=ALU.mult)
                S.vector.scalar_tensor_tensor(out=comb[:, tt, :], in0=sm["mk2"][:], scalar=sm["g2"][:, 0:1], in1=comb[:, tt, :],
                                              op0=ALU.mult, op1=ALU.add)
        for e in range(n_exp):
            for (f0, fcw) in chunks:
                b = it % 2
                it += 1
                nft = fcw // 128
                S.gpsimd.dma_start(out=wg_sb[b][:, :, 0:fcw], in_=wg[e, :, f0:f0 + fcw].rearrange("(k p) f -> p k f", p=128))
                S.gpsimd.dma_start(out=wu_sb[b][:, :, 0:fcw], in_=wu[e, :, f0:f0 + fcw].rearrange("(k p) f -> p k f", p=128))
                S.gpsimd.dma_start(out=wd_sb[b][:, 0:nft, :], in_=wd[e, f0:f0 + fcw, :].rearrange("(k p) f -> p k f", p=128))
                for hb in range(TB // HB):
                    hsl = slice(hb * HB, (hb + 1) * HB)
                    for ft in range(nft):
                        for k in range(8):
                            S.tensor.matmul(pg[ft % 2][:, 0:HB], lhsT=wg_sb[b][:, k, ft * 128:(ft + 1) * 128], rhs=x1T[:, k, hsl],
                                            start=(k == 0), stop=(k == 7))
                        for k in range(8):
                            S.tensor.matmul(pu[ft % 2][:, 0:HB], lhsT=wu_sb[b][:, k, ft * 128:(ft + 1) * 128], rhs=x1T[:, k, hsl],
                                            start=(k == 0), stop=(k == 7))
                        S.scalar.activation(out=sg[ft % 2][:], in_=pg[ft % 2][:, 0:HB], func=AF.Silu)
                        S.vector.tensor_tensor(out=hT[b][:, ft, hsl], in0=sg[ft % 2][:], in1=pu[ft % 2][:, 0:HB], op=ALU.mult)
                for tt in range(NT):
                    for half in range(2):
                        ps = pd[(tt * 2 + half) % 2]
                        for ft in range(nft):
                            S.tensor.matmul(ps[:], lhsT=hT[b][:, ft, tt * 128:(tt + 1) * 128],
                                            rhs=wd_sb[b][:, ft, half * 512:(half + 1) * 512],
                                            start=(ft == 0), stop=(ft == nft - 1))
                        fa = facc[:, tt, half * 512:(half + 1) * 512]
                        if n_exp > 1:
                            S.vector.scalar_tensor_tensor(out=fa, in0=ps[:], scalar=comb[:, tt, e:e + 1], in1=fa,
                                                          op0=ALU.mult, op1=ALU.add)
                        else:
                            S.vector.tensor_tensor(out=fa, in0=fa, in1=ps[:], op=ALU.add)
        for tt in range(NT):
            t0 = blk * TB + tt * 128
            layernorm(P, facc[:, tt, :], xt[tt % 2][:], g1[:], b1[:], lns)
            S.sync.dma_start(out=xo[t0:t0 + 128, :], in_=xt[tt % 2][:])
    S.emit()
    return P.nc
def build_mlstm(S_tok, P=None):
    P = P or Prog()
    S = P.S
    z = P.din("z", [S_tok, 2056])
    gb = P.din("gb", [1, 8])
    ng = P.din("ng", [1, 512])
    y = P.dout("y", [S_tok, 512])
    gbb = P.bc_row("gbb", gb, 8)
    ngb = P.bc_row("ngb", ng, 512)
    Caug = P.sb("Caug", [64, 4, 129])
    S.vector.memset(Caug[:], 0.0)
    zt = P.rot("zt", [128, 1544])
    vaug = P.rot("vaug", [128, 4, 129])
    for b in range(2):
        S.vector.memset(vaug[b][:], 1.0)
    yt = P.rot("yt", [128, 512])
    sm = {k: P.sb(f"m_{k}", [128, 8]) for k in ("ig", "gf", "e", "lf", "bb", "eb", "es", "ebt", "tmp", "den", "rec")}
    LFtri = P.rot("LFtri", [128, 128])
    Rm = P.rot("Rm", [128, 128])
    ED = P.rot("ED", [128, 128])
    SDT = P.rot("SDT", [128, 128])
    qT = P.rot("qT", [64, 128])
    kT = P.rot("kT", [64, 128])
    qs = P.rot("qs", [128, 64])
    qsT = P.rot("qsT", [64, 128])
    ks = P.rot("ks", [128, 64])
    hh = P.rot("hh", [128, 128])
    sg = P.rot("sg", [128, 512])
    lns = ln_scratch(P, "mln", D=128, eps=1e-6)
    pT = [S.ps(f"pT{i}", [128, 512]) for i in range(2)]
    pD = S.ps("pD", [128, 512])
    pS = S.ps("pS", [128, 512])
    pN = S.ps("pN", [128, 512])
    pC = S.ps("pC", [128, 512])
    pB = S.ps("pB", [128, 512])
    ident, tri, ones, negT = P.C("ident"), P.C("tri"), P.C("ones"), P.C("negT")
    for t in range(S_tok // 128):
        t0 = t * 128
        Z = zt[t % 2]
        S.sync.dma_start(out=Z[:], in_=z[t0:t0 + 128, 512:2056])
        q, k, v, o = Z[:, 0:256], Z[:, 256:512], Z[:, 512:1024], Z[:, 1024:1536]
        S.vector.tensor_tensor(out=sm["ig"][:, 0:4], in0=Z[:, 1536:1540], in1=gbb[:, 0:4], op=ALU.add)
        S.vector.tensor_tensor(out=sm["gf"][:, 0:4], in0=Z[:, 1540:1544], in1=gbb[:, 4:8], op=ALU.add)
        S.scalar.activation(out=sm["e"][:, 0:4], in_=sm["gf"][:, 0:4], func=AF.Exp, scale=-1.0)
        S.vector.tensor_scalar(out=sm["e"][:, 0:4], in0=sm["e"][:, 0:4], scalar1=1.0, scalar2=None, op0=ALU.add)
        S.scalar.activation(out=sm["lf"][:, 0:4], in_=sm["e"][:, 0:4], func=AF.Ln)
        S.vector.tensor_scalar(out=sm["lf"][:, 0:4], in0=sm["lf"][:, 0:4], scalar1=-1.0, scalar2=None, op0=ALU.mult)
        S.tensor.matmul(pB[:, 0:4], lhsT=tri, rhs=sm["lf"][:, 0:4], start=True, stop=True)
        S.tensor.matmul(pB[:, 4:8], lhsT=ones, rhs=sm["lf"][:, 0:4], start=True, stop=True)
        S.vector.tensor_copy(out=sm["bb"][:], in_=pB[:, 0:8])
        S.scalar.activation(out=sm["eb"][:, 0:4], in_=sm["bb"][:, 0:4], func=AF.Exp)
        S.scalar.activation(out=sm["ebt"][:, 0:4], in_=sm["bb"][:, 4:8], func=AF.Exp)
        S.vector.tensor_tensor(out=sm["tmp"][:, 0:4], in0=sm["bb"][:, 4:8], in1=sm["bb"][:, 0:4], op=ALU.subtract)
        S.vector.tensor_tensor(out=sm["tmp"][:, 0:4], in0=sm["tmp"][:, 0:4], in1=sm["ig"][:, 0:4], op=ALU.add)
        S.scalar.activation(out=sm["es"][:, 0:4], in_=sm["tmp"][:, 0:4], func=AF.Exp)
        VA = vaug[t % 2]
        S.vector.tensor_copy(out=VA[:, :, 0:128], in_=v.rearrange("p (h d) -> p h d", h=4))
        S.scalar.activation(out=sg[t % 2][:], in_=o, func=AF.Sigmoid)
        Y = yt[t % 2]
        for h in range(4):
            u = (t * 4 + h) % 2
            S.vector.tensor_scalar(out=LFtri[u][:], in0=tri, scalar1=sm["lf"][:, h:h + 1], scalar2=None, op0=ALU.mult)
            S.vector.scalar_tensor_tensor(out=Rm[u][:], in0=ident, scalar=sm["ig"][:, h:h + 1], in1=LFtri[u][:],
                                          op0=ALU.mult, op1=ALU.subtract)
            S.tensor.matmul(pD[:, 0:128], lhsT=ones, rhs=LFtri[u][:], start=True, stop=False)
            S.tensor.matmul(pD[:, 0:128], lhsT=Rm[u][:], rhs=ones, start=False, stop=False)
            S.tensor.matmul(pD[:, 0:128], lhsT=ident, rhs=negT, start=False, stop=True)
            S.scalar.activation(out=ED[u][:], in_=pD[:, 0:128], func=AF.Exp)
            qh, kh = q[:, h * 64:(h + 1) * 64], k[:, h * 64:(h + 1) * 64]
            P.transpose(qT[u][:], qh, pT[0][0:64, 0:128], rows=128, cols=64)
            P.transpose(kT[u][:], kh, pT[1][0:64, 0:128], rows=128, cols=64, eng="scalar")
            S.vector.tensor_scalar(out=qs[u][:], in0=qh, scalar1=sm["eb"][:, h:h + 1], scalar2=0.125, op0=ALU.mult, op1=ALU.mult)
            P.transpose(qsT[u][:], qs[u][:], pT[0][0:64, 0:128], rows=128, cols=64)
            S.tensor.matmul(pS[:, 0:128], lhsT=kT[u][:], rhs=qT[u][:], start=True, stop=True)
            S.vector.scalar_tensor_tensor(out=SDT[u][:], in0=pS[:, 0:128], scalar=0.125, in1=ED[u][:], op0=ALU.mult, op1=ALU.mult)
            S.tensor.matmul(pN[:, 0:129], lhsT=SDT[u][:], rhs=VA[:, h, :], start=True, stop=False)
            S.tensor.matmul(pN[:, 0:129], lhsT=qsT[u][:], rhs=Caug[:, h, :], start=False, stop=True)
            S.scalar.activation(out=sm["den"][:, 0:1], in_=pN[:, 128:129], func=AF.Abs)
            S.vector.tensor_scalar_max(out=sm["den"][:, 0:1], in0=sm["den"][:, 0:1], scalar1=1.0)
            S.vector.reciprocal(out=sm["rec"][:, 0:1], in_=sm["den"][:, 0:1])
            S.vector.tensor_scalar(out=hh[u][:], in0=pN[:, 0:128], scalar1=sm["rec"][:, 0:1], scalar2=None, op0=ALU.mult)
            S.vector.tensor_scalar(out=ks[u][:], in0=kh, scalar1=sm["es"][:, h:h + 1], scalar2=None, op0=ALU.mult)
            S.tensor.matmul(pC[0:64, 0:129], lhsT=ks[u][:], rhs=VA[:, h, :], start=True, stop=True)
            S.vector.scalar_tensor_tensor(out=Caug[:, h, :], in0=Caug[:, h, :], scalar=sm["ebt"][0:64, h:h + 1], in1=pC[0:64, 0:129],
                                          op0=ALU.mult, op1=ALU.add)
            layernorm(P, hh[u][:], Y[:, h * 128:(h + 1) * 128], None, None, lns, D=128, eps=1e-6)
        S.vector.tensor_tensor(out=Y[:], in0=Y[:], in1=ngb[:], op=ALU.mult)
        S.vector.tensor_tensor(out=Y[:], in0=Y[:], in1=sg[t % 2][:], op=ALU.mult)
        S.gpsimd.dma_start(out=y[t0:t0 + 128, :], in_=Y[:])
    S.emit()
    return P.nc
class _Stop(Exception):
    pass


def build_gdn(S_tok, stop=99, P=None):
    try:
        return _build_gdn(S_tok, stop, P)
    except _Stop as e:
        P = e.args[0]
        P.S.emit()
        return P.nc


def _build_gdn(S_tok, stop, P=None):
    P = P or Prog()
    S = P.S
    z = P.din("z", [S_tok, 3848])
    conv_w = P.din("conv_w", [4, 1536])
    a_log = P.din("a_log", [1, 4])
    dt_bias = P.din("dt_bias", [1, 4])
    norm_g = P.din("norm_g", [1, 128])
    y = P.dout("y", [S_tok, 512])
    cw = [P.bc_row(f"cw{i}", conv_w[i:i + 1, :], 1536) for i in range(4)]
    alb = P.bc_row("alb", a_log, 4)
    dtb = P.bc_row("dtb", dt_bias, 4)
    ngb = P.bc_row("ngb", norm_g, 128)
    ea = P.sb("ea", [128, 4])
    S.scalar.activation(out=ea[:], in_=alb[:], func=AF.Exp)
    state = P.sb("state", [128, 4, 128])
    S.vector.memset(state[:], 0.0)
    xs = [P.rot(f"xs{k}", [128, 1536]) for k in range(4)]
    gt = P.rot("gt", [128, 520])
    acc = P.rot("acc", [128, 1536])
    tmp = P.rot("ctmp", [128, 1536])
    sq = P.sb("sq", [128, 1024])
    yt = P.rot("yt", [128, 512])
    sgt = P.rot("sgt", [128, 512])
    sm = {k: P.sb(f"g_{k}", [128, 8]) for k in ("ss", "rn", "beta", "sp", "g", "dd", "edec", "ek", "edl", "t", "ss2", "rs")}
    eps6 = P.sb("eps6", [128, 1])
    S.vector.memset(eps6[:], 1e-6)
    names = ["Gtri", "nGtri", "GT", "GTs", "G", "Gs", "kb", "kT", "kbT", "qn", "qT", "kn", "rhs_v", "rhs_k", "nwT", "u", "QKT",
             "qd", "qdT", "kd", "o", "osq"]
    B = {n: P.rot(n, [128, 128]) for n in names}
    bank = [[S.ps(f"bk{u}_{i}", [128, 512]) for i in range(4)] for u in range(2)]
    pS = bank[0][3]
    X2 = [P.rot(f"X{u}", [128, 128]) for u in range(2)]
    Y2 = [P.rot(f"Y{u}", [128, 128]) for u in range(2)]
    P2 = [P.rot(f"Pm{u}", [128, 128]) for u in range(2)]
    ss2 = P.rot("ss2", [128, 1]); rs2 = P.rot("rs2", [128, 1])
    ident, tri, ones, negT, neg = P.C("ident"), P.C("tri"), P.C("ones"), P.C("negT"), P.C("neg")
    mT_s, m_s = P.C("mT_strict"), P.C("m_strict")
    for t in range(S_tok // 128):
        t0 = t * 128
        b = t % 2
        for k in range(4):
            if t0 - k < 0:
                S.vector.memset(xs[k][b][0:32, :], 0.0)
                S.sync.dma_start(out=xs[k][b][k:128, :], in_=z[0:128 - k, 1792:3328])
            else:
                S.sync.dma_start(out=xs[k][b][:], in_=z[t0 - k:t0 - k + 128, 1792:3328])
        G = gt[b]
        S.sync.dma_start(out=G[:], in_=z[t0:t0 + 128, 3328:3848])
        S.vector.tensor_tensor(out=acc[b][:], in0=xs[0][b][:], in1=cw[3][:], op=ALU.mult)
        for k in (1, 2, 3):
            S.vector.tensor_tensor(out=xs[k][b][:], in0=xs[k][b][:], in1=cw[3 - k][:], op=ALU.mult)
            S.vector.tensor_tensor(out=acc[b][:], in0=acc[b][:], in1=xs[k][b][:], op=ALU.add)
        A = acc[b]
        if stop == 1:
            raise _Stop(P)
        S.scalar.activation(out=A[:], in_=A[:], func=AF.Silu)
        S.scalar.activation(out=sq[:], in_=A[:, 0:1024], func=AF.Square)
        S.vector.reduce_sum(out=sm["ss"][:, 0:8], in_=sq[:].rearrange("p (h d) -> p h d", h=8), axis=AX.X)
        S.scalar.activation(out=sm["ss"][:, 0:8], in_=sm["ss"][:, 0:8], func=AF.Sqrt, bias=eps6[:, 0:1], scale=1.0)
        S.vector.reciprocal(out=sm["rn"][:, 0:8], in_=sm["ss"][:, 0:8])
        S.scalar.activation(out=sm["beta"][:, 0:4], in_=G[:, 512:516], func=AF.Sigmoid)
        S.vector.tensor_tensor(out=sm["sp"][:, 0:4], in0=G[:, 516:520], in1=dtb[:], op=ALU.add)
        S.scalar.activation(out=sm["sp"][:, 0:4], in_=sm["sp"][:, 0:4], func=AF.Exp)
        S.vector.tensor_scalar(out=sm["sp"][:, 0:4], in0=sm["sp"][:, 0:4], scalar1=1.0, scalar2=None, op0=ALU.add)
        S.scalar.activation(out=sm["sp"][:, 0:4], in_=sm["sp"][:, 0:4], func=AF.Ln)
        S.vector.scalar_tensor_tensor(out=sm["g"][:, 0:4], in0=sm["sp"][:, 0:4], scalar=-1.0, in1=ea[:], op0=ALU.mult, op1=ALU.mult)
        S.tensor.matmul(pS[:, 0:4], lhsT=tri, rhs=sm["g"][:, 0:4], start=True, stop=True)
        S.tensor.matmul(pS[:, 4:8], lhsT=ones, rhs=sm["g"][:, 0:4], start=True, stop=True)
        S.vector.tensor_copy(out=sm["dd"][:], in_=pS[:, 0:8])
        S.scalar.activation(out=sm["edec"][:, 0:4], in_=sm["dd"][:, 0:4], func=AF.Exp)
        S.scalar.activation(out=sm["edl"][:, 0:4], in_=sm["dd"][:, 4:8], func=AF.Exp)
        S.vector.tensor_tensor(out=sm["t"][:, 0:4], in0=sm["dd"][:, 4:8], in1=sm["dd"][:, 0:4], op=ALU.subtract)
        S.scalar.activation(out=sm["ek"][:, 0:4], in_=sm["t"][:, 0:4], func=AF.Exp)
        S.scalar.activation(out=sgt[b][:], in_=G[:, 0:512], func=AF.Silu)
        if stop == 2:
            raise _Stop(P)
        Yt = yt[b]
        def head_body(h, u):
            T_ = {n: B[n][u] for n in names}
            b0_, b1_, b2_, b3_ = bank[u]
            pGa, pGb, pTa, pTb = b0_[:, 0:128], b0_[:, 128:256], b0_[:, 256:384], b0_[:, 384:512]
            pA0, pA1, pA2 = b1_[:, 0:128], b1_[:, 128:256], b1_[:, 256:384]
            pI0, pI1, pI2 = b2_[:, 0:128], b2_[:, 128:256], b2_[:, 256:384]
            pU0, pU1, pS1, pS2 = b3_[:, 0:128], b3_[:, 128:256], b3_[:, 256:384], b3_[:, 384:512]
            Xu, Yu, Pu = X2[u], Y2[u], P2[u]
            qh, kh, vh = A[:, h * 128:(h + 1) * 128], A[:, 512 + h * 128:512 + (h + 1) * 128], A[:, 1024 + h * 128:1024 + (h + 1) * 128]
            S.vector.tensor_scalar(out=T_["Gtri"][:], in0=tri, scalar1=sm["g"][:, h:h + 1], scalar2=None, op0=ALU.mult)
            S.vector.tensor_scalar(out=T_["nGtri"][:], in0=T_["Gtri"][:], scalar1=-1.0, scalar2=None, op0=ALU.mult)
            yield
            S.tensor.matmul(pGa, lhsT=ones, rhs=T_["Gtri"][:], start=True, stop=False)
            S.tensor.matmul(pGa, lhsT=T_["nGtri"][:], rhs=ones, start=False, stop=False)
            S.tensor.matmul(pGa, lhsT=ident, rhs=negT, start=False, stop=True)
            S.tensor.matmul(pGb, lhsT=T_["Gtri"][:], rhs=ones, start=True, stop=False)
            S.tensor.matmul(pGb, lhsT=ones, rhs=T_["nGtri"][:], start=False, stop=False)
            S.tensor.matmul(pGb, lhsT=ident, rhs=neg, start=False, stop=True)
            yield
            S.scalar.activation(out=T_["GT"][:], in_=pGa, func=AF.Exp)
            S.scalar.activation(out=T_["G"][:], in_=pGb, func=AF.Exp)
            yield
            S.vector.tensor_tensor(out=T_["GTs"][:], in0=T_["GT"][:], in1=mT_s, op=ALU.mult)
            S.vector.tensor_tensor(out=T_["Gs"][:], in0=T_["G"][:], in1=m_s, op=ALU.mult)
            S.vector.tensor_scalar(out=T_["qn"][:], in0=qh, scalar1=sm["rn"][:, h:h + 1], scalar2=128 ** -0.5, op0=ALU.mult, op1=ALU.mult)
            S.vector.tensor_scalar(out=T_["kn"][:], in0=kh, scalar1=sm["rn"][:, 4 + h:5 + h], scalar2=None, op0=ALU.mult)
            S.vector.tensor_scalar(out=T_["kb"][:], in0=T_["kn"][:], scalar1=sm["beta"][:, h:h + 1], scalar2=None, op0=ALU.mult)
            yield
            P.transpose(T_["kT"][:], T_["kn"][:], pTa)
            yield
            P.transpose(T_["kbT"][:], T_["kb"][:], pTb, eng="scalar")
            yield
            P.transpose(T_["qT"][:], T_["qn"][:], pTa)
            S.vector.tensor_scalar(out=T_["qd"][:], in0=T_["qn"][:], scalar1=sm["edec"][:, h:h + 1], scalar2=None, op0=ALU.mult)
            yield
            P.transpose(T_["qdT"][:], T_["qd"][:], pTb, eng="scalar")
            S.vector.tensor_scalar(out=T_["kd"][:], in0=T_["kn"][:], scalar1=sm["ek"][:, h:h + 1], scalar2=None, op0=ALU.mult)
            S.vector.tensor_scalar(out=T_["rhs_v"][:], in0=vh, scalar1=sm["beta"][:, h:h + 1], scalar2=None, op0=ALU.mult)
            S.vector.tensor_scalar(out=T_["rhs_k"][:], in0=T_["kb"][:], scalar1=sm["edec"][:, h:h + 1], scalar2=None, op0=ALU.mult)
            yield
            S.tensor.matmul(pA0, lhsT=T_["kT"][:], rhs=T_["kbT"][:], start=True, stop=True)
            S.tensor.matmul(pA1, lhsT=T_["kbT"][:], rhs=T_["kT"][:], start=True, stop=True)
            S.tensor.matmul(pA2, lhsT=T_["kT"][:], rhs=T_["qT"][:], start=True, stop=True)
            yield
            S.vector.scalar_tensor_tensor(out=Xu[0][:], in0=pA0, scalar=-1.0, in1=T_["GTs"][:], op0=ALU.mult, op1=ALU.mult)
            S.vector.scalar_tensor_tensor(out=Yu[0][:], in0=pA1, scalar=-1.0, in1=T_["Gs"][:], op0=ALU.mult, op1=ALU.mult)
            S.vector.tensor_tensor(out=T_["QKT"][:], in0=pA2, in1=T_["GT"][:], op=ALU.mult)
            S.vector.tensor_tensor(out=Pu[0][:], in0=Xu[0][:], in1=ident, op=ALU.add)
            yield
            nst = 6
            for n in range(nst):
                a, c = n % 2, (n + 1) % 2
                if n < nst - 1:
                    S.tensor.matmul(pI0, lhsT=Yu[a][:], rhs=Xu[a][:], start=True, stop=True)
                S.tensor.matmul(pI1, lhsT=Xu[a][:], rhs=Yu[a][:], start=True, stop=True)
                yield
                if n < nst - 1:
                    S.scalar.copy(out=Xu[c][:], in_=pI0)
                S.vector.tensor_copy(out=Yu[c][:], in_=pI1)
                yield
                S.tensor.matmul(pI2, lhsT=Yu[c][:], rhs=Pu[a][:], start=True, stop=True)
                yield
                S.vector.tensor_tensor(out=Pu[c][:], in0=Pu[a][:], in1=pI2, op=ALU.add)
                yield
            TT = Pu[nst % 2]
            S.tensor.matmul(pS1, lhsT=T_["rhs_k"][:], rhs=TT[:], start=True, stop=True)
            yield
            S.scalar.mul(out=T_["nwT"][:], in_=pS1, mul=-1.0)
            yield
            S.tensor.matmul(pU0, lhsT=TT[:], rhs=T_["rhs_v"][:], start=True, stop=False)
            S.tensor.matmul(pU0, lhsT=T_["nwT"][:], rhs=state[:, h, :], start=False, stop=True)
            yield
            S.vector.tensor_copy(out=T_["u"][:], in_=pU0)
            yield
            S.tensor.matmul(pU1, lhsT=T_["qdT"][:], rhs=state[:, h, :], start=True, stop=False)
            S.tensor.matmul(pU1, lhsT=T_["QKT"][:], rhs=T_["u"][:], start=False, stop=True)
            S.tensor.matmul(pS2, lhsT=T_["kd"][:], rhs=T_["u"][:], start=True, stop=True)
            yield
            S.scalar.copy(out=T_["o"][:], in_=pU1)
            S.vector.scalar_tensor_tensor(out=state[:, h, :], in0=state[:, h, :], scalar=sm["edl"][:, h:h + 1], in1=pS2,
                                          op0=ALU.mult, op1=ALU.add)
            yield
            S.scalar.activation(out=T_["osq"][:], in_=T_["o"][:], func=AF.Square, accum_out=ss2[u][:, 0:1])
            S.scalar.activation(out=rs2[u][:, 0:1], in_=ss2[u][:, 0:1], func=AF.Sqrt, bias=eps6[:, 0:1], scale=1.0 / 128)
            S.vector.reciprocal(out=rs2[u][:, 0:1], in_=rs2[u][:, 0:1])
            S.vector.scalar_tensor_tensor(out=Yt[:, h * 128:(h + 1) * 128], in0=T_["o"][:], scalar=rs2[u][:, 0:1], in1=ngb[:],
                                          op0=ALU.mult, op1=ALU.mult)

        for hp in range(2):
            gens = [head_body(2 * hp, 0), head_body(2 * hp + 1, 1)]
            while gens:
                for g_ in list(gens):
                    try:
                        next(g_)
                    except StopIteration:
                        gens.remove(g_)
        S.vector.tensor_tensor(out=Yt[:], in0=Yt[:], in1=sgt[b][:], op=ALU.mult)
        S.gpsimd.dma_start(out=y[t0:t0 + 128, :], in_=Yt[:])
    S.emit()
    return P.nc
def build_rwkv(S_tok, P=None):
    P = P or Prog()
    S = P.S
    z = P.din("z", [S_tok, 3848])
    vecs = P.din("vecs", [8, 512])
    mu = P.din("mu", [1, 1792])
    w_up = P.din("w_up", [64, 512])
    a_up = P.din("a_up", [64, 512])
    g_up = P.din("g_up", [128, 512])
    y = P.dout("y", [S_tok, 512])
    mub = P.bc_row("mub", mu, 1792)
    vb = [P.bc_row(f"vb{i}", vecs[i:i + 1, :], 512) for i in range(7)]
    w0b, a0b, kkb, kab, rkb, lgb, lbb = vb
    wup = P.sb("wup", [64, 512]); aup = P.sb("aup", [64, 512]); gup = P.sb("gup", [128, 512])
    S.sync.dma_start(out=wup[:], in_=w_up); S.sync.dma_start(out=aup[:], in_=a_up); S.sync.dma_start(out=gup[:], in_=g_up)
    H = P.sb("H", [64, 8, 64])
    S.vector.memset(H[:], 0.0)
    L = 64
    cur = P.rot("cur", [L, 1792]); prev = P.rot("prev", [L, 1792])
    bigs = [{n: P.sb(f"r{i}_{n}", [L, 512]) for n in ("logw", "a", "g", "kk", "kp", "b", "ecl", "ecm", "eni", "rt", "kat", "kt", "bt", "nbt",
                                                        "tmp", "Y", "Yn", "sq")} for i in range(2)]
    sm = {n: P.sb(f"rs_{n}", [L, 8]) for n in ("ss", "rn", "bs")}
    tw = P.sb("tw", [L, 64]); twT = P.sb("twT", [64, L]); adT = P.sb("adT", [64, L])
    sgd = P.sb("sgd", [L, 128]); sgdT = P.sb("sgdT", [128, L])
    PL = P.sb("PL", [64, 8])
    eps6 = P.sb("eps6", [128, 1])
    S.vector.memset(eps6[:], 1e-6)
    TN = ["rtT", "katT", "ktT", "btT"]
    TT_ = {n: P.rot(n, [64, L], 4) for n in TN}
    MN = ["AkvT", "BrkT", "nBrbT"]
    M_ = {n: P.rot(n, [L, L], 4) for n in MN}
    X2 = [P.rot(f"X{u}", [L, L]) for u in range(4)]; Y2 = [P.rot(f"Yq{u}", [L, L]) for u in range(4)]; P2 = [P.rot(f"Pm{u}", [L, L]) for u in range(4)]
    Wsb = P.rot("Wsb", [L, 64], 4); Usb = P.rot("Usb", [L, 64], 4)
    lns2 = [ln_scratch(P, f"rln{u}", D=64, eps=64e-5) for u in range(4)]
    bkA = [S.ps(f"bkA{u}", [128, 512]) for u in range(4)]
    bkB = [S.ps(f"bkB{u}", [128, 512]) for u in range(4)]
    pL = bkA[0]
    pT = [bkB[0], bkB[1]]
    ident, btri, chunkind = P.C("ident", L, L), P.C("tri", L, L), P.C("ones", L, 1)
    bmT_i, bmT_s, bm_s = P.C("tri", L, L), P.C("mT_strict", L, L), P.C("m_strict", L, L)
    for t in range(S_tok // L):
        t0 = t * L
        b = t % 2
        B = bigs[b]
        Cc, Pp = cur[b], prev[b]
        S.sync.dma_start(out=Cc[:], in_=z[t0:t0 + L, 0:1792])
        if t == 0:
            S.vector.memset(Pp[0:32, :], 0.0)
            S.sync.dma_start(out=Pp[1:L, :], in_=z[0:L - 1, 0:1792])
        else:
            S.sync.dma_start(out=Pp[:], in_=z[t0 - 1:t0 + L - 1, 0:1792])
        S.vector.tensor_tensor(out=Pp[:], in0=Pp[:], in1=Cc[:], op=ALU.subtract)
        S.vector.tensor_tensor(out=Pp[:], in0=Pp[:], in1=mub[0:L, :], op=ALU.mult)
        S.vector.tensor_tensor(out=Cc[:], in0=Cc[:], in1=Pp[:], op=ALU.add)
        r, k, v = Cc[:, 0:512], Cc[:, 512:1024], Cc[:, 1024:1536]
        S.scalar.activation(out=tw[:], in_=Cc[:, 1536:1600], func=AF.Tanh)
        P.transpose(twT[:], tw[:], pT[0][0:64, 0:L], rows=L, cols=64)
        P.transpose(adT[:], Cc[:, 1600:1664], pT[1][0:64, 0:L], rows=L, cols=64, eng="scalar")
        S.scalar.activation(out=sgd[:], in_=Cc[:, 1664:1792], func=AF.Sigmoid)
        P.transpose(sgdT[:], sgd[:], pT[0][:, 0:L], rows=L, cols=128)
        S.tensor.matmul(pL[0:L, :], lhsT=twT[:], rhs=wup[:], start=True, stop=True)
        S.vector.tensor_tensor(out=B["logw"][:], in0=pL[0:L, :], in1=w0b[0:L, :], op=ALU.add)
        S.scalar.activation(out=B["logw"][:], in_=B["logw"][:], func=AF.Sigmoid)
        S.vector.tensor_scalar(out=B["logw"][:], in0=B["logw"][:], scalar1=-0.6065306597126334, scalar2=None, op0=ALU.mult)
        S.tensor.matmul(pL[0:L, :], lhsT=adT[:], rhs=aup[:], start=True, stop=True)
        S.vector.tensor_tensor(out=B["a"][:], in0=pL[0:L, :], in1=a0b[0:L, :], op=ALU.add)
        S.scalar.activation(out=B["a"][:], in_=B["a"][:], func=AF.Sigmoid)
        S.tensor.matmul(pL[0:L, :], lhsT=sgdT[:], rhs=gup[:], start=True, stop=True)
        S.scalar.copy(out=B["g"][:], in_=pL[0:L, :])
        S.vector.tensor_tensor(out=B["kk"][:], in0=k, in1=kkb[0:L, :], op=ALU.mult)
        S.scalar.activation(out=B["sq"][:], in_=B["kk"][:], func=AF.Square)
        S.vector.reduce_sum(out=sm["ss"][:, 0:8], in_=B["sq"][:].rearrange("p (h d) -> p h d", h=8), axis=AX.X)
        S.scalar.activation(out=sm["ss"][:, 0:8], in_=sm["ss"][:, 0:8], func=AF.Sqrt, bias=eps6[0:L, 0:1], scale=1.0)
        S.vector.reciprocal(out=sm["rn"][:, 0:8], in_=sm["ss"][:, 0:8])
        for h in range(8):
            S.vector.tensor_scalar(out=B["kk"][:, h * 64:(h + 1) * 64], in0=B["kk"][:, h * 64:(h + 1) * 64],
                                   scalar1=sm["rn"][:, h:h + 1], scalar2=None, op0=ALU.mult)
        S.vector.scalar_tensor_tensor(out=B["tmp"][:], in0=B["a"][:], scalar=-1.0, in1=kab[0:L, :], op0=ALU.add, op1=ALU.mult)
        S.vector.scalar_tensor_tensor(out=B["kp"][:], in0=B["tmp"][:], scalar=1.0, in1=k, op0=ALU.add, op1=ALU.mult)
        S.vector.tensor_tensor(out=B["b"][:], in0=B["kk"][:], in1=B["a"][:], op=ALU.mult)
        S.tensor.matmul(pL[0:L, :], lhsT=btri, rhs=B["logw"][:], start=True, stop=True)
        S.scalar.activation(out=B["ecl"][:], in_=pL[0:L, :], func=AF.Exp)
        S.scalar.activation(out=B["eni"][:], in_=pL[0:L, :], func=AF.Exp, scale=-1.0)
        S.vector.tensor_tensor(out=B["tmp"][:], in0=pL[0:L, :], in1=B["logw"][:], op=ALU.subtract)
        S.scalar.activation(out=B["ecm"][:], in_=B["tmp"][:], func=AF.Exp)
        S.vector.tensor_tensor(out=B["rt"][:], in0=r, in1=B["ecl"][:], op=ALU.mult)
        S.vector.tensor_tensor(out=B["kat"][:], in0=B["kk"][:], in1=B["ecm"][:], op=ALU.mult)
        S.vector.tensor_tensor(out=B["kt"][:], in0=B["kp"][:], in1=B["eni"][:], op=ALU.mult)
        S.vector.tensor_tensor(out=B["bt"][:], in0=B["b"][:], in1=B["eni"][:], op=ALU.mult)
        S.vector.tensor_scalar(out=B["nbt"][:], in0=B["bt"][:], scalar1=-1.0, scalar2=None, op0=ALU.mult)
        for h in range(8):
            S.tensor.matmul(pL[0:64, h:h + 1], lhsT=B["logw"][:, h * 64:(h + 1) * 64], rhs=chunkind, start=True, stop=True)
        S.scalar.activation(out=PL[:], in_=pL[0:64, 0:8], func=AF.Exp)
        S.vector.tensor_tensor(out=B["tmp"][:], in0=r, in1=B["kp"][:], op=ALU.mult)
        S.vector.tensor_tensor(out=B["tmp"][:], in0=B["tmp"][:], in1=rkb[0:L, :], op=ALU.mult)
        S.vector.reduce_sum(out=sm["bs"][:, 0:8], in_=B["tmp"][:].rearrange("p (h d) -> p h d", h=8), axis=AX.X)
        def head_body(h, u):
            hs = slice(h * 64, (h + 1) * 64)
            T_ = {n: TT_[n][u] for n in TN}
            Mh = {n: M_[n][u] for n in MN}
            pAu = bkA[u]
            pC = bkB[u]
            c0 = 0
            Xu, Yu, Pu = X2[u], Y2[u], P2[u]
            P.transpose(T_["rtT"][:], B["rt"][:, hs], pC[0:64, 256:256 + L], rows=L, cols=64)
            yield
            P.transpose(T_["katT"][:], B["kat"][:, hs], pC[0:64, 256:256 + L], rows=L, cols=64, eng="scalar")
            yield
            P.transpose(T_["ktT"][:], B["kt"][:, hs], pC[0:64, 256:256 + L], rows=L, cols=64)
            yield
            P.transpose(T_["btT"][:], B["bt"][:, hs], pC[0:64, 256:256 + L], rows=L, cols=64, eng="scalar")
            yield
            S.tensor.matmul(pAu[0:L, 0:64], lhsT=T_["btT"][:], rhs=T_["katT"][:], start=True, stop=True)
            S.tensor.matmul(pAu[0:L, 64:128], lhsT=T_["katT"][:], rhs=T_["btT"][:], start=True, stop=True)
            S.tensor.matmul(pAu[0:L, 128:192], lhsT=T_["ktT"][:], rhs=T_["katT"][:], start=True, stop=True)
            S.tensor.matmul(pAu[0:L, 192:256], lhsT=T_["ktT"][:], rhs=T_["rtT"][:], start=True, stop=True)
            S.tensor.matmul(pAu[0:L, 256:320], lhsT=T_["btT"][:], rhs=T_["rtT"][:], start=True, stop=True)
            yield
            S.vector.scalar_tensor_tensor(out=Xu[0][:], in0=pAu[0:L, 0:64], scalar=-1.0, in1=bmT_s, op0=ALU.mult, op1=ALU.mult)
            S.vector.scalar_tensor_tensor(out=Yu[0][:], in0=pAu[0:L, 64:128], scalar=-1.0, in1=bm_s, op0=ALU.mult, op1=ALU.mult)
            S.vector.tensor_tensor(out=Mh["AkvT"][:], in0=pAu[0:L, 128:192], in1=bmT_s, op=ALU.mult)
            S.vector.tensor_tensor(out=Mh["BrkT"][:], in0=pAu[0:L, 192:256], in1=bmT_i, op=ALU.mult)
            S.vector.scalar_tensor_tensor(out=Mh["nBrbT"][:], in0=pAu[0:L, 256:320], scalar=-1.0, in1=bmT_i, op0=ALU.mult, op1=ALU.mult)
            S.vector.tensor_tensor(out=Pu[0][:], in0=Xu[0][:], in1=ident, op=ALU.add)
            yield
            nst = 5
            for n in range(nst):
                a_, c_ = n % 2, (n + 1) % 2
                if n < nst - 1:
                    S.tensor.matmul(pAu[0:L, 320:384], lhsT=Yu[a_][:], rhs=Xu[a_][:], start=True, stop=True)
                S.tensor.matmul(pAu[0:L, 384:448], lhsT=Xu[a_][:], rhs=Yu[a_][:], start=True, stop=True)
                yield
                if n < nst - 1:
                    S.scalar.copy(out=Xu[c_][:], in_=pAu[0:L, 320:384])
                S.vector.tensor_copy(out=Yu[c_][:], in_=pAu[0:L, 384:448])
                yield
                S.tensor.matmul(pAu[0:L, 448:512], lhsT=Yu[c_][:], rhs=Pu[a_][:], start=True, stop=True)
                yield
                S.vector.tensor_tensor(out=Pu[c_][:], in0=Pu[a_][:], in1=pAu[0:L, 448:512], op=ALU.add)
                yield
            TT = Pu[nst % 2]
            cs = slice(0, L)
            H0 = H[:, h, :]
            S.tensor.matmul(pC[cs, c0:c0 + 64], lhsT=T_["katT"][:, cs], rhs=H0, start=True, stop=False)
            S.tensor.matmul(pC[cs, c0:c0 + 64], lhsT=Mh["AkvT"][cs, cs], rhs=v[cs, hs], start=False, stop=True)
            yield
            S.vector.tensor_copy(out=Wsb[u][cs, :], in_=pC[cs, c0:c0 + 64])
            yield
            S.tensor.matmul(pC[cs, c0 + 64:c0 + 128], lhsT=TT[cs, cs], rhs=Wsb[u][cs, :], start=True, stop=True)
            yield
            S.vector.tensor_copy(out=Usb[u][cs, :], in_=pC[cs, c0 + 64:c0 + 128])
            yield
            S.tensor.matmul(pC[cs, c0 + 128:c0 + 192], lhsT=T_["rtT"][:, cs], rhs=H0, start=True, stop=False)
            S.tensor.matmul(pC[cs, c0 + 128:c0 + 192], lhsT=Mh["BrkT"][cs, cs], rhs=v[cs, hs], start=False, stop=False)
            S.tensor.matmul(pC[cs, c0 + 128:c0 + 192], lhsT=Mh["nBrbT"][cs, cs], rhs=Usb[u][cs, :], start=False, stop=True)
            S.tensor.matmul(pC[0:64, c0 + 192:c0 + 256], lhsT=B["kt"][cs, hs], rhs=v[cs, hs], start=True, stop=False)
            S.tensor.matmul(pC[0:64, c0 + 192:c0 + 256], lhsT=B["nbt"][cs, hs], rhs=Usb[u][cs, :], start=False, stop=True)
            yield
            S.scalar.copy(out=B["Y"][cs, hs], in_=pC[cs, c0 + 128:c0 + 192])
            S.vector.tensor_tensor(out=H0, in0=H0, in1=pC[0:64, c0 + 192:c0 + 256], op=ALU.add)
            S.vector.tensor_scalar(out=H0, in0=H0, scalar1=PL[:, h:h + 1], scalar2=None, op0=ALU.mult)
            yield
            layernorm(P, B["Y"][:, hs], B["Yn"][:, hs], None, None, lns2[u], D=64, eps=64e-5)

        for hp in range(2):
            gens = [head_body(4 * hp + j, j) for j in range(4)]
            while gens:
                for g_ in list(gens):
                    try:
                        next(g_)
                    except StopIteration:
                        gens.remove(g_)
        S.vector.tensor_tensor(out=B["Yn"][:], in0=B["Yn"][:], in1=lgb[0:L, :], op=ALU.mult)
        S.vector.tensor_tensor(out=B["Yn"][:], in0=B["Yn"][:], in1=lbb[0:L, :], op=ALU.add)
        for h in range(8):
            hs = slice(h * 64, (h + 1) * 64)
            S.vector.scalar_tensor_tensor(out=B["Yn"][:, hs], in0=v[:, hs], scalar=sm["bs"][:, h:h + 1], in1=B["Yn"][:, hs],
                                          op0=ALU.mult, op1=ALU.add)
        S.vector.tensor_tensor(out=B["Yn"][:], in0=B["Yn"][:], in1=B["g"][:], op=ALU.mult)
        S.gpsimd.dma_start(out=y[t0:t0 + L, :], in_=B["Yn"][:])
    S.emit()
    return P.nc
TWO_PI = 6.283185307179586


def s5_host_layout(lam_re, lam_im, log_dt, b_re, b_im, c_re, c_im, d_skip, w_glu, b_glu):
    G, Pn = 32, 64
    rows = np.stack([lam_re.reshape(-1), lam_im.reshape(-1), np.repeat(log_dt, Pn)]).astype(np.float32)
    cols = np.stack([lam_re.reshape(16, 128).T, lam_im.reshape(16, 128).T, np.repeat(log_dt, Pn).reshape(16, 128).T], 0).astype(np.float32)
    bpad = np.zeros((2, 4, 128, 512), np.float32)
    for ct in range(4):
        for gl in range(8):
            g = 8 * ct + gl
            bpad[0, ct, gl * 16:(gl + 1) * 16, gl * 64:(gl + 1) * 64] = b_re[g].T
            bpad[1, ct, gl * 16:(gl + 1) * 16, gl * 64:(gl + 1) * 64] = b_im[g].T
    cpad = np.zeros((2, 16, 128, 128), np.float32)
    for st in range(16):
        for gg in range(2):
            g = 2 * st + gg
            gl = g % 8
            cpad[0, st, gg * 64:(gg + 1) * 64, gl * 16:(gl + 1) * 16] = c_re[g].T
            cpad[1, st, gg * 64:(gg + 1) * 64, gl * 16:(gl + 1) * 16] = c_im[g].T
    dcol = np.stack([d_skip.reshape(4, 128).T, b_glu.reshape(4, 128).T], 0).astype(np.float32)
    return {"rows": rows, "cols": cols, "bpad": bpad, "cpad": cpad, "dcol": dcol, "w_glu": np.ascontiguousarray(w_glu, np.float32)}


def build_s5(S_tok, P=None):
    P = P or Prog()
    S = P.S
    z = P.din("z", [S_tok, 2056])
    rows = P.din("rows", [3, 2048])
    cols = P.din("cols", [3, 128, 16])
    bpad = P.din("bpad", [2, 4, 128, 512])
    cpad = P.din("cpad", [2, 16, 128, 128])
    dcol = P.din("dcol", [2, 128, 4])
    w_glu = P.din("w_glu", [512, 512])
    y = P.dout("y", [S_tok, 512])
    I32_ = mybir.dt.int32
    R = lambda n: P.sb(n, [128, 2048])
    lr = P.bc_row("lr", rows[0:1, :], 2048)
    li = P.bc_row("li", rows[1:2, :], 2048)
    dt = P.bc_row("dtr", rows[2:3, :], 2048)
    a_r, th_r, s1, s2, s3, s4 = R("a_r"), R("th_r"), R("s1"), R("s2"), R("s3"), R("s4")
    si = P.sb("si", [128, 2048], I32_)
    Ei_re, Ei_im = R("Ei_re"), R("Ei_im")
    iota_col, iota_row = P.C("iota_col", 128, 1), P.C("iota_row")

    def sincos(turns, o_sin, o_cos, n):
        for off, o in ((0.0, o_sin), (0.25, o_cos)):
            if off:
                S.vector.tensor_scalar(out=s4[:, 0:n], in0=turns, scalar1=off, scalar2=None, op0=ALU.add)
                src = s4[:, 0:n]
            else:
                src = turns
            S.vector.tensor_copy(out=si[:, 0:n], in_=src)
            S.vector.tensor_copy(out=o, in_=si[:, 0:n])
            S.vector.tensor_tensor(out=o, in0=src, in1=o, op=ALU.subtract)
            S.scalar.activation(out=o, in_=o, func=AF.Sin, scale=TWO_PI)

    S.scalar.activation(out=dt[:], in_=dt[:], func=AF.Exp)
    S.vector.tensor_tensor(out=a_r[:], in0=lr[:], in1=dt[:], op=ALU.mult)
    S.vector.tensor_tensor(out=th_r[:], in0=li[:], in1=dt[:], op=ALU.mult)
    S.vector.tensor_scalar(out=th_r[:], in0=th_r[:], scalar1=1.0 / TWO_PI, scalar2=None, op0=ALU.mult)
    sincos(th_r[:], s1[:], s2[:], 2048)
    S.scalar.activation(out=s3[:], in_=a_r[:], func=AF.Exp)
    S.vector.tensor_tensor(out=s1[:], in0=s1[:], in1=s3[:], op=ALU.mult)
    S.vector.tensor_tensor(out=s2[:], in0=s2[:], in1=s3[:], op=ALU.mult)
    S.vector.tensor_scalar(out=s2[:], in0=s2[:], scalar1=-1.0, scalar2=None, op0=ALU.add)
    fr, fi = R("fr"), R("fi")
    S.vector.tensor_tensor(out=s3[:], in0=lr[:], in1=lr[:], op=ALU.mult)
    S.vector.tensor_tensor(out=s4[:], in0=li[:], in1=li[:], op=ALU.mult)
    S.vector.tensor_tensor(out=s3[:], in0=s3[:], in1=s4[:], op=ALU.add)
    S.vector.reciprocal(out=s3[:], in_=s3[:])
    S.vector.tensor_tensor(out=fr[:], in0=s2[:], in1=lr[:], op=ALU.mult)
    S.vector.tensor_tensor(out=s4[:], in0=s1[:], in1=li[:], op=ALU.mult)
    S.vector.tensor_tensor(out=fr[:], in0=fr[:], in1=s4[:], op=ALU.add)
    S.vector.tensor_tensor(out=fr[:], in0=fr[:], in1=s3[:], op=ALU.mult)
    S.vector.tensor_tensor(out=fi[:], in0=s1[:], in1=lr[:], op=ALU.mult)
    S.vector.tensor_tensor(out=s4[:], in0=s2[:], in1=li[:], op=ALU.mult)
    S.vector.tensor_tensor(out=fi[:], in0=fi[:], in1=s4[:], op=ALU.subtract)
    S.vector.tensor_tensor(out=fi[:], in0=fi[:], in1=s3[:], op=ALU.mult)
    bp_re, bp_im = P.sb("bp_re", [128, 4, 512]), P.sb("bp_im", [128, 4, 512])
    for ct in range(4):
        S.sync.dma_start(out=bp_re[:, ct, :], in_=bpad[0, ct])
        S.sync.dma_start(out=bp_im[:, ct, :], in_=bpad[1, ct])
    Bb_re, Bb_im = P.sb("Bb_re", [128, 2048]), P.sb("Bb_im", [128, 2048])
    bpr, bpi = bp_re[:].rearrange("p a b -> p (a b)"), bp_im[:].rearrange("p a b -> p (a b)")
    S.vector.tensor_tensor(out=Bb_re[:], in0=fr[:], in1=bpr, op=ALU.mult)
    S.vector.tensor_tensor(out=s4[:], in0=fi[:], in1=bpi, op=ALU.mult)
    S.vector.tensor_tensor(out=Bb_re[:], in0=Bb_re[:], in1=s4[:], op=ALU.subtract)
    S.vector.tensor_tensor(out=Bb_im[:], in0=fr[:], in1=bpi, op=ALU.mult)
    S.vector.tensor_tensor(out=s4[:], in0=fi[:], in1=bpr, op=ALU.mult)
    S.vector.tensor_tensor(out=Bb_im[:], in0=Bb_im[:], in1=s4[:], op=ALU.add)
    S.vector.tensor_scalar(out=s3[:], in0=th_r[:], scalar1=iota_col, scalar2=None, op0=ALU.mult)
    sincos(s3[:], s1[:], s2[:], 2048)
    S.vector.tensor_scalar(out=s3[:], in0=a_r[:], scalar1=iota_col, scalar2=-1.0, op0=ALU.mult, op1=ALU.mult)
    S.scalar.activation(out=s3[:], in_=s3[:], func=AF.Exp)
    S.vector.tensor_tensor(out=Ei_re[:], in0=s2[:], in1=s3[:], op=ALU.mult)
    S.vector.scalar_tensor_tensor(out=Ei_im[:], in0=s1[:], scalar=-1.0, in1=s3[:], op0=ALU.mult, op1=ALU.mult)
    EL_re, EL_im = fr, fi
    S.vector.tensor_scalar(out=s3[:], in0=th_r[:], scalar1=128.0, scalar2=None, op0=ALU.mult)
    sincos(s3[:], s1[:], s2[:], 2048)
    S.scalar.activation(out=s3[:], in_=a_r[:], func=AF.Exp, scale=128.0)
    S.vector.tensor_tensor(out=EL_re[:], in0=s2[:], in1=s3[:], op=ALU.mult)
    S.vector.tensor_tensor(out=EL_im[:], in0=s1[:], in1=s3[:], op=ALU.mult)
    cl = P.sb("cl", [128, 3, 16])
    for i in range(3):
        S.sync.dma_start(out=cl[:, i, :], in_=cols[i])
    S.scalar.activation(out=cl[:, 2, :], in_=cl[:, 2, :], func=AF.Exp)
    S.vector.tensor_tensor(out=cl[:, 0, :], in0=cl[:, 0, :], in1=cl[:, 2, :], op=ALU.mult)
    S.vector.tensor_tensor(out=cl[:, 1, :], in0=cl[:, 1, :], in1=cl[:, 2, :], op=ALU.mult)
    S.vector.tensor_scalar(out=cl[:, 1, :], in0=cl[:, 1, :], scalar1=1.0 / TWO_PI, scalar2=None, op0=ALU.mult)
    E_re, E_im = P.sb("E_re", [128, 16, 128]), P.sb("E_im", [128, 16, 128])
    for st in range(16):
        S.vector.tensor_scalar(out=s3[:, 0:128], in0=iota_row, scalar1=cl[:, 1, st:st + 1], scalar2=None, op0=ALU.mult)
        sincos(s3[:, 0:128], s1[:, 0:128], s2[:, 0:128], 128)
        S.scalar.activation(out=s3[:, 0:128], in_=iota_row, func=AF.Exp, scale=cl[:, 0, st:st + 1])
        S.vector.tensor_tensor(out=E_re[:, st, :], in0=s2[:, 0:128], in1=s3[:, 0:128], op=ALU.mult)
        S.vector.tensor_tensor(out=E_im[:, st, :], in0=s1[:, 0:128], in1=s3[:, 0:128], op=ALU.mult)
    cp_re, cp_im = P.sb("cp_re", [128, 16, 128]), P.sb("cp_im", [128, 16, 128])
    for st in range(16):
        S.sync.dma_start(out=cp_re[:, st, :], in_=cpad[0, st])
        S.sync.dma_start(out=cp_im[:, st, :], in_=cpad[1, st])
    dc = P.sb("dc", [128, 2, 4])
    for i in range(2):
        S.sync.dma_start(out=dc[:, i, :], in_=dcol[i])
    wg = P.sb("wg", [128, 4, 512])
    for k in range(4):
        S.sync.dma_start(out=wg[:, k, :], in_=w_glu[k * 128:(k + 1) * 128, :])
    Xr_re, Xr_im, Xn_re, Xn_im, Xt = lr[0:1, :], li[0:1, :], dt[0:1, :], th_r[0:1, :], bpr[0:1, :]
    S.vector.memset(Xr_re, 0.0)
    S.vector.memset(Xr_im, 0.0)
    ut = P.rot("ut", [128, 512])
    uT = P.rot("uT", [128, 4, 128])
    V_re, V_im = s1, s2
    x_re, nx_im = s3, a_r
    tA, tB = s4[:, 0:512], s4[:, 512:1024]
    gl = P.sb("gl", [128, 4, 128])
    g1, g2 = P.sb("g1", [128, 128]), P.sb("g2", [128, 128])
    yaT = P.sb("yaT", [128, 4, 128])
    yt = ut
    pT = [S.ps(f"pT{i}", [128, 512]) for i in range(2)]
    pB = [S.ps(f"pB{i}", [128, 512]) for i in range(2)]
    pS_ = [S.ps(f"pS{i}", [128, 512]) for i in range(2)]
    pY = S.ps("pY", [128, 512])
    pX = S.ps("pX", [128, 512])
    ident, tri, ones = P.C("ident"), P.C("tri"), P.C("ones")
    for t in range(S_tok // 128):
        t0 = t * 128
        b = t % 2
        S.sync.dma_start(out=ut[b][:], in_=z[t0:t0 + 128, 0:512])
        for k in range(4):
            P.transpose(uT[b][:, k, :], ut[b][:, k * 128:(k + 1) * 128], pT[k % 2][:, 0:128], eng="vector" if k % 2 == 0 else "scalar")
        for ct in range(4):
            cs = slice(ct * 512, (ct + 1) * 512)
            S.tensor.matmul(pB[0][:], lhsT=uT[b][:, ct, :], rhs=Bb_re[:, cs], start=True, stop=True)
            S.tensor.matmul(pB[1][:], lhsT=uT[b][:, ct, :], rhs=Bb_im[:, cs], start=True, stop=True)
            S.vector.tensor_tensor(out=V_re[:, cs], in0=pB[0][:], in1=Ei_re[:, cs], op=ALU.mult)
            S.vector.tensor_tensor(out=tA, in0=pB[1][:], in1=Ei_im[:, cs], op=ALU.mult)
            S.vector.tensor_tensor(out=V_re[:, cs], in0=V_re[:, cs], in1=tA, op=ALU.subtract)
            S.vector.tensor_tensor(out=V_im[:, cs], in0=pB[0][:], in1=Ei_im[:, cs], op=ALU.mult)
            S.vector.tensor_tensor(out=tB, in0=pB[1][:], in1=Ei_re[:, cs], op=ALU.mult)
            S.vector.tensor_tensor(out=V_im[:, cs], in0=V_im[:, cs], in1=tB, op=ALU.add)
        S.vector.tensor_tensor(out=V_re[0:1, :], in0=V_re[0:1, :], in1=Xr_re, op=ALU.add)
        S.vector.tensor_tensor(out=V_im[0:1, :], in0=V_im[0:1, :], in1=Xr_im, op=ALU.add)
        for q in range(4):
            S.tensor.matmul(pX[:, :], lhsT=ones, rhs=V_re[:, q * 512:(q + 1) * 512], start=True, stop=True)
            S.vector.tensor_copy(out=Xn_re[:, q * 512:(q + 1) * 512], in_=pX[0:1, :])
            S.tensor.matmul(pX[:, :], lhsT=ones, rhs=V_im[:, q * 512:(q + 1) * 512], start=True, stop=True)
            S.vector.tensor_copy(out=Xn_im[:, q * 512:(q + 1) * 512], in_=pX[0:1, :])
        S.vector.tensor_tensor(out=Xr_re, in0=Xn_re, in1=EL_re[0:1, :], op=ALU.mult)
        S.vector.tensor_tensor(out=Xt, in0=Xn_im, in1=EL_im[0:1, :], op=ALU.mult)
        S.vector.tensor_tensor(out=Xr_re, in0=Xr_re, in1=Xt, op=ALU.subtract)
        S.vector.tensor_tensor(out=Xr_im, in0=Xn_re, in1=EL_im[0:1, :], op=ALU.mult)
        S.vector.tensor_tensor(out=Xt, in0=Xn_im, in1=EL_re[0:1, :], op=ALU.mult)
        S.vector.tensor_tensor(out=Xr_im, in0=Xr_im, in1=Xt, op=ALU.add)
        for ft in range(4):
            for q in range(4):
                st = ft * 4 + q
                S.tensor.matmul(pS_[0][:, q * 128:(q + 1) * 128], lhsT=V_re[:, st * 128:(st + 1) * 128], rhs=tri, start=True, stop=True)
                S.tensor.matmul(pS_[1][:, q * 128:(q + 1) * 128], lhsT=V_im[:, st * 128:(st + 1) * 128], rhs=tri, start=True, stop=True)
            cs = slice(ft * 512, (ft + 1) * 512)
            Er = E_re[:, ft * 4:(ft + 1) * 4, :].rearrange("p a b -> p (a b)")
            Em = E_im[:, ft * 4:(ft + 1) * 4, :].rearrange("p a b -> p (a b)")
            S.vector.tensor_tensor(out=x_re[:, cs], in0=pS_[0][:], in1=Er, op=ALU.mult)
            S.vector.tensor_tensor(out=tA, in0=pS_[1][:], in1=Em, op=ALU.mult)
            S.vector.tensor_tensor(out=x_re[:, cs], in0=x_re[:, cs], in1=tA, op=ALU.subtract)
            S.vector.tensor_tensor(out=nx_im[:, cs], in0=pS_[1][:], in1=Er, op=ALU.mult)
            S.vector.tensor_tensor(out=tB, in0=pS_[0][:], in1=Em, op=ALU.mult)
            S.vector.scalar_tensor_tensor(out=nx_im[:, cs], in0=nx_im[:, cs], scalar=-1.0, in1=tB, op0=ALU.mult, op1=ALU.subtract)
            for q in range(4):
                st = ft * 4 + q
                S.tensor.matmul(pY[:, 0:128], lhsT=cp_re[:, st, :], rhs=x_re[:, st * 128:(st + 1) * 128], start=(q == 0), stop=False)
                S.tensor.matmul(pY[:, 0:128], lhsT=cp_im[:, st, :], rhs=nx_im[:, st * 128:(st + 1) * 128], start=False, stop=(q == 3))
            S.vector.scalar_tensor_tensor(out=g1[:], in0=uT[b][:, ft, :], scalar=dc[:, 0, ft:ft + 1], in1=pY[:, 0:128], op0=ALU.mult, op1=ALU.add)
            S.vector.tensor_tensor(out=g2[:], in0=g1[:], in1=g1[:], op=ALU.mult)
            S.vector.tensor_scalar(out=g2[:], in0=g2[:], scalar1=0.044715, scalar2=1.0, op0=ALU.mult, op1=ALU.add)
            S.vector.tensor_tensor(out=g2[:], in0=g2[:], in1=g1[:], op=ALU.mult)
            S.scalar.activation(out=g2[:], in_=g2[:], func=AF.Tanh, scale=0.7978845608028654)
            S.vector.scalar_tensor_tensor(out=g2[:], in0=g2[:], scalar=1.0, in1=g1[:], op0=ALU.add, op1=ALU.mult)
            S.vector.tensor_scalar(out=gl[:, ft, :], in0=g2[:], scalar1=0.5, scalar2=None, op0=ALU.mult)
        for f2 in range(4):
            for ft in range(4):
                S.tensor.matmul(pY[:, 128:256], lhsT=wg[:, ft, f2 * 128:(f2 + 1) * 128], rhs=gl[:, ft, :], start=(ft == 0), stop=(ft == 3))
            S.scalar.activation(out=g1[:], in_=pY[:, 128:256], func=AF.Sigmoid, bias=dc[:, 1, f2:f2 + 1], scale=1.0)
            S.vector.tensor_tensor(out=yaT[:, f2, :], in0=g1[:], in1=gl[:, f2, :], op=ALU.mult)
        for k in range(4):
            P.transpose(yt[b][:, k * 128:(k + 1) * 128], yaT[:, k, :], pT[k % 2][:, 0:128], eng="vector" if k % 2 == 0 else "scalar")
        S.gpsimd.dma_start(out=y[t0:t0 + 128, :], in_=yt[b][:])
    S.emit()
    return P.nc
def _stage(nc, idx, fn, *args, io):
    with nc.cleanup_on_exit():
        P = Prog(nc=nc, prefix=f"s{idx}_", io=io)
        fn(*args, P=P)
        nc.all_engine_barrier()


def build_fused(S_tok, shapes):
    nc = bass.Bass("TRN2", target_bir_lowering=False)

    def ein(name):
        return nc.dram_tensor(name, list(shapes[name]), F32, kind="ExternalInput").ap()

    def internal(name, shape):
        return nc.dram_tensor(name, list(shape), F32).ap()

    consts = ein("consts")
    xa = ein("x")
    out = nc.dram_tensor("out", [S_tok, 1024], F32, kind="ExternalOutput").ap()
    sid = 0
    for layer in range(4):
        L = f"L{layer}_"
        xo = out if layer == 3 else internal(f"xact{layer}", [S_tok, 1024])
        ya = internal(f"ya{layer}", [S_tok, 512])
        yb = internal(f"yb{layer}", [S_tok, 512])
        if layer % 2 == 0:
            z = internal(f"z{layer}", [S_tok, 2056])
            _stage(nc, sid, build_proj, S_tok, 2056, io={"consts": consts, "x": xa, "w": ein(L + "w_in"), "z": z}); sid += 1
            io = {"consts": consts, "z": z, "y": ya}
            for k in ("rows", "cols", "bpad", "cpad", "dcol", "w_glu"):
                io[k] = ein(L + k)
            _stage(nc, sid, build_s5, S_tok, io=io); sid += 1
            _stage(nc, sid, build_mlstm, S_tok, io={"consts": consts, "z": z, "y": yb, "gb": ein(L + "gb"), "ng": ein(L + "ng")}); sid += 1
            io = {"consts": consts, "x": xa, "ya": ya, "yb": yb, "xo": xo}
            for k in ("w_out", "lnp", "wg", "wu", "wd"):
                io[k] = ein(L + k)
            _stage(nc, sid, build_cf, S_tok, 1, 2816, io=io); sid += 1
        else:
            z = internal(f"z{layer}", [S_tok, 3848])
            _stage(nc, sid, build_proj, S_tok, 3848, io={"consts": consts, "x": xa, "w": ein(L + "w_in"), "z": z}); sid += 1
            io = {"consts": consts, "z": z, "y": ya}
            for k in ("vecs", "mu", "w_up", "a_up", "g_up"):
                io[k] = ein(L + k)
            _stage(nc, sid, build_rwkv, S_tok, io=io); sid += 1
            io = {"consts": consts, "z": z, "y": yb}
            for k in ("conv_w", "a_log", "dt_bias", "norm_g"):
                io[k] = ein(L + k)
            _stage(nc, sid, build_gdn, S_tok, 99, io=io); sid += 1
            io = {"consts": consts, "x": xa, "ya": ya, "yb": yb, "xo": xo}
            for k in ("w_out", "lnp", "wg", "wu", "wd", "router"):
                io[k] = ein(L + k)
            _stage(nc, sid, build_cf, S_tok, 8, 3584, io=io); sid += 1
        xa = xo
    return nc


def host_inputs(inp):
    W = {"consts": CONST_ARR}
    for layer in range(4):
        i = layer // 2
        L = f"L{layer}_"
        W[L + "lnp"] = np.stack([inp["ln_g"][layer, 0], inp["ln_b"][layer, 0], inp["ln_g"][layer, 1], inp["ln_b"][layer, 1]])
        if layer % 2 == 0:
            W[L + "w_in"] = inp["ev_w_in"][i]
            lay = s5_host_layout(inp["s5_lam_re"][i], inp["s5_lam_im"][i], inp["s5_log_dt"][i], inp["s5_b_re"][i], inp["s5_b_im"][i],
                                 inp["s5_c_re"][i], inp["s5_c_im"][i], inp["s5_d"][i], inp["s5_w_glu"][i], inp["s5_b_glu"][i])
            for k, v in lay.items():
                W[L + k] = v
            W[L + "gb"] = inp["ml_gate_bias"][i][None]
            W[L + "ng"] = inp["ml_norm_g"][i][None]
            W[L + "w_out"] = inp["ev_w_out"][i]
            W[L + "wg"] = inp["ffn_w_gate"][i][None]
            W[L + "wu"] = inp["ffn_w_up"][i][None]
            W[L + "wd"] = inp["ffn_w_down"][i][None]
        else:
            W[L + "w_in"] = inp["od_w_in"][i]
            W[L + "vecs"] = np.stack([inp["rw_w0"][i], inp["rw_a0"][i], inp["rw_k_k"][i], inp["rw_k_a"][i], inp["rw_r_k"][i],
                                      inp["rw_ln_g"][i], inp["rw_ln_b"][i], inp["rw_ln_b"][i]])
            W[L + "mu"] = inp["rw_mu"][i][None]
            W[L + "w_up"] = inp["rw_w_up"][i]
            W[L + "a_up"] = inp["rw_a_up"][i]
            W[L + "g_up"] = inp["rw_g_up"][i]
            W[L + "conv_w"] = inp["gd_conv"][i]
            W[L + "a_log"] = inp["gd_a_log"][i][None]
            W[L + "dt_bias"] = inp["gd_dt_bias"][i][None]
            W[L + "norm_g"] = inp["gd_norm_g"][i][None]
            W[L + "w_out"] = inp["od_w_out"][i]
            W[L + "wg"] = inp["moe_w_gate"][i]
            W[L + "wu"] = inp["moe_w_up"][i]
            W[L + "wd"] = inp["moe_w_down"][i]
            W[L + "router"] = inp["moe_router"][i]
    return {k: np.ascontiguousarray(v, dtype=np.float32) for k, v in W.items()}


def kernel(_n_cores=8, **inp):
    inp = {k: np.asarray(v, dtype=np.float32) for k, v in inp.items()}
    S_tok = inp["x"].shape[1]
    W = host_inputs(inp)
    shapes = {k: v.shape for k, v in W.items()}
    shapes["x"] = (S_tok, 1024)
    nc = build_fused(S_tok, shapes)
    in_maps = []
    for c in range(_n_cores):
        d = dict(W)
        d["x"] = np.ascontiguousarray(inp["x"][c])
        in_maps.append(d)
    res = run_bass_kernel_spmd(nc, in_maps, core_ids=list(range(_n_cores)))
    return np.stack([np.asarray(r["out"]) for r in res.results]).astype(np.float32)
```
